# Optimizing a Trainium2 kernel written in Bass

```python
import jax, jax.numpy as jnp
from jax import lax
import numpy as np

D_MODEL = 1024
BATCH = 8
SEQ = 8192
DEPTH = 1

RWKV_HEAD = 64
RWKV_HEADS = D_MODEL // RWKV_HEAD
RWKV_WIDTH = RWKV_HEADS * RWKV_HEAD
DECAY_LORA = 64
ICLR_LORA = 64
GATE_LORA = 128
LNX_EPS = 64e-5
ATTN_HEAD = 64
ATTN_Q_HEADS = 16
ATTN_KV_HEADS = 2
ATTN_GROUP = ATTN_Q_HEADS // ATTN_KV_HEADS
ATTN_WIDTH = ATTN_Q_HEADS * ATTN_HEAD
KV_WIDTH = ATTN_KV_HEADS * ATTN_HEAD
WINDOW = 128
BLOCK = 128
ATTN_SCALE = ATTN_HEAD ** -0.5
NEG_INF = -1e30
N_KEYS = 128
N_EXPERTS = N_KEYS * N_KEYS
PEER_HEADS = 8
PEER_QDIM = 256
PEER_HALF = PEER_QDIM // 2
PEER_TOPK = 16
PEER_CHUNK = 128
NORM_EPS = 1e-6
N_ADA = 6
N_SHIFT = 3 * RWKV_WIDTH + DECAY_LORA + ICLR_LORA + GATE_LORA
SHIFT_SPLITS = (RWKV_WIDTH, 2 * RWKV_WIDTH, 3 * RWKV_WIDTH,
                3 * RWKV_WIDTH + DECAY_LORA, 3 * RWKV_WIDTH + DECAY_LORA + ICLR_LORA)
REST_SPLITS = (ATTN_WIDTH, ATTN_WIDTH + KV_WIDTH, ATTN_WIDTH + 2 * KV_WIDTH,
               ATTN_WIDTH + 2 * KV_WIDTH + D_MODEL)
IN_WIDTH = N_SHIFT + ATTN_WIDTH + 2 * KV_WIDTH + 2 * D_MODEL

kernel_name = 'rwkv7_swa_sink_peer_hybrid'


def rms_norm(x, w):
    xf = x.astype(jnp.float32)
    y = xf * lax.rsqrt(jnp.mean(xf * xf, axis=-1, keepdims=True) + NORM_EPS)
    return (y * w.astype(jnp.float32)).astype(x.dtype)


def token_shift(p):
    return jnp.pad(p, ((0, 0), (1, 0), (0, 0)))[:, :-1, :]


def rwkv7_recurrence(r, decay, k, v, a, b):
    bsz, _, n_h, n = r.shape
    xs = tuple(jnp.moveaxis(t.astype(jnp.float32), 1, 0) for t in (r, decay, k, v, a, b))

    def step(state, inp):
        r_t, w_t, k_t, v_t, a_t, b_t = inp
        sa = jnp.einsum('bhvk,bhk->bhv', state, a_t)
        state = (state * w_t[:, :, None, :] + sa[..., None] * b_t[:, :, None, :]
                 + v_t[..., None] * k_t[:, :, None, :])
        return state, jnp.einsum('bhvk,bhk->bhv', state, r_t)

    state0 = jnp.zeros((bsz, n_h, n, n), jnp.float32)
    _, ys = lax.scan(step, state0, xs)
    return jnp.moveaxis(ys, 0, 1)


def rwkv7_mixer(pr, pk, pv, pw, pa, pg, w0, w_up, a0, a_up, g_up, k_k, k_a, r_k, lnx_w, lnx_b):
    bsz, seq, _ = pr.shape
    f32 = jnp.float32
    heads = lambda t: t.reshape(bsz, seq, RWKV_HEADS, RWKV_HEAD)
    w_log = -jax.nn.softplus(-(w0 + jnp.tanh(pw) @ w_up).astype(f32)) - 0.5
    decay = jnp.exp(-jnp.exp(w_log))
    a = jax.nn.sigmoid((a0 + pa @ a_up).astype(f32))
    g = jax.nn.sigmoid(pg) @ g_up
    kk = heads((pk * k_k).astype(f32))
    kk = kk / jnp.maximum(jnp.sqrt(jnp.sum(kk * kk, axis=-1, keepdims=True)), 1e-12)
    k = pk.astype(f32) * (1.0 + (a - 1.0) * k_a)
    r_h, k_h, v_h, a_h = heads(pr.astype(f32)), heads(k), heads(pv.astype(f32)), heads(a)
    y = rwkv7_recurrence(r_h, heads(decay), k_h, v_h, -kk, kk * a_h)
    mu = jnp.mean(y, axis=-1, keepdims=True)
    var = jnp.mean(jnp.square(y - mu), axis=-1, keepdims=True)
    y = ((y - mu) * lax.rsqrt(var + LNX_EPS)).reshape(bsz, seq, RWKV_WIDTH) * lnx_w + lnx_b
    bonus = jnp.sum(r_h * k_h * r_k, axis=-1, keepdims=True) * v_h
    y = (y + bonus.reshape(bsz, seq, RWKV_WIDTH)) * g
    return y.astype(pr.dtype)


def swa_sink_attention(q, k, v, q_norm_w, k_norm_w, sinks):
    bsz, seq, _ = q.shape
    nb = seq // BLOCK
    f32 = jnp.float32
    q = rms_norm(q.reshape(bsz, seq, ATTN_Q_HEADS, ATTN_HEAD), q_norm_w)
    k = rms_norm(k.reshape(bsz, seq, ATTN_KV_HEADS, ATTN_HEAD), k_norm_w)
    v = v.reshape(bsz, seq, ATTN_KV_HEADS, ATTN_HEAD)
    qb = q.reshape(bsz, nb, BLOCK, ATTN_KV_HEADS, ATTN_GROUP, ATTN_HEAD)

    def band(t):
        tb = t.reshape(bsz, nb, BLOCK, ATTN_KV_HEADS, ATTN_HEAD)
        prev = jnp.pad(tb, ((0, 0), (1, 0), (0, 0), (0, 0), (0, 0)))[:, :-1]
        return jnp.concatenate([prev, tb], axis=2)

    kb, vb = band(k), band(v)
    sink = sinks.astype(f32).reshape(ATTN_KV_HEADS, ATTN_GROUP)[None, :, :, None]
    qi = jnp.arange(BLOCK)[:, None]
    kj = jnp.arange(2 * BLOCK)[None, :]
    rel = kj - qi
    in_window = (rel >= BLOCK - WINDOW + 1) & (rel <= BLOCK)

    def one_block(args):
        q_blk, k_blk, v_blk, blk = args
        s = jnp.einsum('bqgnd,bkgd->bgnqk', q_blk, k_blk, preferred_element_type=f32) * ATTN_SCALE
        valid = in_window & (blk * BLOCK - BLOCK + kj >= 0)
        s = jnp.where(valid, s, NEG_INF)
        m = jnp.maximum(jnp.max(s, axis=-1), sink)
        p = jnp.exp(s - m[..., None])
        denom = jnp.sum(p, axis=-1) + jnp.exp(sink - m)
        p = p / denom[..., None]
        return jnp.einsum('bgnqk,bkgd->bqgnd', p.astype(v_blk.dtype), v_blk)

    out = lax.map(one_block, (jnp.moveaxis(qb, 1, 0), jnp.moveaxis(kb, 1, 0),
                              jnp.moveaxis(vb, 1, 0), jnp.arange(nb)))
    return jnp.moveaxis(out, 0, 1).reshape(bsz, seq, ATTN_WIDTH)


def peer_ffn(h, w_q, keys_1, keys_2, u_tab, v_tab):
    bsz, seq, d = h.shape
    chunks = h.reshape(bsz * seq // PEER_CHUNK, PEER_CHUNK, d)

    def one_chunk(xc):
        q = (xc @ w_q).reshape(PEER_CHUNK, PEER_HEADS, 2, PEER_HALF)
        s1 = jnp.einsum('chd,nd->chn', q[:, :, 0], keys_1)
        s2 = jnp.einsum('chd,nd->chn', q[:, :, 1], keys_2)
        v1, i1 = lax.top_k(s1, PEER_TOPK)
        v2, i2 = lax.top_k(s2, PEER_TOPK)
        cand = (v1[..., :, None] + v2[..., None, :]).reshape(PEER_CHUNK, PEER_HEADS, PEER_TOPK * PEER_TOPK)
        cidx = (i1[..., :, None] * N_KEYS + i2[..., None, :]).reshape(PEER_CHUNK, PEER_HEADS, PEER_TOPK * PEER_TOPK)
        sc, pos = lax.top_k(cand, PEER_TOPK)
        idx = jnp.take_along_axis(cidx, pos, axis=-1)
        gate = jax.nn.softmax(sc.astype(jnp.float32), axis=-1)
        u = jnp.take(u_tab, idx, axis=0)
        act = jax.nn.gelu(jnp.einsum('chkd,cd->chk', u, xc).astype(jnp.float32), approximate=False)
        wts = (gate * act).astype(xc.dtype)
        return jnp.einsum('chk,chkd->cd', wts, jnp.take(v_tab, idx, axis=0))

    return lax.map(one_chunk, chunks).reshape(bsz, seq, d)


def setup_inputs(seed: int = 0) -> dict:
    key = jax.random.key(seed)
    ks = jax.random.split(key, 27)
    f32 = jnp.float32
    nrm = lambda k, shape, s: jax.random.normal(k, shape, f32) * s
    L = DEPTH
    return {
        'x': nrm(ks[0], (BATCH, SEQ, D_MODEL), 1.0),
        'c': nrm(ks[1], (BATCH, D_MODEL), 1.0),
        'ada_w': nrm(ks[2], (L, D_MODEL, N_ADA * D_MODEL), 0.5 * D_MODEL ** -0.5),
        'ada_b': nrm(ks[3], (L, N_ADA * D_MODEL), 0.02),
        'norm1_w': 1.0 + nrm(ks[4], (L, D_MODEL), 0.05),
        'norm2_w': 1.0 + nrm(ks[5], (L, D_MODEL), 0.05),
        'w_in': nrm(ks[6], (L, D_MODEL, IN_WIDTH), D_MODEL ** -0.5),
        'shift_mu': jax.random.uniform(ks[7], (L, N_SHIFT), f32),
        'w0': jax.random.uniform(ks[8], (L, RWKV_WIDTH), f32, -5.0, 1.0),
        'w_lora_up': nrm(ks[9], (L, DECAY_LORA, RWKV_WIDTH), DECAY_LORA ** -0.5),
        'a0': nrm(ks[10], (L, RWKV_WIDTH), 0.5),
        'a_lora_up': nrm(ks[11], (L, ICLR_LORA, RWKV_WIDTH), ICLR_LORA ** -0.5),
        'g_lora_up': nrm(ks[12], (L, GATE_LORA, RWKV_WIDTH), GATE_LORA ** -0.5),
        'k_k': 0.85 + nrm(ks[13], (L, RWKV_WIDTH), 0.1),
        'k_a': 1.0 + nrm(ks[14], (L, RWKV_WIDTH), 0.1),
        'r_k': nrm(ks[15], (L, RWKV_HEADS, RWKV_HEAD), 0.1),
        'lnx_w': 1.0 + nrm(ks[16], (L, RWKV_WIDTH), 0.05),
        'lnx_b': nrm(ks[17], (L, RWKV_WIDTH), 0.02),
        'q_norm_w': 1.0 + nrm(ks[18], (L, ATTN_HEAD), 0.05),
        'k_norm_w': 1.0 + nrm(ks[19], (L, ATTN_HEAD), 0.05),
        'sinks': nrm(ks[20], (L, ATTN_Q_HEADS), 0.5),
        'w_out': nrm(ks[21], (L, D_MODEL, D_MODEL), D_MODEL ** -0.5),
        'peer_w_q': nrm(ks[22], (L, D_MODEL, PEER_HEADS * PEER_QDIM), D_MODEL ** -0.5),
        'peer_keys_1': nrm(ks[23], (L, N_KEYS, PEER_HALF), PEER_HALF ** -0.5),
        'peer_keys_2': nrm(ks[24], (L, N_KEYS, PEER_HALF), PEER_HALF ** -0.5),
        'peer_u': nrm(ks[25], (L, N_EXPERTS, D_MODEL), D_MODEL ** -0.5),
        'peer_v': nrm(ks[26], (L, N_EXPERTS, D_MODEL), PEER_HEADS ** -0.5),
    }


def reference(x, c, ada_w, ada_b, norm1_w, norm2_w, w_in, shift_mu, w0, w_lora_up, a0,
              a_lora_up, g_lora_up, k_k, k_a, r_k, lnx_w, lnx_b, q_norm_w, k_norm_w, sinks,
              w_out, peer_w_q, peer_keys_1, peer_keys_2, peer_u, peer_v):
    cond = jax.nn.silu(c)
    for l in range(DEPTH):
        ada = cond @ ada_w[l] + ada_b[l]
        sh1, sc1, gt1, sh2, sc2, gt2 = [t[:, None, :] for t in jnp.split(ada, N_ADA, axis=-1)]

        h = rms_norm(x, norm1_w[l]) * (1.0 + sc1) + sh1
        proj = h @ w_in[l]
        shifted, rest = proj[..., :N_SHIFT], proj[..., N_SHIFT:]
        shifted = shifted + (token_shift(shifted) - shifted) * shift_mu[l]
        pr, pk, pv, pw, pa, pg = jnp.split(shifted, SHIFT_SPLITS, axis=-1)
        aq, ak, av, gate_a, gate_b = jnp.split(rest, REST_SPLITS, axis=-1)
        y_a = rwkv7_mixer(pr, pk, pv, pw, pa, pg, w0[l], w_lora_up[l], a0[l], a_lora_up[l],
                          g_lora_up[l], k_k[l], k_a[l], r_k[l], lnx_w[l], lnx_b[l])
        y_b = swa_sink_attention(aq, ak, av, q_norm_w[l], k_norm_w[l], sinks[l])
        mixed = jax.nn.sigmoid(gate_a) * y_a + jax.nn.sigmoid(gate_b) * y_b
        x = x + gt1 * (mixed @ w_out[l])

        h2 = rms_norm(x, norm2_w[l]) * (1.0 + sc2) + sh2
        x = x + gt2 * peer_ffn(h2, peer_w_q[l], peer_keys_1[l], peer_keys_2[l], peer_u[l], peer_v[l])
    return x
```

```python
import numpy as np
import os
STOP = int(os.environ.get('STOP', '99'))
from contextlib import ExitStack
import concourse.bass as bass
import concourse.mybir as mybir
from concourse.bass_utils import run_bass_kernel_spmd

F32 = mybir.dt.float32
U32 = mybir.dt.uint32
I32 = mybir.dt.int32
ALU = mybir.AluOpType
AF = mybir.ActivationFunctionType
AX = mybir.AxisListType

D = 1024
NSH = 3328
INW = 6656
P = 128

ENG_EPOCH = int(os.environ.get('ENG_EPOCH', '30000'))
LANE_EPOCH = int(os.environ.get('LANE_EPOCH', '1900'))
N_LANES = 16


class Res:
    __slots__ = ("name", "w", "rd", "excl")

    def __init__(self, name="", excl=False):
        self.name = name
        self.w = None
        self.rd = {}
        self.excl = excl


def RL(n, name=""):
    return [Res(f"{name}{i}") for i in range(n)]


class Sched:
    def __init__(self, nc):
        self.nc = nc
        self.streams = {k: [] for k in ("pe", "act", "dve", "pool", "sp")}
        self.sems = {}
        self.ecount = {k: 0 for k in self.streams}
        self.eepoch = {k: 0 for k in self.streams}
        self.known = {k: {} for k in self.streams}
        self.lanes = {k: [[0, 0] for _ in range(N_LANES)] for k in self.streams}
        self.lane_i = {k: 0 for k in self.streams}
        self.n_ops = 0

    def op(self, eng, fn, reads=(), writes=(), dma=False):
        self.n_ops += 1
        deps = {}

        def add(ev):
            if ev is None:
                return
            k, v = ev
            if deps.get(k, 0) < v:
                deps[k] = v

        xr = [r for r in reads if r.excl]
        if xr:
            reads = [r for r in reads if not r.excl]
            writes = list(writes) + xr
        for r in reads:
            add(r.w)
        for w in writes:
            add(w.w)
            for k, v in w.rd.items():
                add((k, v))
        if dma:
            li = self.lane_i[eng]
            self.lane_i[eng] = (li + 1) % N_LANES
            lane = self.lanes[eng][li]
            if lane[1] >= LANE_EPOCH:
                add((("lane", eng, li, lane[0]), lane[1] * 16))
                lane[0] += 1
                lane[1] = 0
            key = ("lane", eng, li, lane[0])
            if lane[1] > 0:
                add((key, lane[1] * 16))
            lane[1] += 1
            ev = (key, lane[1] * 16)
            inc = 16
        else:
            if self.ecount[eng] >= ENG_EPOCH:
                self.eepoch[eng] += 1
                self.ecount[eng] = 0
            key = ("eng", eng, self.eepoch[eng])
            self.ecount[eng] += 1
            ev = (key, self.ecount[eng])
            inc = 1
        waits = []
        kn = self.known[eng]
        for k, v in deps.items():
            if eng == "pe" and k[0] == "eng" and k[1] == "pe":
                continue
            if kn.get(k, 0) >= v:
                continue
            kn[k] = v
            waits.append((k, v))
        self.streams[eng].append((waits, fn, ev[0], inc))
        for w in writes:
            w.w = ev
            w.rd = {}
        for r in reads:
            if r.rd.get(ev[0], 0) < ev[1]:
                r.rd[ev[0]] = ev[1]
        return ev

    def barrier(self):
        evs = []
        for e in self.streams:
            if self.ecount[e] > 0:
                evs.append((("eng", e, self.eepoch[e]), self.ecount[e]))
        for le in self.lanes:
            for li, lane in enumerate(self.lanes[le]):
                if lane[1] > 0:
                    evs.append((("lane", le, li, lane[0]), lane[1] * 16))
        for e in self.streams:
            waits = []
            for k, v in evs:
                if k[0] == "eng" and k[1] == e:
                    continue
                if self.known[e].get(k, 0) >= v:
                    continue
                self.known[e][k] = v
                waits.append((k, v))
            self.streams[e].append((waits, None, None, 0))

    def final_wait(self, eng, evs):
        self.streams[eng].append((list(evs), None, None, 0))

    def emit(self):
        nc = self.nc
        for eng, st in self.streams.items():
            for waits, fn, key, inc in st:
                for k, v in waits:
                    if k not in self.sems:
                        self.sems[k] = nc.alloc_semaphore("s%d" % len(self.sems))
                if key is not None and key not in self.sems:
                    self.sems[key] = nc.alloc_semaphore("s%d" % len(self.sems))
        streams = self.streams
        self.streams = {k: [] for k in streams}
        with nc.Block() as block:
            def mk(engname):
                def body(e):
                    for waits, fn, key, inc in streams[engname]:
                        for k, v in waits:
                            e.wait_ge(self.sems[k], v)
                        if fn is not None:
                            fn(e).then_inc(self.sems[key], inc)
                return body
            block.tensor(mk("pe"))
            block.scalar(mk("act"))
            block.vector(mk("dve"))
            block.gpsimd(mk("pool"))
            block.sync(mk("sp"))


class Ctx:
    def __init__(self, nc):
        self.nc = nc
        self.S = Sched(nc)
        self.n = 0

    def sb(self, es, shape, dt=F32, name=None):
        self.n += 1
        return es.enter_context(self.nc.sbuf_tensor(name or f"t{self.n}", list(shape), dt))

    def ps(self, es, shape, dt=F32, name=None):
        self.n += 1
        return es.enter_context(self.nc.psum_tensor(name or f"p{self.n}", list(shape), dt))


from collections import deque
STG = int(os.environ.get('STG', '9'))

C0 = float(np.exp(-0.5))


class TL:
    __slots__ = ("t", "r")

    def __init__(self, t, r):
        self.t = t
        self.r = r


class FPool:
    def __init__(self, items):
        self.q = deque(items)

    def get(self):
        self.out = getattr(self, "out", 0) + 1
        self.peak = max(getattr(self, "peak", 0), self.out)
        return self.q.popleft()

    def put(self, *items):
        for it in items:
            self.out -= 1
            self.q.append(it)


def build(S_LEN, dbg=None, phases=(1, 2)):
    nc = bass.Bass("TRN2", target_bir_lowering=False)
    NT = S_LEN // P
    C = Ctx(nc)
    S = C.S
    op = S.op

    def din(name, shape, dt=F32):
        return nc.dram_tensor(name, list(shape), dt, kind="ExternalInput").ap()

    x_d = din("x", [S_LEN, D])
    cT_d = din("cT", [P, 8])
    ada_w_d = din("ada_w", [D, 6 * D])
    ada_b_d = din("ada_b", [1, 6 * D])
    n1w_d = din("norm1_w", [1, D])
    n2w_d = din("norm2_w", [1, D])
    w_in_d = din("w_in", [D, INW])
    mu_d = din("shift_mu", [1, NSH])
    w0a0_d = din("w0a0", [1, 2 * D])
    wa_up_d = din("wa_up", [P, D])
    g_up_d = din("g_up", [P, D])
    vec6_d = din("vec6", [6, D])
    qk_nw_d = din("qk_nw", [1, 128])
    sinks_d = din("sinks", [1, 16])
    w_out_d = din("w_out", [D, D])
    wq_d = din("peer_w_q", [D, 2048])
    keysT_d = din("keysT", [P, 256])
    pu_d = din("peer_u", [16384, D])
    pv_d = din("peer_v", [16384, D])
    out_d = nc.dram_tensor("out", [S_LEN, D], F32, kind="ExternalOutput").ap()
    dbg_d = {}

    def dump(name, ap_sb, shape, reads, row0=None, nrows=None, total_rows=None):
        if not dbg or name not in dbg:
            return
        if name not in dbg_d:
            dbg_d[name] = nc.dram_tensor("dbg_" + name, [total_rows or shape[0]] + list(shape[1:]), F32,
                                         kind="ExternalOutput").ap()
        dst = dbg_d[name] if row0 is None else dbg_d[name][row0:row0 + nrows]
        ev = op("sp", lambda e: e.dma_start(out=dst, in_=ap_sb), reads, [], dma=True)
        out_evs.append(ev)

    ada_s = nc.dram_tensor("ada_s", [1, 6 * D], F32, kind="Internal").ap()
    w1_s = nc.dram_tensor("w1_s", [D, NSH], F32, kind="Internal").ap()
    w2_s = nc.dram_tensor("w2_s", [D, NSH], F32, kind="Internal").ap()

    out_evs = []
    r_w1s = Res(); r_w2s = Res(); r_x1 = RL(NT)

    def tt(eng, out, in0, in1, opc, R, W):
        return op(eng, lambda e: e.tensor_tensor(out=out, in0=in0, in1=in1, op=opc), R, W)

    def ts(eng, out, in0, s1, s2, o0, o1, R, W):
        if o1 is None:
            return op(eng, lambda e: e.tensor_scalar(out=out, in0=in0, scalar1=s1, scalar2=None, op0=o0), R, W)
        return op(eng, lambda e: e.tensor_scalar(out=out, in0=in0, scalar1=s1, scalar2=s2, op0=o0, op1=o1), R, W)

    def stt(out, in0, sc, in1, o0, o1, R, W):
        return op("dve", lambda e: e.scalar_tensor_tensor(out=out, in0=in0, scalar=sc, in1=in1, op0=o0, op1=o1), R, W)

    def act(out, in_, func, R, W, scale=None, bias=None, accum=None):
        kw = {}
        if scale is not None:
            kw["scale"] = scale
        if bias is not None:
            kw["bias"] = bias
        if accum is not None:
            kw["accum_out"] = accum
        return op("act", lambda e: e.activation(out=out, in_=in_, func=func, **kw), R, W)

    def mm(out, lhsT, rhs, start, stop, R, W):
        return op("pe", lambda e: e.matmul(out, lhsT, rhs, start=start, stop=stop), R, W)

    def tr(out, in_, R, W):
        return op("pe", lambda e: e.transpose(out, in_, ident[0:in_.shape[0], 0:in_.shape[0]]), list(R) + [r_ident], W)

    def red(out, in_, R, W, opc=ALU.add):
        return op("dve", lambda e: e.tensor_reduce(out=out, in_=in_, axis=AX.X, op=opc), R, W)

    def dma(eng, out, in_, R, W):
        return op(eng, lambda e: e.dma_start(out=out, in_=in_), R, W, dma=True)

    def h3(ap, k=64):
        return ap.rearrange("p (h k) -> p h k", k=k)

    def bch(ap16, n=16, k=64):
        return ap16.unsqueeze(2).to_broadcast([ap16.shape[0], n, k])

    with ExitStack() as es0:
        ident = C.sb(es0, [P, P]); r_ident = Res("ident")
        ones_row = C.sb(es0, [1, P]); r_ones = Res("ones")
        op("pool", lambda e: e.memset(ident[:], 0.0), [], [r_ident])
        op("pool", lambda e: e.affine_select(out=ident[:], in_=ident[:], pattern=[[-1, P]],
                                            compare_op=ALU.not_equal, fill=1.0, base=0,
                                            channel_multiplier=1), [r_ident], [r_ident])
        op("pool", lambda e: e.memset(ones_row[:], 1.0), [], [r_ones])

        with ExitStack() as es:
            cT = C.sb(es, [P, 8]); r_cT = Res()
            cond = C.sb(es, [P, 8]); r_cond = Res()
            arow = C.sb(es, [1, 6 * D]); r_arow = Res()
            brow = C.sb(es, [1, 6 * D]); r_brow = Res()
            nrow = C.sb(es, [1, 2 * D]); r_nrow = Res()
            wb = [C.sb(es, [P, 8, 512]) for _ in range(2)]; r_wb = RL(2)
            pa = [C.ps(es, [1, 512]) for _ in range(2)]; r_pa = RL(2)
            dma("sp", cT[:], cT_d, [], [r_cT])
            dma("sp", brow[:], ada_b_d, [], [r_brow])
            dma("sp", nrow[:, 0:D], n1w_d, [], [r_nrow])
            dma("sp", nrow[:, D:2 * D], n2w_d, [], [r_nrow])
            act(cond[:], cT[:], AF.Silu, [r_cT], [r_cond])
            aw = ada_w_d.rearrange("(kc p) n -> p kc n", p=P)
            for g in range(12):
                b = g % 2
                dma("sp", wb[b][:], aw[:, :, g * 512:(g + 1) * 512], [], [r_wb[b]])
                for kc in range(8):
                    mm(pa[b][:], cond[:, kc:kc + 1], wb[b][:, kc, :], kc == 0, kc == 7, [r_cond, r_wb[b]], [r_pa[b]])
                tt("dve", arow[:, g * 512:(g + 1) * 512], pa[b][:], brow[:, g * 512:(g + 1) * 512], ALU.add,
                   [r_pa[b], r_brow], [r_arow])
            for (slot, off) in ((1, 0), (4, D)):
                stt(arow[:, slot * D:(slot + 1) * D], arow[:, slot * D:(slot + 1) * D], 1.0, nrow[:, off:off + D],
                    ALU.add, ALU.mult, [r_arow, r_nrow], [r_arow])
            r_ada_s = Res()
            dma("sp", ada_s, arow[:], [r_arow], [r_ada_s])
            if 1 in phases:
                mub = C.sb(es, [P, NSH]); r_mub = Res()
                wld = [C.sb(es, [P, NSH]) for _ in range(2)]; r_wld = RL(2)
                w2t = [C.sb(es, [P, NSH]) for _ in range(2)]; r_w2t = RL(2)
                dma("sp", mub[:], mu_d.partition_broadcast(P), [], [r_mub])
                for kc in range(8):
                    b = kc % 2
                    dma("sp", wld[b][:], w_in_d[kc * P:(kc + 1) * P, 0:NSH], [], [r_wld[b]])
                    tt("dve", w2t[b][:], wld[b][:], mub[:], ALU.mult, [r_wld[b], r_mub], [r_w2t[b]])
                    tt("pool", wld[b][:], wld[b][:], w2t[b][:], ALU.subtract, [r_wld[b], r_w2t[b]], [r_wld[b]])
                    dma("sp", w1_s[kc * P:(kc + 1) * P, :], wld[b][:], [r_wld[b]], [r_w1s])
                    dma("sp", w2_s[kc * P:(kc + 1) * P, :], w2t[b][:], [r_w2t[b]], [r_w2s])

            S.barrier()
            S.emit()

        if 1 in phases:
          with ExitStack() as es:
            bc = C.sb(es, [P, 3, D]); r_bc = RL(3)
            for i, slot in enumerate((1, 0, 2)):
                dma("sp", bc[:, i, :], ada_s[:, slot * D:(slot + 1) * D].partition_broadcast(P), [r_ada_s], [r_bc[i]])
            v6 = C.sb(es, [P, 6, D]); r_v6 = RL(6)
            for i in range(5):
                dma("sp", v6[:, i, :], vec6_d[i:i + 1, :].partition_broadcast(P), [], [r_v6[i]])
            ts("pool", v6[:, 5, :], v6[:, 1, :], -1.0, 1.0, ALU.mult, ALU.add, [r_v6[1]], [r_v6[5]])
            KKB, KAB, RKB, LWB, LBB, OKA = range(6)
            wa_up = C.sb(es, [P, D]); r_waup = Res()
            g_up = C.sb(es, [P, D]); r_gup = Res()
            w0a0 = C.sb(es, [1, 2 * D]); r_w0a0 = Res()
            dma("sp", wa_up[:], wa_up_d, [], [r_waup])
            dma("sp", g_up[:], g_up_d, [], [r_gup])
            dma("sp", w0a0[:], w0a0_d, [], [r_w0a0])
            tri = C.sb(es, [P, 3, P]); r_tri = Res()
            op("pool", lambda e: e.memset(tri[:], 1.0), [], [r_tri])
            for i, (pat, cm, cop) in enumerate((([[1, P]], -1, ALU.is_ge), ([[1, P]], -1, ALU.is_gt),
                                                ([[-1, P]], 1, ALU.is_gt))):
                op("pool", lambda e, i=i, pat=pat, cm=cm, cop=cop: e.affine_select(
                    out=tri[:, i, :], in_=tri[:, i, :], pattern=pat, compare_op=cop, fill=0.0, base=0,
                    channel_multiplier=cm), [r_tri], [r_tri])
            TRI, TRIS, TRIL = tri[:, 0, :], tri[:, 1, :], tri[:, 2, :]
            mask2 = C.sb(es, [P, 2 * P]); r_mask2 = Res()
            op("pool", lambda e: e.tensor_copy(out=mask2[:, 0:P], in_=TRIS), [r_tri], [r_mask2])
            op("pool", lambda e: e.tensor_copy(out=mask2[:, P:2 * P], in_=TRI), [r_tri], [r_mask2])
            qkw = C.sb(es, [P, 128]); r_qkw = Res()
            dma("sp", qkw[:], qk_nw_d.partition_broadcast(P), [], [r_qkw])
            esk = C.sb(es, [P, 16]); r_esk = Res()
            dma("sp", esk[:], sinks_d.partition_broadcast(P), [], [r_esk])
            act(esk[:], esk[:], AF.Exp, [r_esk], [r_esk])

            xt = [C.sb(es, [P, D]) for _ in range(2)]; r_xt = RL(2)
            st = C.sb(es, [P, 64]); r_st = Res()
            hT = [C.sb(es, [P, 8, P + 1]) for _ in range(2)]; r_hT = RL(2)
            NWK = 4
            wk = [C.sb(es, [P, 8, 256]) for _ in range(NWK)]; r_wk = RL(NWK)
            kT = [C.sb(es, [64, 2, P]) for _ in range(2)]; r_kT = RL(2)
            vv = [C.sb(es, [P, 2, 65]) for _ in range(2)]; r_vv = RL(2)
            STt = C.sb(es, [64, 16, 64]); r_ST = Res()
            WC = C.sb(es, [64, 16]); r_WC = Res()
            op("pool", lambda e: e.memset(STt[:], 0.0), [], [r_ST])
            for b_ in range(2):
                op("pool", lambda e, b_=b_: e.memset(vv[b_][:], 1.0), [], [r_vv[b_]])
            NPOOL = 24
            pool = FPool([TL(C.sb(es, [P, D]), Res(f"pl{i}")) for i in range(NPOOL)])
            pp = FPool([TL(C.ps(es, [P, 1024]), Res(f"ps{i}", excl=True)) for i in range(4)])
            w1v = w1_s.rearrange("(kc p) n -> p kc n", p=P)
            w2v = w2_s.rearrange("(kc p) n -> p kc n", p=P)
            wiv = w_in_d.rearrange("(kc p) n -> p kc n", p=P)
            wov = w_out_d.rearrange("(kc p) n -> p kc n", p=P)
            wki = [0]
            op("pool", lambda e: e.memset(hT[1][:, :, P:P + 1], 0.0), [], [r_hT[1]])

            def proj_cols(b, kind, c0, width, ps):
                for g0 in range(0, width, 256):
                    wd = min(256, width - g0)
                    cc = c0 + g0
                    if kind == "s":
                        s1 = wki[0] % NWK; s2 = (wki[0] + 1) % NWK; wki[0] += 2
                        dma("sp", wk[s1][:, :, 0:wd], w1v[:, :, cc:cc + wd], [r_w1s], [r_wk[s1]])
                        dma("sp", wk[s2][:, :, 0:wd], w2v[:, :, cc:cc + wd], [r_w2s], [r_wk[s2]])
                        for kc in range(8):
                            mm(ps.t[:, g0:g0 + wd], hT[b][:, kc, 1:P + 1], wk[s1][:, kc, 0:wd], kc == 0, False,
                               [r_hT[b], r_wk[s1]], [ps.r])
                        for kc in range(8):
                            mm(ps.t[:, g0:g0 + wd], hT[b][:, kc, 0:P], wk[s2][:, kc, 0:wd], False, kc == 7,
                               [r_hT[b], r_wk[s2]], [ps.r])
                    else:
                        s1 = wki[0] % NWK; wki[0] += 1
                        src = wiv if kind == "r" else wov
                        off = NSH if kind == "r" else 0
                        dma("sp", wk[s1][:, :, 0:wd], src[:, :, off + cc:off + cc + wd], [], [r_wk[s1]])
                        for kc in range(8):
                            mm(ps.t[:, g0:g0 + wd], hT[b][:, kc, 1:P + 1], wk[s1][:, kc, 0:wd], kc == 0, kc == 7,
                               [r_hT[b], r_wk[s1]], [ps.r])

            def rstd_of(ss_ap, n, scale, eps, R):
                ts("dve", ss_ap, ss_ap, scale, eps, ALU.mult, ALU.add, R, R)
                act(ss_ap, ss_ap, AF.Sqrt, R, R)
                op("dve", lambda e: e.reciprocal(out=ss_ap, in_=ss_ap), R, R)

            for t in range(NT):
                b = t % 2
                dma("sp", xt[b][:], x_d[t * P:(t + 1) * P, :], [], [r_xt[b]])
                hh = pool.get()
                act(hh.t[:], xt[b][:], AF.Square, [r_xt[b]], [hh.r, r_st], accum=st[:, 0:1])
                rstd_of(st[:, 0:1], 1, 1.0 / D, 1e-6, [r_st])
                stt(hh.t[:], xt[b][:], st[:, 0:1], bc[:, 0, :], ALU.mult, ALU.mult, [r_xt[b], r_st, r_bc[0]], [hh.r])
                tt("pool", hh.t[:], hh.t[:], bc[:, 1, :], ALU.add, [hh.r, r_bc[1]], [hh.r])
                op("pool", lambda e, b=b: e.tensor_copy(out=hT[b][:, :, 0:1], in_=hT[1 - b][:, :, P:P + 1]),
                   [r_hT[1 - b]], [r_hT[b]])
                ps = pp.get()
                for kc in range(8):
                    tr(ps.t[:, kc * P:(kc + 1) * P], hh.t[:, kc * P:(kc + 1) * P], [hh.r], [ps.r])
                act(hT[b][:, :, 1:P + 1], ps.t[:].rearrange("p (q n) -> p q n", q=8), AF.Copy, [ps.r], [r_hT[b]])
                pp.put(ps); pool.put(hh)

                if STG <= 1:
                    continue
                aq = pool.get(); akav = pool.get()
                ps = pp.get(); proj_cols(b, "r", 0, 1024, ps)
                act(aq.t[:], ps.t[:], AF.Copy, [ps.r], [aq.r]); pp.put(ps)
                ps = pp.get(); proj_cols(b, "r", 1024, 256, ps)
                act(akav.t[:, 0:256], ps.t[:, 0:256], AF.Copy, [ps.r], [akav.r]); pp.put(ps)
                sq = pool.get()
                tt("pool", sq.t[:], aq.t[:], aq.t[:], ALU.mult, [aq.r], [sq.r])
                red(st[:, 8:24], h3(sq.t[:]), [sq.r], [r_st])
                tt("pool", sq.t[:, 0:128], akav.t[:, 0:128], akav.t[:, 0:128], ALU.mult, [akav.r], [sq.r])
                red(st[:, 24:26], h3(sq.t[:, 0:128]), [sq.r], [r_st])
                pool.put(sq)
                rstd_of(st[:, 8:26], 18, 1.0 / 64, 1e-6, [r_st])
                tt("dve", h3(aq.t[:]), h3(aq.t[:]), bch(st[:, 8:24]), ALU.mult, [aq.r, r_st], [aq.r])
                tt("pool", h3(aq.t[:]), h3(aq.t[:]), qkw[:, 0:64].unsqueeze(1).to_broadcast([P, 16, 64]), ALU.mult,
                   [aq.r, r_qkw], [aq.r])
                tt("dve", h3(akav.t[:, 0:128]), h3(akav.t[:, 0:128]), bch(st[:, 24:26], 2), ALU.mult,
                   [akav.r, r_st], [akav.r])
                tt("pool", h3(akav.t[:, 0:128]), h3(akav.t[:, 0:128]),
                   qkw[:, 64:128].unsqueeze(1).to_broadcast([P, 2, 64]), ALU.mult, [akav.r, r_qkw], [akav.r])
                op("pool", lambda e, b=b, akav=akav: e.tensor_copy(out=vv[b][:, :, 0:64], in_=h3(akav.t[:, 128:256])),
                   [akav.r], [r_vv[b]])
                qT = [pool.get(), pool.get()]
                for half in range(2):
                    ps = pp.get()
                    for j in range(8):
                        hd = half * 8 + j
                        tr(ps.t[0:64, j * P:(j + 1) * P], aq.t[:, hd * 64:(hd + 1) * 64], [aq.r], [ps.r])
                    act(qT[half].t[0:64, :], ps.t[0:64, :], AF.Copy, [ps.r], [qT[half].r]); pp.put(ps)
                ps = pp.get()
                for g in range(2):
                    tr(ps.t[0:64, g * P:(g + 1) * P], akav.t[:, g * 64:(g + 1) * 64], [akav.r], [ps.r])
                act(kT[b][:].rearrange("p g n -> p (g n)"), ps.t[0:64, 0:256], AF.Copy, [ps.r], [r_kT[b]]); pp.put(ps)
                pool.put(aq, akav)
                yb = pool.get()
                for g in range(2):
                    Ec = pool.get(); Ep = pool.get()
                    for (E, kb, mask) in ((Ec, b, TRI), (Ep, 1 - b, TRIL)):
                        if E is Ep and t == 0:
                            continue
                        ps = pp.get()
                        for hf in range(2):
                            mm(ps.t[:, hf * 512:(hf + 1) * 512], kT[kb][:, g, :], qT[g].t[0:64, hf * 512:(hf + 1) * 512],
                               True, True, [r_kT[kb], qT[g].r], [ps.r])
                        act(E.t[:], ps.t[:], AF.Exp, [ps.r], [E.r], scale=0.125); pp.put(ps)
                        tt("pool", E.t[:].rearrange("p (h n) -> p h n", n=P), E.t[:].rearrange("p (h n) -> p h n", n=P),
                           mask.unsqueeze(1).to_broadcast([P, 8, P]), ALU.mult, [E.r, r_tri], [E.r])
                    ps = pp.get()
                    for j in range(8):
                        mm(ps.t[:, j * 128:j * 128 + 65], Ec.t[:, j * P:(j + 1) * P], vv[b][:, g, :], True, t == 0,
                           [Ec.r, r_vv[b]], [ps.r])
                        if t > 0:
                            mm(ps.t[:, j * 128:j * 128 + 65], Ep.t[:, j * P:(j + 1) * P], vv[1 - b][:, g, :], False, True,
                               [Ep.r, r_vv[1 - b]], [ps.r])
                    o3 = ps.t[:].rearrange("p (h n) -> p h n", n=128)
                    tt("dve", st[:, 32:40], o3[:, :, 64], esk[:, g * 8:(g + 1) * 8], ALU.add, [ps.r, r_esk], [r_st])
                    op("dve", lambda e: e.reciprocal(out=st[:, 32:40], in_=st[:, 32:40]), [r_st], [r_st])
                    tt("dve", h3(yb.t[:, g * 512:(g + 1) * 512]), o3[:, :, 0:64], bch(st[:, 32:40], 8), ALU.mult,
                       [ps.r, r_st], [yb.r])
                    pp.put(ps); pool.put(Ec, Ep)
                pool.put(*qT)
                dump("yb", yb.t[:], [P, D], [yb.r], t * P, P, S_LEN)

                if STG <= 2:
                    continue
                pr = pool.get(); pk = pool.get(); pv = pool.get(); lw = pool.get()
                for (dst, c0) in ((pr, 0), (pk, 1024), (pv, 2048)):
                    ps = pp.get(); proj_cols(b, "s", c0, 1024, ps)
                    act(dst.t[:], ps.t[:], AF.Copy, [ps.r], [dst.r]); pp.put(ps)
                ps = pp.get(); proj_cols(b, "s", 3072, 256, ps)
                act(lw.t[:, 0:64], ps.t[:, 0:64], AF.Tanh, [ps.r], [lw.r])
                act(lw.t[:, 64:128], ps.t[:, 64:128], AF.Copy, [ps.r], [lw.r])
                act(lw.t[:, 128:256], ps.t[:, 128:256], AF.Sigmoid, [ps.r], [lw.r]); pp.put(ps)
                ps = pp.get()
                tr(ps.t[:, 0:P], lw.t[:, 0:128], [lw.r], [ps.r])
                tr(ps.t[:, P:2 * P], lw.t[:, 128:256], [lw.r], [ps.r])
                act(lw.t[:, 256:512], ps.t[:, 0:256], AF.Copy, [ps.r], [lw.r]); pp.put(ps)
                lwT = lw.t[:, 256:384]; sgT = lw.t[:, 384:512]
                sw = pool.get(); a_ = pool.get(); g_sb = pool.get()
                ps = pp.get()
                for hf in range(2):
                    cs = slice(hf * 512, (hf + 1) * 512)
                    mm(ps.t[:, cs], lw.t[0:64, 256:384], wa_up[0:64, cs], True, False, [lw.r, r_waup], [ps.r])
                    mm(ps.t[:, cs], ones_row[0:1, :], w0a0[0:1, hf * 512:(hf + 1) * 512], False, True,
                       [r_ones, r_w0a0], [ps.r])
                act(sw.t[:], ps.t[:], AF.Sigmoid, [ps.r], [sw.r]); pp.put(ps)
                ps = pp.get()
                for hf in range(2):
                    cs = slice(hf * 512, (hf + 1) * 512)
                    mm(ps.t[:, cs], lw.t[64:128, 256:384], wa_up[64:128, cs], True, False, [lw.r, r_waup], [ps.r])
                    mm(ps.t[:, cs], ones_row[0:1, :], w0a0[0:1, D + hf * 512:D + (hf + 1) * 512], False, True,
                       [r_ones, r_w0a0], [ps.r])
                act(a_.t[:], ps.t[:], AF.Sigmoid, [ps.r], [a_.r]); pp.put(ps)
                ps = pp.get()
                for hf in range(2):
                    cs = slice(hf * 512, (hf + 1) * 512)
                    mm(ps.t[:, cs], sgT, g_up[:, cs], True, True, [lw.r, r_gup], [ps.r])
                act(g_sb.t[:], ps.t[:], AF.Copy, [ps.r], [g_sb.r]); pp.put(ps)
                pool.put(lw)
                eW = pool.get(); eWi = pool.get(); eWx = pool.get()
                ps = pp.get()
                for hf in range(2):
                    cs = slice(hf * 512, (hf + 1) * 512)
                    mm(ps.t[:, cs], TRI, sw.t[:, cs], True, True, [r_tri, sw.r], [ps.r])
                act(eW.t[:], ps.t[:], AF.Exp, [ps.r], [eW.r], scale=-C0)
                act(eWi.t[:], ps.t[:], AF.Exp, [ps.r], [eWi.r], scale=C0); pp.put(ps)
                ps = pp.get()
                for hf in range(2):
                    cs = slice(hf * 512, (hf + 1) * 512)
                    mm(ps.t[:, cs], TRIS, sw.t[:, cs], True, True, [r_tri, sw.r], [ps.r])
                act(eWx.t[:], ps.t[:], AF.Exp, [ps.r], [eWx.r], scale=-C0); pp.put(ps)
                pool.put(sw)
                kk = pool.get(); tmp = pool.get()
                tt("dve", kk.t[:], pk.t[:], v6[:, KKB, :], ALU.mult, [pk.r, r_v6[KKB]], [kk.r])
                tt("pool", tmp.t[:], kk.t[:], kk.t[:], ALU.mult, [kk.r], [tmp.r])
                red(st[:, 40:56], h3(tmp.t[:]), [tmp.r], [r_st])
                act(st[:, 40:56], st[:, 40:56], AF.Sqrt, [r_st], [r_st])
                ts("dve", st[:, 40:56], st[:, 40:56], 1e-12, None, ALU.max, None, [r_st], [r_st])
                op("dve", lambda e: e.reciprocal(out=st[:, 40:56], in_=st[:, 40:56]), [r_st], [r_st])
                tt("dve", h3(kk.t[:]), h3(kk.t[:]), bch(st[:, 40:56]), ALU.mult, [kk.r, r_st], [kk.r])
                kv_ = pool.get()
                tt("pool", tmp.t[:], a_.t[:], v6[:, KAB, :], ALU.mult, [a_.r, r_v6[KAB]], [tmp.r])
                tt("pool", tmp.t[:], tmp.t[:], v6[:, OKA, :], ALU.add, [tmp.r, r_v6[OKA]], [tmp.r])
                tt("dve", kv_.t[:], pk.t[:], tmp.t[:], ALU.mult, [pk.r, tmp.r], [kv_.r])
                pool.put(pk)
                At = pool.get(); Bt = pool.get(); Kt = pool.get(); Rt = pool.get()
                stt(At.t[:], kk.t[:], -1.0, eWx.t[:], ALU.mult, ALU.mult, [kk.r, eWx.r], [At.r])
                tt("pool", Bt.t[:], kk.t[:], a_.t[:], ALU.mult, [kk.r, a_.r], [Bt.r])
                tt("dve", Bt.t[:], Bt.t[:], eWi.t[:], ALU.mult, [Bt.r, eWi.r], [Bt.r])
                tt("dve", Kt.t[:], kv_.t[:], eWi.t[:], ALU.mult, [kv_.r, eWi.r], [Kt.r])
                tt("pool", Rt.t[:], pr.t[:], eW.t[:], ALU.mult, [pr.r, eW.r], [Rt.r])
                pool.put(kk, a_, eWi, eWx)
                tt("pool", tmp.t[:], pr.t[:], kv_.t[:], ALU.mult, [pr.r, kv_.r], [tmp.r])
                tt("dve", tmp.t[:], tmp.t[:], v6[:, RKB, :], ALU.mult, [tmp.r, r_v6[RKB]], [tmp.r])
                bs = pool.get()
                red(bs.t[:, 0:16], h3(tmp.t[:]), [tmp.r], [bs.r])
                tt("dve", h3(tmp.t[:]), h3(pv.t[:]), bch(bs.t[:, 0:16]), ALU.mult, [pv.r, bs.r, tmp.r], [tmp.r])
                bonus = tmp
                pool.put(pr, kv_, bs)
                ps = pp.get()
                for hd in range(16):
                    mm(ps.t[0:64, hd:hd + 1], eW.t[:, hd * 64:(hd + 1) * 64], ident[:, P - 1:P], True, True,
                       [eW.r, r_ident], [ps.r])
                act(WC[:], ps.t[0:64, 0:16], AF.Copy, [ps.r], [r_WC]); pp.put(ps)
                pool.put(eW)
                if STG <= 3:
                    continue
                RTt = [pool.get(), pool.get()]
                Arb = [pool.get(), pool.get()]
                Ark = [pool.get(), pool.get()]
                P1s = [pool.get(), pool.get()]
                Uv = pool.get(); Qs = pool.get()
                for hg in range(4):
                    FTa = pool.get(); FTb = pool.get()
                    for j in range(4):
                        hd = hg * 4 + j
                        hs = slice(hd * 64, (hd + 1) * 64)
                        ps = pp.get()
                        tr(ps.t[0:64, 0:128], At.t[:, hs], [At.r], [ps.r])
                        tr(ps.t[0:64, 128:256], Rt.t[:, hs], [Rt.r], [ps.r])
                        tr(ps.t[0:64, 256:384], Bt.t[:, hs], [Bt.r], [ps.r])
                        tr(ps.t[0:64, 384:512], Kt.t[:, hs], [Kt.r], [ps.r])
                        act(FTa.t[0:64, j * 256:(j + 1) * 256], ps.t[0:64, 0:256], AF.Copy, [ps.r], [FTa.r])
                        act(FTb.t[0:64, j * 256:(j + 1) * 256], ps.t[0:64, 256:512], AF.Copy, [ps.r], [FTb.r])
                        hh_, jj = hd // 8, hd % 8
                        op("pool", lambda e, FTa=FTa, j=j, RT_=RTt[hh_], jj=jj: e.tensor_copy(
                            out=RT_.t[0:64, jj * P:(jj + 1) * P], in_=FTa.t[0:64, j * 256 + 128:j * 256 + 256]),
                           [FTa.r], [RTt[hh_].r])
                        pp.put(ps)
                    MX = pool.get(); Lp = pool.get(); Aak = pool.get()
                    MX4 = MX.t[:].rearrange("p (h s n) -> p h s n", h=4, s=2)
                    psM = pp.get(); psK = pp.get(); psL = pp.get()
                    for j in range(4):
                        fa = FTa.t[0:64, j * 256:(j + 1) * 256]
                        mm(psM.t[:, j * 256:(j + 1) * 256], FTb.t[0:64, j * 256:j * 256 + 128], fa, True, True,
                           [FTa.r, FTb.r], [psM.r])
                        mm(psK.t[:, j * 256:(j + 1) * 256], FTb.t[0:64, j * 256 + 128:(j + 1) * 256], fa, True, True,
                           [FTa.r, FTb.r], [psK.r])
                        mm(psL.t[:, j * 128:(j + 1) * 128], FTa.t[0:64, j * 256:j * 256 + 128],
                           FTb.t[0:64, j * 256:j * 256 + 128], True, True, [FTa.r, FTb.r], [psL.r])
                    hh_ = hg // 2; j0 = (hg % 2) * 4
                    pM4 = psM.t[:].rearrange("p (h s n) -> p h s n", h=4, s=2)
                    pK4 = psK.t[:].rearrange("p (h s n) -> p h s n", h=4, s=2)
                    m_s = TRIS.unsqueeze(1).to_broadcast([P, 4, P]); m_i = TRI.unsqueeze(1).to_broadcast([P, 4, P])
                    m_l = TRIL.unsqueeze(1).to_broadcast([P, 4, P])
                    arb_v = Arb[hh_].t[:, j0 * P:(j0 + 4) * P].rearrange("p (h n) -> p h n", n=P)
                    ark_v = Ark[hh_].t[:, j0 * P:(j0 + 4) * P].rearrange("p (h n) -> p h n", n=P)
                    tt("dve", MX4[:, :, 0, :], pM4[:, :, 0, :], m_s, ALU.mult, [psM.r, r_tri], [MX.r])
                    tt("dve", arb_v, pM4[:, :, 1, :], m_i, ALU.mult, [psM.r, r_tri], [Arb[hh_].r])
                    tt("dve", Aak.t[:, 0:512].rearrange("p (h n) -> p h n", n=P), pK4[:, :, 0, :], m_s, ALU.mult,
                       [psK.r, r_tri], [Aak.r])
                    tt("dve", ark_v, pK4[:, :, 1, :], m_i, ALU.mult, [psK.r, r_tri], [Ark[hh_].r])
                    tt("dve", Lp.t[:, 0:512].rearrange("p (h n) -> p h n", n=P),
                       psL.t[:, 0:512].rearrange("p (h n) -> p h n", n=P), m_l, ALU.mult, [psL.r, r_tri], [Lp.r])
                    pp.put(psM, psK, psL)
                    tt("pool", MX4[:, :, 1, :], MX4[:, :, 0, :], ident[:].unsqueeze(1).to_broadcast([P, 4, P]), ALU.add,
                       [MX.r, r_ident], [MX.r])
                    psQ = pp.get()
                    for j in range(4):
                        hd = hg * 4 + j
                        mm(psQ.t[:, j * 64:(j + 1) * 64], Aak.t[:, j * P:(j + 1) * P], pv.t[:, hd * 64:(hd + 1) * 64],
                           True, True, [Aak.r, pv.r], [psQ.r])
                    act(Qs.t[:, hg * 256:(hg + 1) * 256], psQ.t[:, 0:256], AF.Copy, [psQ.r], [Qs.r]); pp.put(psQ)
                    pool.put(Aak)
                    p_ = 1
                    while p_ <= 64:
                        if p_ == 1:
                            psA = pp.get(); psB = pp.get()
                            for j in range(4):
                                mm(psA.t[:, j * 256:j * 256 + 128], Lp.t[:, j * P:(j + 1) * P], MX4[:, j, 0, :], True, True,
                                   [Lp.r, MX.r], [psA.r])
                                mm(psB.t[:, j * P:(j + 1) * P], MX4[:, j, 0, :], Lp.t[:, j * P:(j + 1) * P], True, True,
                                   [Lp.r, MX.r], [psB.r])
                            pA4 = psA.t[:].rearrange("p (h s n) -> p h s n", h=4, s=2)
                            act(MX4[:, :, 0, :], pA4[:, :, 0, :], AF.Copy, [psA.r], [MX.r])
                            act(Lp.t[:, 0:512], psB.t[:, 0:512], AF.Copy, [psB.r], [Lp.r])
                            pp.put(psA, psB)
                        elif p_ < 64:
                            psA = pp.get(); psB = pp.get()
                            for j in range(4):
                                mm(psA.t[:, j * 256:(j + 1) * 256], Lp.t[:, j * P:(j + 1) * P],
                                   MX.t[:, j * 256:(j + 1) * 256], True, True, [Lp.r, MX.r], [psA.r])
                                mm(psB.t[:, j * P:(j + 1) * P], MX4[:, j, 0, :], Lp.t[:, j * P:(j + 1) * P], True, True,
                                   [Lp.r, MX.r], [psB.r])
                            pA4 = psA.t[:].rearrange("p (h s n) -> p h s n", h=4, s=2)
                            act(MX4[:, :, 0, :], pA4[:, :, 0, :], AF.Copy, [psA.r], [MX.r])
                            tt("dve", MX4[:, :, 1, :], pA4[:, :, 1, :], MX4[:, :, 1, :], ALU.add, [psA.r, MX.r], [MX.r])
                            act(Lp.t[:, 0:512], psB.t[:, 0:512], AF.Copy, [psB.r], [Lp.r])
                            pp.put(psA, psB)
                        else:
                            psA = pp.get()
                            for j in range(4):
                                mm(psA.t[:, j * P:(j + 1) * P], Lp.t[:, j * P:(j + 1) * P], MX4[:, j, 1, :], True, True,
                                   [Lp.r, MX.r], [psA.r])
                            tt("dve", MX4[:, :, 1, :], psA.t[:, 0:512].rearrange("p (h n) -> p h n", n=P), MX4[:, :, 1, :],
                               ALU.add, [psA.r, MX.r], [MX.r])
                            pp.put(psA)
                        p_ *= 2
                    psP = pp.get(); psU = pp.get()
                    for j in range(4):
                        hd = hg * 4 + j
                        mm(psP.t[0:64, j * P:(j + 1) * P], At.t[:, hd * 64:(hd + 1) * 64], MX4[:, j, 1, :], True, True,
                           [At.r, MX.r], [psP.r])
                        mm(psU.t[:, j * 64:(j + 1) * 64], MX4[:, j, 1, :], Qs.t[:, hd * 64:(hd + 1) * 64], True, True,
                           [MX.r, Qs.r], [psU.r])
                    act(P1s[hh_].t[0:64, j0 * P:(j0 + 4) * P], psP.t[0:64, 0:512], AF.Copy, [psP.r], [P1s[hh_].r])
                    act(Uv.t[:, hg * 256:(hg + 1) * 256], psU.t[:, 0:256], AF.Copy, [psU.r], [Uv.r])
                    pp.put(psP, psU)
                    pool.put(FTa, FTb, MX, Lp)
                pool.put(Rt, Qs)
                if STG <= 4:
                    continue
                SAs = pool.get()
                psS = pp.get()
                for hd in range(16):
                    hh_, jj = hd // 8, hd % 8
                    mm(psS.t[:, hd * 64:(hd + 1) * 64], P1s[hh_].t[0:64, jj * P:(jj + 1) * P], STt[:, hd, :], True, True,
                       [P1s[hh_].r, r_ST], [psS.r])
                tt("dve", SAs.t[:], psS.t[:], Uv.t[:], ALU.add, [psS.r, Uv.r], [SAs.r]); pp.put(psS)
                psY = pp.get()
                for hd in range(16):
                    hh_, jj = hd // 8, hd % 8
                    hs = slice(hd * 64, (hd + 1) * 64)
                    mm(psY.t[:, hs], RTt[hh_].t[0:64, jj * P:(jj + 1) * P], STt[:, hd, :], True, False,
                       [RTt[hh_].r, r_ST], [psY.r])
                    mm(psY.t[:, hs], Arb[hh_].t[:, jj * P:(jj + 1) * P], SAs.t[:, hs], False, False,
                       [Arb[hh_].r, SAs.r], [psY.r])
                    mm(psY.t[:, hs], Ark[hh_].t[:, jj * P:(jj + 1) * P], pv.t[:, hs], False, True,
                       [Ark[hh_].r, pv.r], [psY.r])
                psN = pp.get()
                for hd in range(16):
                    hs = slice(hd * 64, (hd + 1) * 64)
                    mm(psN.t[0:64, hs], Bt.t[:, hs], SAs.t[:, hs], True, False, [Bt.r, SAs.r], [psN.r])
                    mm(psN.t[0:64, hs], Kt.t[:, hs], pv.t[:, hs], False, True, [Kt.r, pv.r], [psN.r])
                ST2 = STt[:].rearrange("p h v -> p (h v)")
                tt("dve", ST2, psN.t[0:64, :], ST2, ALU.add, [psN.r, r_ST], [r_ST])
                tt("dve", STt[:], STt[:], WC[:].unsqueeze(2).to_broadcast([64, 16, 64]), ALU.mult, [r_ST, r_WC], [r_ST])
                pp.put(psN)
                pool.put(SAs, Uv, At, Bt, Kt, *RTt, *Arb, *Ark, *P1s)
                yc = pool.get(); sq = pool.get()
                if dbg and "yrec" in dbg:
                    act(sq.t[:], psY.t[:], AF.Copy, [psY.r], [sq.r])
                    dump("yrec", sq.t[:], [P, D], [sq.r], t * P, P, S_LEN)
                red(st[:, 8:24], h3(psY.t[:]), [psY.r], [r_st])
                ts("dve", st[:, 8:24], st[:, 8:24], -1.0 / 64, None, ALU.mult, None, [r_st], [r_st])
                tt("dve", h3(yc.t[:]), h3(psY.t[:]), bch(st[:, 8:24]), ALU.add, [psY.r, r_st], [yc.r]); pp.put(psY)
                tt("pool", sq.t[:], yc.t[:], yc.t[:], ALU.mult, [yc.r], [sq.r])
                red(st[:, 8:24], h3(sq.t[:]), [sq.r], [r_st])
                rstd_of(st[:, 8:24], 16, 1.0 / 64, 64e-5, [r_st])
                tt("dve", h3(yc.t[:]), h3(yc.t[:]), bch(st[:, 8:24]), ALU.mult, [yc.r, r_st], [yc.r])
                tt("pool", yc.t[:], yc.t[:], v6[:, LWB, :], ALU.mult, [yc.r, r_v6[LWB]], [yc.r])
                tt("pool", yc.t[:], yc.t[:], v6[:, LBB, :], ALU.add, [yc.r, r_v6[LBB]], [yc.r])
                tt("pool", yc.t[:], yc.t[:], bonus.t[:], ALU.add, [yc.r, bonus.r], [yc.r])
                tt("dve", yc.t[:], yc.t[:], g_sb.t[:], ALU.mult, [yc.r, g_sb.r], [yc.r])
                pool.put(sq, bonus, g_sb, pv)
                dump("ya", yc.t[:], [P, D], [yc.r], t * P, P, S_LEN)
                if STG <= 5:
                    continue
                for (src_t, c0) in ((yc, 1280), (yb, 2304)):
                    ps = pp.get(); proj_cols(b, "r", c0, 1024, ps)
                    gsb = pool.get()
                    act(gsb.t[:], ps.t[:], AF.Sigmoid, [ps.r], [gsb.r]); pp.put(ps)
                    tt("dve", src_t.t[:], src_t.t[:], gsb.t[:], ALU.mult, [src_t.r, gsb.r], [src_t.r])
                    pool.put(gsb)
                tt("pool", yc.t[:], yc.t[:], yb.t[:], ALU.add, [yc.r, yb.r], [yc.r])
                pool.put(yb)
                dump("mixed", yc.t[:], [P, D], [yc.r], t * P, P, S_LEN)
                mT = pool.get()
                ps = pp.get()
                for kc in range(8):
                    tr(ps.t[:, kc * P:(kc + 1) * P], yc.t[:, kc * P:(kc + 1) * P], [yc.r], [ps.r])
                act(mT.t[:], ps.t[:], AF.Copy, [ps.r], [mT.r]); pp.put(ps)
                pool.put(yc)
                ps = pp.get()
                for g0 in range(0, 1024, 256):
                    s1 = wki[0] % NWK; wki[0] += 1
                    dma("sp", wk[s1][:], wov[:, :, g0:g0 + 256], [], [r_wk[s1]])
                    for kc in range(8):
                        mm(ps.t[:, g0:g0 + 256], mT.t[:, kc * P:(kc + 1) * P], wk[s1][:, kc, :], kc == 0, kc == 7,
                           [mT.r, r_wk[s1]], [ps.r])
                x1 = pool.get()
                tt("dve", x1.t[:], ps.t[:], bc[:, 2, :], ALU.mult, [ps.r, r_bc[2]], [x1.r]); pp.put(ps)
                tt("pool", x1.t[:], x1.t[:], xt[b][:], ALU.add, [x1.r, r_xt[b]], [x1.r])
                ev = dma("pool", out_d[t * P:(t + 1) * P, :], x1.t[:], [x1.r], [r_x1[t]])
                out_evs.append(ev)
                pool.put(mT, x1)

            S.barrier()
            S.emit()

        if 2 in phases:
          with ExitStack() as es:
            bc = C.sb(es, [P, 3, D]); r_bc = RL(3)
            for i, slot in enumerate((4, 3, 5)):
                dma("sp", bc[:, i, :], ada_s[:, slot * D:(slot + 1) * D].partition_broadcast(P), [r_ada_s], [r_bc[i]])
            keysT = C.sb(es, [P, 256]); r_keys = Res()
            dma("sp", keysT[:], keysT_d, [], [r_keys])
            iot = C.sb(es, [P, 16]); r_iot = Res()
            op("pool", lambda e: e.iota(iot[:], pattern=[[1, 16]], base=0, channel_multiplier=0,
                                        allow_small_or_imprecise_dtypes=True), [], [r_iot])
            xt = [C.sb(es, [P, D]) for _ in range(2)]; r_xt = RL(2)
            st = C.sb(es, [P, 64]); r_st = Res()
            NWK = 3
            wk = [C.sb(es, [P, 8, 256]) for _ in range(NWK)]; r_wk = RL(NWK)
            h2 = C.sb(es, [P, D]); r_h2 = Res()
            h2T = C.sb(es, [P, 8, P]); r_h2T = Res()
            qT = C.sb(es, [P, 16, P]); r_qT = Res()
            s12 = C.sb(es, [P, 16, P]); r_s12 = Res()
            tmpk = C.sb(es, [P, 256]); r_tmpk = Res()
            V12 = C.sb(es, [P, 16, 16]); r_V12 = Res()
            I12 = C.sb(es, [P, 16, 16], U32); r_I12 = Res()
            I12f = C.sb(es, [P, 16, 16]); r_I12f = Res()
            cand = C.sb(es, [P, 8, 256]); r_cand = Res()
            scv = C.sb(es, [P, 8, 16]); r_scv = Res()
            posu = C.sb(es, [P, 8, 16], U32); r_posu = Res()
            pa_u = C.sb(es, [P, 2, 128], U32); r_pau = Res()
            pa_f = C.sb(es, [P, 2, 128]); r_paf = Res()
            oh = C.sb(es, [P, 8, 16, 16]); r_oh = Res()
            isel = C.sb(es, [P, 2, 128]); r_isel = Res()
            idxf = C.sb(es, [P, 128]); r_idxf = Res()
            idxu = [C.sb(es, [P, 128], I32) for _ in range(2)]; r_idxu = RL(2)
            gate = C.sb(es, [P, 8, 16]); r_gate = Res()
            acts = C.sb(es, [P, 128]); r_acts = Res()
            wts = C.sb(es, [P, 128]); r_wts = Res()
            acc = C.sb(es, [P, D]); r_acc = Res()
            junk = C.sb(es, [P, D]); r_junk = Res()
            NG = 12
            gs = FPool([TL(C.sb(es, [P, D]), Res(f"g{i}")) for i in range(NG)])
            pp = FPool([TL(C.ps(es, [P, 1024]), Res(f"ps{i}", excl=True)) for i in range(4)])
            wqv = wq_d.rearrange("(kc p) n -> p kc n", p=P)
            wki = [0]

            def top16(src, dstv, dsti, n, R_src):
                op("dve", lambda e: e.max(out=dstv[:, 0:8], in_=src), R_src, [r_V12])
                op("dve", lambda e: e.match_replace(out=tmpk[:, 0:n], in_to_replace=dstv[:, 0:8], in_values=src,
                                                    imm_value=-1e30), R_src + [r_V12], [r_tmpk])
                op("dve", lambda e: e.max(out=dstv[:, 8:16], in_=tmpk[:, 0:n]), [r_tmpk], [r_V12])
                op("dve", lambda e: e.max_index(out=dsti[:, 0:8], in_max=dstv[:, 0:8], in_values=src),
                   R_src + [r_V12], [r_I12])
                op("dve", lambda e: e.max_index(out=dsti[:, 8:16], in_max=dstv[:, 8:16], in_values=tmpk[:, 0:n]),
                   [r_tmpk, r_V12], [r_I12])

            for t in range(NT):
                b = t % 2
                dma("sp", xt[b][:], out_d[t * P:(t + 1) * P, :], [r_x1[t]], [r_xt[b]])
                act(junk[:], xt[b][:], AF.Square, [r_xt[b]], [r_junk, r_st], accum=st[:, 0:1])
                ts("dve", st[:, 0:1], st[:, 0:1], 1.0 / D, 1e-6, ALU.mult, ALU.add, [r_st], [r_st])
                act(st[:, 0:1], st[:, 0:1], AF.Sqrt, [r_st], [r_st])
                op("dve", lambda e: e.reciprocal(out=st[:, 0:1], in_=st[:, 0:1]), [r_st], [r_st])
                stt(h2[:], xt[b][:], st[:, 0:1], bc[:, 0, :], ALU.mult, ALU.mult, [r_xt[b], r_st, r_bc[0]], [r_h2])
                tt("pool", h2[:], h2[:], bc[:, 1, :], ALU.add, [r_h2, r_bc[1]], [r_h2])
                ps = pp.get()
                for kc in range(8):
                    tr(ps.t[:, kc * P:(kc + 1) * P], h2[:, kc * P:(kc + 1) * P], [r_h2], [ps.r])
                act(h2T[:].rearrange("p q n -> p (q n)"), ps.t[:], AF.Copy, [ps.r], [r_h2T]); pp.put(ps)
                for g in range(8):
                    s1 = wki[0] % NWK; wki[0] += 1
                    dma("sp", wk[s1][:], wqv[:, :, g * 256:(g + 1) * 256], [], [r_wk[s1]])
                    if g % 4 == 0:
                        ps = pp.get()
                    for j in range(2):
                        hj = g * 2 + j
                        o_ = ps.t[:, (hj % 8) * P:(hj % 8 + 1) * P]
                        for kc in range(8):
                            mm(o_, wk[s1][:, kc, j * P:(j + 1) * P], h2T[:, kc, :], kc == 0, kc == 7,
                               [r_wk[s1], r_h2T], [ps.r])
                    if g % 4 == 3:
                        half = g // 4
                        act(qT[:, half * 8:(half + 1) * 8, :].rearrange("p q n -> p (q n)"), ps.t[:], AF.Copy,
                            [ps.r], [r_qT]); pp.put(ps)
                for half in range(2):
                    ps = pp.get()
                    for q_ in range(8):
                        hj = half * 8 + q_
                        j = hj % 2
                        mm(ps.t[:, q_ * P:(q_ + 1) * P], qT[:, hj, :], keysT[:, j * P:(j + 1) * P], True, True,
                           [r_qT, r_keys], [ps.r])
                    act(s12[:, half * 8:(half + 1) * 8, :].rearrange("p q n -> p (q n)"), ps.t[:], AF.Copy,
                        [ps.r], [r_s12]); pp.put(ps)
                for hj in range(16):
                    top16(s12[:, hj, :], V12[:, hj, :], I12[:, hj, :], 128, [r_s12])
                V4 = V12[:].rearrange("p (h j) k -> p h j k", j=2)
                tt("dve", cand[:].rearrange("p h (a b) -> p h a b", b=16),
                   V4[:, :, 0, :].unsqueeze(3).to_broadcast([P, 8, 16, 16]),
                   V4[:, :, 1, :].unsqueeze(2).to_broadcast([P, 8, 16, 16]), ALU.add, [r_V12], [r_cand])
                op("dve", lambda e: e.tensor_copy(out=I12f[:], in_=I12[:]), [r_I12], [r_I12f])
                for h in range(8):
                    top16(cand[:, h, :], scv[:, h, :], posu[:, h, :], 256, [r_cand])
                posf2 = posu[:].rearrange("p h k -> p (h k)")
                op("dve", lambda e: e.tensor_single_scalar(out=pa_u[:, 0, :], in_=posf2, scalar=4,
                                                           op=ALU.logical_shift_right), [r_I12], [r_pau])
                op("dve", lambda e: e.tensor_single_scalar(out=pa_u[:, 1, :], in_=posf2, scalar=15,
                                                           op=ALU.bitwise_and), [r_I12], [r_pau])
                op("dve", lambda e: e.tensor_copy(out=pa_f[:], in_=pa_u[:]), [r_pau], [r_paf])
                I4 = I12f[:].rearrange("p (h j) k -> p h j k", j=2)
                for j in range(2):
                    sel_f = pa_f[:, j, :].rearrange("p (h k) -> p h k", k=16)
                    tt("dve", oh[:], sel_f.unsqueeze(3).to_broadcast([P, 8, 16, 16]),
                       iot[:].unsqueeze(1).unsqueeze(1).to_broadcast([P, 8, 16, 16]), ALU.is_equal,
                       [r_paf, r_iot], [r_oh])
                    tt("dve", oh[:], oh[:], I4[:, :, j, :].unsqueeze(2).to_broadcast([P, 8, 16, 16]), ALU.mult,
                       [r_oh, r_I12f], [r_oh])
                    red(isel[:, j, :], oh[:].rearrange("p h k a -> p (h k) a"), [r_oh], [r_isel])
                stt(idxf[:], isel[:, 0, :], 128.0, isel[:, 1, :], ALU.mult, ALU.add, [r_isel], [r_idxf])
                op("dve", lambda e, b=b: e.tensor_copy(out=idxu[b][:], in_=idxf[:]), [r_idxf], [r_idxu[b]])
                dump("idx", idxf[:], [P, 128], [r_idxf], t * P, P, S_LEN)
                tt("dve", gate[:], scv[:], scv[:, :, 0:1].to_broadcast([P, 8, 16]), ALU.subtract, [r_V12], [r_gate])
                act(gate[:], gate[:], AF.Exp, [r_gate], [r_gate])
                red(st[:, 8:16], gate[:], [r_gate], [r_st])
                op("dve", lambda e: e.reciprocal(out=st[:, 8:16], in_=st[:, 8:16]), [r_st], [r_st])
                tt("dve", gate[:], gate[:], st[:, 8:16].unsqueeze(2).to_broadcast([P, 8, 16]), ALU.mult,
                   [r_gate, r_st], [r_gate])
                for k in range(128):
                    g_ = gs.get()
                    op("pool", lambda e, g_=g_, k=k, b=b: e.indirect_dma_start(
                        out=g_.t[:, :], out_offset=None, in_=pu_d[:, :],
                        in_offset=bass.IndirectOffsetOnAxis(ap=idxu[b][:, k:k + 1], axis=0)),
                       [r_idxu[b]], [g_.r], dma=True)
                    op("dve", lambda e, g_=g_, k=k: e.scalar_tensor_tensor(
                        out=junk[:], in0=g_.t[:], scalar=1.0, in1=h2[:], op0=ALU.mult, op1=ALU.mult,
                        accum_out=acts[:, k:k + 1]), [g_.r, r_h2], [r_junk, r_acts])
                    gs.put(g_)
                dump("acts", acts[:], [P, 128], [r_acts], t * P, P, S_LEN)
                act(wts[:], acts[:], AF.Gelu, [r_acts], [r_wts])
                tt("dve", wts[:], wts[:], gate[:].rearrange("p h k -> p (h k)"), ALU.mult, [r_wts, r_gate], [r_wts])
                dump("wts", wts[:], [P, 128], [r_wts], t * P, P, S_LEN)
                for k in range(128):
                    g_ = gs.get()
                    op("pool", lambda e, g_=g_, k=k, b=b: e.indirect_dma_start(
                        out=g_.t[:, :], out_offset=None, in_=pv_d[:, :],
                        in_offset=bass.IndirectOffsetOnAxis(ap=idxu[b][:, k:k + 1], axis=0)),
                       [r_idxu[b]], [g_.r], dma=True)
                    if k == 0:
                        ts("dve", acc[:], g_.t[:], wts[:, 0:1], None, ALU.mult, None, [g_.r, r_wts], [r_acc])
                    else:
                        stt(acc[:], g_.t[:], wts[:, k:k + 1], acc[:], ALU.mult, ALU.add, [g_.r, r_wts, r_acc], [r_acc])
                    gs.put(g_)
                dump("peer", acc[:], [P, D], [r_acc], t * P, P, S_LEN)
                tt("dve", acc[:], acc[:], bc[:, 2, :], ALU.mult, [r_acc, r_bc[2]], [r_acc])
                tt("pool", acc[:], acc[:], xt[b][:], ALU.add, [r_acc, r_xt[b]], [r_acc])
                ev = dma("sp", out_d[t * P:(t + 1) * P, :], acc[:], [r_acc], [r_x1[t]])
                out_evs.append(ev)
            S.barrier()
            S.emit()

        S.final_wait("sp", out_evs)
    S.emit()
    return nc


def pack_inputs(x, c, p):
    f = np.float32
    A = lambda a: np.ascontiguousarray(np.asarray(a, f))
    d = {
        "x": A(x), "cT": A(np.asarray(c, f).reshape(8, 128).T),
        "ada_w": A(p["ada_w"]), "ada_b": A(p["ada_b"]).reshape(1, -1),
        "norm1_w": A(p["norm1_w"]).reshape(1, -1), "norm2_w": A(p["norm2_w"]).reshape(1, -1),
        "w_in": A(p["w_in"]), "shift_mu": A(p["shift_mu"]).reshape(1, -1),
        "w0a0": A(np.concatenate([np.asarray(p["w0"], f), np.asarray(p["a0"], f)])).reshape(1, -1),
        "wa_up": A(np.concatenate([np.asarray(p["w_lora_up"], f), np.asarray(p["a_lora_up"], f)], 0)),
        "g_up": A(p["g_lora_up"]),
        "vec6": A(np.stack([np.asarray(p[k], f).reshape(-1) for k in ("k_k", "k_a", "r_k", "lnx_w", "lnx_b", "lnx_b")])),
        "qk_nw": A(np.concatenate([np.asarray(p["q_norm_w"], f), np.asarray(p["k_norm_w"], f)])).reshape(1, -1),
        "sinks": A(p["sinks"]).reshape(1, -1),
        "w_out": A(p["w_out"]),
    }
    if "peer_w_q" in p:
        d["peer_w_q"] = A(p["peer_w_q"])
        d["keysT"] = A(np.concatenate([np.asarray(p["peer_keys_1"], f).T, np.asarray(p["peer_keys_2"], f).T], 1))
        d["peer_u"] = A(p["peer_u"])
        d["peer_v"] = A(p["peer_v"])
    return d


def kernel(**inputs):
    x = np.asarray(inputs["x"], np.float32)
    c = np.asarray(inputs["c"], np.float32)
    B, S_LEN, _ = x.shape
    p = {k: np.asarray(v, np.float32)[0] for k, v in inputs.items() if k not in ("x", "c")}
    nc = build(S_LEN)
    in_maps = [pack_inputs(x[b], c[b], p) for b in range(B)]
    res = run_bass_kernel_spmd(nc, in_maps, core_ids=list(range(B)))
    return np.stack([np.asarray(r["out"], np.float32) for r in res.results], 0)
```

```python
import numpy as np
import os
STOP = int(os.environ.get('STOP', '99'))
from contextlib import ExitStack
import concourse.bass as bass
import concourse.mybir as mybir
from concourse.bass_utils import run_bass_kernel_spmd

F32 = mybir.dt.float32
U32 = mybir.dt.uint32
I32 = mybir.dt.int32
ALU = mybir.AluOpType
AF = mybir.ActivationFunctionType
AX = mybir.AxisListType

D = 1024
NSH = 3328
INW = 6656
P = 128

ENG_EPOCH = int(os.environ.get('ENG_EPOCH', '30000'))
LANE_EPOCH = int(os.environ.get('LANE_EPOCH', '1900'))
N_LANES = 16


class Res:
    __slots__ = ("name", "w", "rd", "excl")

    def __init__(self, name="", excl=False):
        self.name = name
        self.w = None
        self.rd = {}
        self.excl = excl


def RL(n, name=""):
    return [Res(f"{name}{i}") for i in range(n)]


class Sched:
    def __init__(self, nc):
        self.nc = nc
        self.streams = {k: [] for k in ("pe", "act", "dve", "pool", "sp")}
        self.sems = {}
        self.ecount = {k: 0 for k in self.streams}
        self.eepoch = {k: 0 for k in self.streams}
        self.known = {k: {} for k in self.streams}
        self.lanes = {k: [[0, 0] for _ in range(N_LANES)] for k in self.streams}
        self.lane_i = {k: 0 for k in self.streams}
        self.n_ops = 0

    def op(self, eng, fn, reads=(), writes=(), dma=False):
        self.n_ops += 1
        deps = {}

        def add(ev):
            if ev is None:
                return
            k, v = ev
            if deps.get(k, 0) < v:
                deps[k] = v

        xr = [r for r in reads if r.excl]
        if xr:
            reads = [r for r in reads if not r.excl]
            writes = list(writes) + xr
        for r in reads:
            add(r.w)
        for w in writes:
            add(w.w)
            for k, v in w.rd.items():
                add((k, v))
        if dma:
            li = self.lane_i[eng]
            self.lane_i[eng] = (li + 1) % N_LANES
            lane = self.lanes[eng][li]
            if lane[1] >= LANE_EPOCH:
                add((("lane", eng, li, lane[0]), lane[1] * 16))
                lane[0] += 1
                lane[1] = 0
            key = ("lane", eng, li, lane[0])
            if lane[1] > 0:
                add((key, lane[1] * 16))
            lane[1] += 1
            ev = (key, lane[1] * 16)
            inc = 16
        else:
            if self.ecount[eng] >= ENG_EPOCH:
                self.eepoch[eng] += 1
                self.ecount[eng] = 0
            key = ("eng", eng, self.eepoch[eng])
            self.ecount[eng] += 1
            ev = (key, self.ecount[eng])
            inc = 1
        waits = []
        kn = self.known[eng]
        for k, v in deps.items():
            if eng == "pe" and k[0] == "eng" and k[1] == "pe":
                continue
            if kn.get(k, 0) >= v:
                continue
            kn[k] = v
            waits.append((k, v))
        self.streams[eng].append((waits, fn, ev[0], inc))
        for w in writes:
            w.w = ev
            w.rd = {}
        for r in reads:
            if r.rd.get(ev[0], 0) < ev[1]:
                r.rd[ev[0]] = ev[1]
        return ev

    def barrier(self):
        evs = []
        for e in self.streams:
            if self.ecount[e] > 0:
                evs.append((("eng", e, self.eepoch[e]), self.ecount[e]))
        for le in self.lanes:
            for li, lane in enumerate(self.lanes[le]):
                if lane[1] > 0:
                    evs.append((("lane", le, li, lane[0]), lane[1] * 16))
        for e in self.streams:
            waits = []
            for k, v in evs:
                if k[0] == "eng" and k[1] == e:
                    continue
                if self.known[e].get(k, 0) >= v:
                    continue
                self.known[e][k] = v
                waits.append((k, v))
            self.streams[e].append((waits, None, None, 0))

    def final_wait(self, eng, evs):
        self.streams[eng].append((list(evs), None, None, 0))

    def emit(self):
        nc = self.nc
        for eng, st in self.streams.items():
            for waits, fn, key, inc in st:
                for k, v in waits:
                    if k not in self.sems:
                        self.sems[k] = nc.alloc_semaphore("s%d" % len(self.sems))
                if key is not None and key not in self.sems:
                    self.sems[key] = nc.alloc_semaphore("s%d" % len(self.sems))
        streams = self.streams
        self.streams = {k: [] for k in streams}
        with nc.Block() as block:
            def mk(engname):
                def body(e):
                    for waits, fn, key, inc in streams[engname]:
                        for k, v in waits:
                            e.wait_ge(self.sems[k], v)
                        if fn is not None:
                            fn(e).then_inc(self.sems[key], inc)
                return body
            block.tensor(mk("pe"))
            block.scalar(mk("act"))
            block.vector(mk("dve"))
            block.gpsimd(mk("pool"))
            block.sync(mk("sp"))


class Ctx:
    def __init__(self, nc):
        self.nc = nc
        self.S = Sched(nc)
        self.n = 0

    def sb(self, es, shape, dt=F32, name=None):
        self.n += 1
        return es.enter_context(self.nc.sbuf_tensor(name or f"t{self.n}", list(shape), dt))

    def ps(self, es, shape, dt=F32, name=None):
        self.n += 1
        return es.enter_context(self.nc.psum_tensor(name or f"p{self.n}", list(shape), dt))


from collections import deque
STG = int(os.environ.get('STG', '9'))

C0 = float(np.exp(-0.5))


class TL:
    __slots__ = ("t", "r")

    def __init__(self, t, r):
        self.t = t
        self.r = r


class FPool:
    def __init__(self, items):
        self.q = deque(items)

    def get(self):
        self.out = getattr(self, "out", 0) + 1
        self.peak = max(getattr(self, "peak", 0), self.out)
        return self.q.popleft()

    def put(self, *items):
        for it in items:
            self.out -= 1
            self.q.append(it)


def build(S_LEN, dbg=None, phases=(1, 2)):
    nc = bass.Bass("TRN2", target_bir_lowering=False)
    NT = S_LEN // P
    C = Ctx(nc)
    S = C.S
    op = S.op

    def din(name, shape, dt=F32):
        return nc.dram_tensor(name, list(shape), dt, kind="ExternalInput").ap()

    x_d = din("x", [S_LEN, D])
    cT_d = din("cT", [P, 8])
    ada_w_d = din("ada_w", [D, 6 * D])
    ada_b_d = din("ada_b", [1, 6 * D])
    n1w_d = din("norm1_w", [1, D])
    n2w_d = din("norm2_w", [1, D])
    w_in_d = din("w_in", [D, INW])
    mu_d = din("shift_mu", [1, NSH])
    w0a0_d = din("w0a0", [1, 2 * D])
    wa_up_d = din("wa_up", [P, D])
    g_up_d = din("g_up", [P, D])
    vec6_d = din("vec6", [6, D])
    qk_nw_d = din("qk_nw", [1, 128])
    sinks_d = din("sinks", [1, 16])
    w_out_d = din("w_out", [D, D])
    wq_d = din("peer_w_q", [D, 2048])
    keysT_d = din("keysT", [P, 256])
    pu_d = din("peer_u", [16384, D])
    pv_d = din("peer_v", [16384, D])
    out_d = nc.dram_tensor("out", [S_LEN, D], F32, kind="ExternalOutput").ap()
    dbg_d = {}

    def dump(name, ap_sb, shape, reads, row0=None, nrows=None, total_rows=None):
        if not dbg or name not in dbg:
            return
        if name not in dbg_d:
            dbg_d[name] = nc.dram_tensor("dbg_" + name, [total_rows or shape[0]] + list(shape[1:]), F32,
                                         kind="ExternalOutput").ap()
        dst = dbg_d[name] if row0 is None else dbg_d[name][row0:row0 + nrows]
        ev = op("sp", lambda e: e.dma_start(out=dst, in_=ap_sb), reads, [], dma=True)
        out_evs.append(ev)

    ada_s = nc.dram_tensor("ada_s", [1, 6 * D], F32, kind="Internal").ap()
    w1_s = nc.dram_tensor("w1_s", [D, NSH], F32, kind="Internal").ap()
    w2_s = nc.dram_tensor("w2_s", [D, NSH], F32, kind="Internal").ap()

    out_evs = []
    r_w1s = Res(); r_w2s = Res(); r_x1 = RL(NT)

    def tt(eng, out, in0, in1, opc, R, W):
        return op(eng, lambda e: e.tensor_tensor(out=out, in0=in0, in1=in1, op=opc), R, W)

    def ts(eng, out, in0, s1, s2, o0, o1, R, W):
        if o1 is None:
            return op(eng, lambda e: e.tensor_scalar(out=out, in0=in0, scalar1=s1, scalar2=None, op0=o0), R, W)
        return op(eng, lambda e: e.tensor_scalar(out=out, in0=in0, scalar1=s1, scalar2=s2, op0=o0, op1=o1), R, W)

    def stt(out, in0, sc, in1, o0, o1, R, W):
        return op("dve", lambda e: e.scalar_tensor_tensor(out=out, in0=in0, scalar=sc, in1=in1, op0=o0, op1=o1), R, W)

    def act(out, in_, func, R, W, scale=None, bias=None, accum=None):
        kw = {}
        if scale is not None:
            kw["scale"] = scale
        if bias is not None:
            kw["bias"] = bias
        if accum is not None:
            kw["accum_out"] = accum
        return op("act", lambda e: e.activation(out=out, in_=in_, func=func, **kw), R, W)

    def mm(out, lhsT, rhs, start, stop, R, W):
        return op("pe", lambda e: e.matmul(out, lhsT, rhs, start=start, stop=stop), R, W)

    def tr(out, in_, R, W):
        return op("pe", lambda e: e.transpose(out, in_, ident[0:in_.shape[0], 0:in_.shape[0]]), list(R) + [r_ident], W)

    def red(out, in_, R, W, opc=ALU.add):
        return op("dve", lambda e: e.tensor_reduce(out=out, in_=in_, axis=AX.X, op=opc), R, W)

    def dma(eng, out, in_, R, W):
        return op(eng, lambda e: e.dma_start(out=out, in_=in_), R, W, dma=True)

    def h3(ap, k=64):
        return ap.rearrange("p (h k) -> p h k", k=k)

    def bch(ap16, n=16, k=64):
        return ap16.unsqueeze(2).to_broadcast([ap16.shape[0], n, k])

    with ExitStack() as es0:
        ident = C.sb(es0, [P, P]); r_ident = Res("ident")
        ones_row = C.sb(es0, [1, P]); r_ones = Res("ones")
        op("pool", lambda e: e.memset(ident[:], 0.0), [], [r_ident])
        op("pool", lambda e: e.affine_select(out=ident[:], in_=ident[:], pattern=[[-1, P]],
                                            compare_op=ALU.not_equal, fill=1.0, base=0,
                                            channel_multiplier=1), [r_ident], [r_ident])
        op("pool", lambda e: e.memset(ones_row[:], 1.0), [], [r_ones])

        with ExitStack() as es:
            cT = C.sb(es, [P, 8]); r_cT = Res()
            cond = C.sb(es, [P, 8]); r_cond = Res()
            arow = C.sb(es, [1, 6 * D]); r_arow = Res()
            brow = C.sb(es, [1, 6 * D]); r_brow = Res()
            nrow = C.sb(es, [1, 2 * D]); r_nrow = Res()
            wb = [C.sb(es, [P, 8, 512]) for _ in range(2)]; r_wb = RL(2)
            pa = [C.ps(es, [1, 512]) for _ in range(2)]; r_pa = RL(2)
            dma("sp", cT[:], cT_d, [], [r_cT])
            dma("sp", brow[:], ada_b_d, [], [r_brow])
            dma("sp", nrow[:, 0:D], n1w_d, [], [r_nrow])
            dma("sp", nrow[:, D:2 * D], n2w_d, [], [r_nrow])
            act(cond[:], cT[:], AF.Silu, [r_cT], [r_cond])
            aw = ada_w_d.rearrange("(kc p) n -> p kc n", p=P)
            for g in range(12):
                b = g % 2
                dma("sp", wb[b][:], aw[:, :, g * 512:(g + 1) * 512], [], [r_wb[b]])
                for kc in range(8):
                    mm(pa[b][:], cond[:, kc:kc + 1], wb[b][:, kc, :], kc == 0, kc == 7, [r_cond, r_wb[b]], [r_pa[b]])
                tt("dve", arow[:, g * 512:(g + 1) * 512], pa[b][:], brow[:, g * 512:(g + 1) * 512], ALU.add,
                   [r_pa[b], r_brow], [r_arow])
            for (slot, off) in ((1, 0), (4, D)):
                stt(arow[:, slot * D:(slot + 1) * D], arow[:, slot * D:(slot + 1) * D], 1.0, nrow[:, off:off + D],
                    ALU.add, ALU.mult, [r_arow, r_nrow], [r_arow])
            r_ada_s = Res()
            dma("sp", ada_s, arow[:], [r_arow], [r_ada_s])
            if 1 in phases:
                mub = C.sb(es, [P, NSH]); r_mub = Res()
                wld = [C.sb(es, [P, NSH]) for _ in range(2)]; r_wld = RL(2)
                w2t = [C.sb(es, [P, NSH]) for _ in range(2)]; r_w2t = RL(2)
                dma("sp", mub[:], mu_d.partition_broadcast(P), [], [r_mub])
                for kc in range(8):
                    b = kc % 2
                    dma("sp", wld[b][:], w_in_d[kc * P:(kc + 1) * P, 0:NSH], [], [r_wld[b]])
                    tt("dve", w2t[b][:], wld[b][:], mub[:], ALU.mult, [r_wld[b], r_mub], [r_w2t[b]])
                    tt("pool", wld[b][:], wld[b][:], w2t[b][:], ALU.subtract, [r_wld[b], r_w2t[b]], [r_wld[b]])
                    dma("sp", w1_s[kc * P:(kc + 1) * P, :], wld[b][:], [r_wld[b]], [r_w1s])
                    dma("sp", w2_s[kc * P:(kc + 1) * P, :], w2t[b][:], [r_w2t[b]], [r_w2s])

            S.barrier()
            S.emit()

        if 1 in phases:
          with ExitStack() as es:
            wa_up = C.sb(es, [P, D]); r_waup = Res()
            g_up = C.sb(es, [P, D]); r_gup = Res()
            w0a0 = C.sb(es, [1, 2 * D]); r_w0a0 = Res()
            dma("sp", wa_up[:], wa_up_d, [], [r_waup])
            dma("sp", g_up[:], g_up_d, [], [r_gup])
            dma("sp", w0a0[:], w0a0_d, [], [r_w0a0])
            tri = C.sb(es, [P, 3, P]); r_tri = Res()
            op("pool", lambda e: e.memset(tri[:], 1.0), [], [r_tri])
            for i, (pat, cm, cop) in enumerate((([[1, P]], -1, ALU.is_ge), ([[1, P]], -1, ALU.is_gt),
                                                ([[-1, P]], 1, ALU.is_gt))):
                op("pool", lambda e, i=i, pat=pat, cm=cm, cop=cop: e.affine_select(
                    out=tri[:, i, :], in_=tri[:, i, :], pattern=pat, compare_op=cop, fill=0.0, base=0,
                    channel_multiplier=cm), [r_tri], [r_tri])
            TRI, TRIS, TRIL = tri[:, 0, :], tri[:, 1, :], tri[:, 2, :]
            qkw = C.sb(es, [P, 128]); r_qkw = Res()
            dma("sp", qkw[:], qk_nw_d.partition_broadcast(P), [], [r_qkw])
            esk = C.sb(es, [P, 16]); r_esk = Res()
            dma("sp", esk[:], sinks_d.partition_broadcast(P), [], [r_esk])
            act(esk[:], esk[:], AF.Exp, [r_esk], [r_esk])
            keysT = C.sb(es, [P, 256]); r_keys = Res()
            dma("sp", keysT[:], keysT_d, [], [r_keys])
            iot = C.sb(es, [P, 16]); r_iot = Res()
            op("pool", lambda e: e.iota(iot[:], pattern=[[1, 16]], base=0, channel_multiplier=0,
                                        allow_small_or_imprecise_dtypes=True), [], [r_iot])

            xt = [C.sb(es, [P, D]) for _ in range(2)]; r_xt = RL(2)
            st = C.sb(es, [P, 64]); r_st = Res()
            st2 = C.sb(es, [P, 32]); r_st2 = Res()
            hT = [C.sb(es, [P, 8, P + 1]) for _ in range(2)]; r_hT = RL(2)
            NWK = 3
            wk = [C.sb(es, [P, 8, 256]) for _ in range(NWK)]; r_wk = RL(NWK)
            kT = [C.sb(es, [64, 2, P]) for _ in range(2)]; r_kT = RL(2)
            vv = [C.sb(es, [P, 2, 65]) for _ in range(2)]; r_vv = RL(2)
            STt = C.sb(es, [64, 16, 64]); r_ST = Res()
            WC = C.sb(es, [64, 16]); r_WC = Res()
            op("pool", lambda e: e.memset(STt[:], 0.0), [], [r_ST])
            for b_ in range(2):
                op("pool", lambda e, b_=b_: e.memset(vv[b_][:], 1.0), [], [r_vv[b_]])
            tmpk = C.sb(es, [P, 256]); r_tmpk = Res()
            V12 = C.sb(es, [P, 16, 16]); r_V12 = Res()
            I12 = C.sb(es, [P, 16, 16], U32); r_I12 = Res()
            I12f = C.sb(es, [P, 16, 16]); r_I12f = Res()
            scv = C.sb(es, [P, 8, 16]); r_scv = Res()
            posu = C.sb(es, [P, 8, 16], U32); r_posu = Res()
            pa_u = C.sb(es, [P, 2, 128], U32); r_pau = Res()
            pa_f = C.sb(es, [P, 2, 128]); r_paf = Res()
            isel = C.sb(es, [P, 2, 128]); r_isel = Res()
            idxf = C.sb(es, [P, 128]); r_idxf = Res()
            idxu = C.sb(es, [P, 128], I32); r_idxu = Res()
            gate = C.sb(es, [P, 8, 16]); r_gate = Res()
            acts = C.sb(es, [P, 128]); r_acts = Res()
            wts = C.sb(es, [P, 128]); r_wts = Res()
            NG = int(os.environ.get("NG", "10"))
            gs = FPool([TL(C.sb(es, [P, D]), Res(f"g{i}")) for i in range(NG)])
            NPOOL = int(os.environ.get("NPOOL", "20"))
            pool = FPool([TL(C.sb(es, [P, D]), Res(f"pl{i}")) for i in range(NPOOL)])
            pp = FPool([TL(C.ps(es, [P, 1024]), Res(f"ps{i}", excl=True)) for i in range(4)])
            w1v = w1_s.rearrange("(kc p) n -> p kc n", p=P)
            w2v = w2_s.rearrange("(kc p) n -> p kc n", p=P)
            wiv = w_in_d.rearrange("(kc p) n -> p kc n", p=P)
            wov = w_out_d.rearrange("(kc p) n -> p kc n", p=P)
            wqv = wq_d.rearrange("(kc p) n -> p kc n", p=P)
            wki = [0]
            op("pool", lambda e: e.memset(hT[1][:, :, P:P + 1], 0.0), [], [r_hT[1]])

            def wslot():
                s_ = wki[0] % NWK
                wki[0] += 1
                return s_

            def bvec(src_ap):
                tl = pool.get()
                dma("sp", tl.t[:], src_ap.partition_broadcast(P), [r_ada_s], [tl.r])
                return tl

            def ada_row(slot):
                return ada_s[:, slot * D:(slot + 1) * D]

            def proj_cols(b, kind, c0, width, ps):
                for g0 in range(0, width, 256):
                    wd = min(256, width - g0)
                    cc = c0 + g0
                    if kind == "s":
                        s1 = wslot(); s2 = wslot()
                        dma("sp", wk[s1][:, :, 0:wd], w1v[:, :, cc:cc + wd], [r_w1s], [r_wk[s1]])
                        dma("sp", wk[s2][:, :, 0:wd], w2v[:, :, cc:cc + wd], [r_w2s], [r_wk[s2]])
                        for kc in range(8):
                            mm(ps.t[:, g0:g0 + wd], hT[b][:, kc, 1:P + 1], wk[s1][:, kc, 0:wd], kc == 0, False,
                               [r_hT[b], r_wk[s1]], [ps.r])
                        for kc in range(8):
                            mm(ps.t[:, g0:g0 + wd], hT[b][:, kc, 0:P], wk[s2][:, kc, 0:wd], False, kc == 7,
                               [r_hT[b], r_wk[s2]], [ps.r])
                    else:
                        s1 = wslot()
                        dma("sp", wk[s1][:, :, 0:wd], wiv[:, :, NSH + cc:NSH + cc + wd], [], [r_wk[s1]])
                        for kc in range(8):
                            mm(ps.t[:, g0:g0 + wd], hT[b][:, kc, 1:P + 1], wk[s1][:, kc, 0:wd], kc == 0, kc == 7,
                               [r_hT[b], r_wk[s1]], [ps.r])

            def rstd_of(ss_ap, scale, eps, R):
                ts("dve", ss_ap, ss_ap, scale, eps, ALU.mult, ALU.add, R, R)
                act(ss_ap, ss_ap, AF.Sqrt, R, R)
                op("dve", lambda e: e.reciprocal(out=ss_ap, in_=ss_ap), R, R)

            def top16(src, dstv, dsti, n, R_src):
                op("dve", lambda e: e.max(out=dstv[:, 0:8], in_=src), R_src, [r_V12])
                op("dve", lambda e: e.match_replace(out=tmpk[:, 0:n], in_to_replace=dstv[:, 0:8], in_values=src,
                                                    imm_value=-1e30), R_src + [r_V12], [r_tmpk])
                op("dve", lambda e: e.max(out=dstv[:, 8:16], in_=tmpk[:, 0:n]), [r_tmpk], [r_V12])
                op("dve", lambda e: e.max_index(out=dsti[:, 0:8], in_max=dstv[:, 0:8], in_values=src),
                   R_src + [r_V12], [r_I12])
                op("dve", lambda e: e.max_index(out=dsti[:, 8:16], in_max=dstv[:, 8:16], in_values=tmpk[:, 0:n]),
                   [r_tmpk, r_V12], [r_I12])

            def peer_tile(t, x1):
                h2 = pool.get()
                act(h2.t[:], x1.t[:], AF.Square, [x1.r], [h2.r, r_st2], accum=st2[:, 0:1])
                rstd_of(st2[:, 0:1], 1.0 / D, 1e-6, [r_st2])
                g2 = bvec(ada_row(4))
                stt(h2.t[:], x1.t[:], st2[:, 0:1], g2.t[:], ALU.mult, ALU.mult, [x1.r, r_st2, g2.r], [h2.r])
                pool.put(g2)
                sh2 = bvec(ada_row(3))
                tt("pool", h2.t[:], h2.t[:], sh2.t[:], ALU.add, [h2.r, sh2.r], [h2.r])
                pool.put(sh2)
                h2T = pool.get()
                ps = pp.get()
                for kc in range(8):
                    tr(ps.t[:, kc * P:(kc + 1) * P], h2.t[:, kc * P:(kc + 1) * P], [h2.r], [ps.r])
                act(h2T.t[:], ps.t[:], AF.Copy, [ps.r], [h2T.r]); pp.put(ps)
                qT = [pool.get(), pool.get()]
                for g in range(8):
                    s1 = wslot()
                    dma("sp", wk[s1][:], wqv[:, :, g * 256:(g + 1) * 256], [], [r_wk[s1]])
                    if g % 4 == 0:
                        ps = pp.get()
                    for j in range(2):
                        hj = g * 2 + j
                        o_ = ps.t[:, (hj % 8) * P:(hj % 8 + 1) * P]
                        for kc in range(8):
                            mm(o_, wk[s1][:, kc, j * P:(j + 1) * P], h2T.t[:, kc * P:(kc + 1) * P], kc == 0, kc == 7,
                               [r_wk[s1], h2T.r], [ps.r])
                    if g % 4 == 3:
                        act(qT[g // 4].t[:], ps.t[:], AF.Copy, [ps.r], [qT[g // 4].r]); pp.put(ps)
                pool.put(h2T)
                yield
                s12 = [pool.get(), pool.get()]
                for half in range(2):
                    ps = pp.get()
                    for q_ in range(8):
                        j = q_ % 2
                        mm(ps.t[:, q_ * P:(q_ + 1) * P], qT[half].t[:, q_ * P:(q_ + 1) * P], keysT[:, j * P:(j + 1) * P],
                           True, True, [qT[half].r, r_keys], [ps.r])
                    act(s12[half].t[:], ps.t[:], AF.Copy, [ps.r], [s12[half].r]); pp.put(ps)
                pool.put(*qT)
                for hj in range(16):
                    top16(s12[hj // 8].t[:, (hj % 8) * P:(hj % 8 + 1) * P], V12[:, hj, :], I12[:, hj, :], 128,
                          [s12[hj // 8].r])
                    yield
                pool.put(*s12)
                cand = [pool.get(), pool.get()]
                V4 = V12[:].rearrange("p (h j) k -> p h j k", j=2)
                for half in range(2):
                    tt("dve", cand[half].t[:].rearrange("p (h a b) -> p h a b", a=16, b=16),
                       V4[:, half * 4:(half + 1) * 4, 0, :].unsqueeze(3).to_broadcast([P, 4, 16, 16]),
                       V4[:, half * 4:(half + 1) * 4, 1, :].unsqueeze(2).to_broadcast([P, 4, 16, 16]), ALU.add,
                       [r_V12], [cand[half].r])
                op("dve", lambda e: e.tensor_copy(out=I12f[:], in_=I12[:]), [r_I12], [r_I12f])
                yield
                for h in range(8):
                    top16(cand[h // 4].t[:, (h % 4) * 256:(h % 4 + 1) * 256], scv[:, h, :], posu[:, h, :], 256,
                          [cand[h // 4].r])
                    yield
                pool.put(*cand)
                posf2 = posu[:].rearrange("p h k -> p (h k)")
                op("dve", lambda e: e.tensor_single_scalar(out=pa_u[:, 0, :], in_=posf2, scalar=4,
                                                           op=ALU.logical_shift_right), [r_I12], [r_pau])
                op("dve", lambda e: e.tensor_single_scalar(out=pa_u[:, 1, :], in_=posf2, scalar=15,
                                                           op=ALU.bitwise_and), [r_I12], [r_pau])
                op("dve", lambda e: e.tensor_copy(out=pa_f[:], in_=pa_u[:]), [r_pau], [r_paf])
                I4 = I12f[:].rearrange("p (h j) k -> p h j k", j=2)
                for j in range(2):
                    for half in range(2):
                        oh = pool.get()
                        oh4 = oh.t[:].rearrange("p (h k a) -> p h k a", k=16, a=16)
                        sel_f = pa_f[:, j, half * 64:(half + 1) * 64].rearrange("p (h k) -> p h k", k=16)
                        tt("dve", oh4, sel_f.unsqueeze(3).to_broadcast([P, 4, 16, 16]),
                           iot[:].unsqueeze(1).unsqueeze(1).to_broadcast([P, 4, 16, 16]), ALU.is_equal,
                           [r_paf, r_iot], [oh.r])
                        tt("dve", oh4, oh4, I4[:, half * 4:(half + 1) * 4, j, :].unsqueeze(2).to_broadcast([P, 4, 16, 16]),
                           ALU.mult, [oh.r, r_I12f], [oh.r])
                        red(isel[:, j, half * 64:(half + 1) * 64], oh.t[:].rearrange("p (hk a) -> p hk a", a=16),
                            [oh.r], [r_isel])
                        pool.put(oh)
                        yield
                stt(idxf[:], isel[:, 0, :], 128.0, isel[:, 1, :], ALU.mult, ALU.add, [r_isel], [r_idxf])
                op("dve", lambda e: e.tensor_copy(out=idxu[:], in_=idxf[:]), [r_idxf], [r_idxu])
                dump("idx", idxf[:], [P, 128], [r_idxf], t * P, P, S_LEN)
                tt("dve", gate[:], scv[:], scv[:, :, 0:1].to_broadcast([P, 8, 16]), ALU.subtract, [r_V12], [r_gate])
                act(gate[:], gate[:], AF.Exp, [r_gate], [r_gate])
                red(st2[:, 8:16], gate[:], [r_gate], [r_st2])
                op("dve", lambda e: e.reciprocal(out=st2[:, 8:16], in_=st2[:, 8:16]), [r_st2], [r_st2])
                tt("dve", gate[:], gate[:], st2[:, 8:16].unsqueeze(2).to_broadcast([P, 8, 16]), ALU.mult,
                   [r_gate, r_st2], [r_gate])
                yield
                for k in range(128):
                    g_ = gs.get()
                    op("pool", lambda e, g_=g_, k=k: e.indirect_dma_start(
                        out=g_.t[:, :], out_offset=None, in_=pu_d[:, :],
                        in_offset=bass.IndirectOffsetOnAxis(ap=idxu[:, k:k + 1], axis=0)),
                       [r_idxu], [g_.r], dma=True)
                    op("dve", lambda e, g_=g_, k=k, h2=h2: e.scalar_tensor_tensor(
                        out=g_.t[:], in0=g_.t[:], scalar=1.0, in1=h2.t[:], op0=ALU.mult, op1=ALU.mult,
                        accum_out=acts[:, k:k + 1]), [g_.r, h2.r], [g_.r, r_acts])
                    gs.put(g_)
                    yield
                pool.put(h2)
                act(wts[:], acts[:], AF.Gelu, [r_acts], [r_wts])
                tt("dve", wts[:], wts[:], gate[:].rearrange("p h k -> p (h k)"), ALU.mult, [r_wts, r_gate], [r_wts])
                acc = pool.get()
                for k in range(128):
                    g_ = gs.get()
                    op("pool", lambda e, g_=g_, k=k: e.indirect_dma_start(
                        out=g_.t[:, :], out_offset=None, in_=pv_d[:, :],
                        in_offset=bass.IndirectOffsetOnAxis(ap=idxu[:, k:k + 1], axis=0)),
                       [r_idxu], [g_.r], dma=True)
                    if k == 0:
                        ts("dve", acc.t[:], g_.t[:], wts[:, 0:1], None, ALU.mult, None, [g_.r, r_wts], [acc.r])
                    else:
                        stt(acc.t[:], g_.t[:], wts[:, k:k + 1], acc.t[:], ALU.mult, ALU.add, [g_.r, r_wts, acc.r], [acc.r])
                    gs.put(g_)
                    yield
                dump("peer", acc.t[:], [P, D], [acc.r], t * P, P, S_LEN)
                gt2 = bvec(ada_row(5))
                tt("dve", acc.t[:], acc.t[:], gt2.t[:], ALU.mult, [acc.r, gt2.r], [acc.r])
                pool.put(gt2)
                tt("pool", acc.t[:], acc.t[:], x1.t[:], ALU.add, [acc.r, x1.r], [acc.r])
                ev = dma("sp", out_d[t * P:(t + 1) * P, :], acc.t[:], [acc.r], [])
                out_evs.append(ev)
                pool.put(acc, x1)

            gen = [None]
            PUMP = int(os.environ.get("PUMP", "3"))
            base_op = S.op
            busy = [False]

            def pump(n):
                if gen[0] is None or busy[0]:
                    return
                busy[0] = True
                try:
                    for _ in range(n):
                        next(gen[0])
                except StopIteration:
                    gen[0] = None
                busy[0] = False

            def op_p(eng, fn, reads=(), writes=(), dma=False):
                ev = base_op(eng, fn, reads, writes, dma)
                if eng == "dve" and not busy[0]:
                    pump(PUMP)
                return ev

            def drain():
                while gen[0] is not None:
                    pump(64)

            for t in range(NT):
                b = t % 2
                op = op_p
                dma("sp", xt[b][:], x_d[t * P:(t + 1) * P, :], [], [r_xt[b]])
                hh = pool.get()
                act(hh.t[:], xt[b][:], AF.Square, [r_xt[b]], [hh.r, r_st], accum=st[:, 0:1])
                rstd_of(st[:, 0:1], 1.0 / D, 1e-6, [r_st])
                g1 = bvec(ada_row(1))
                stt(hh.t[:], xt[b][:], st[:, 0:1], g1.t[:], ALU.mult, ALU.mult, [r_xt[b], r_st, g1.r], [hh.r])
                pool.put(g1)
                sh1 = bvec(ada_row(0))
                tt("pool", hh.t[:], hh.t[:], sh1.t[:], ALU.add, [hh.r, sh1.r], [hh.r])
                pool.put(sh1)
                op("pool", lambda e, b=b: e.tensor_copy(out=hT[b][:, :, 0:1], in_=hT[1 - b][:, :, P:P + 1]),
                   [r_hT[1 - b]], [r_hT[b]])
                ps = pp.get()
                for kc in range(8):
                    tr(ps.t[:, kc * P:(kc + 1) * P], hh.t[:, kc * P:(kc + 1) * P], [hh.r], [ps.r])
                act(hT[b][:, :, 1:P + 1], ps.t[:].rearrange("p (q n) -> p q n", q=8), AF.Copy, [ps.r], [r_hT[b]])
                pp.put(ps); pool.put(hh)

                aq = pool.get(); akav = pool.get()
                ps = pp.get(); proj_cols(b, "r", 0, 1024, ps)
                act(aq.t[:], ps.t[:], AF.Copy, [ps.r], [aq.r]); pp.put(ps)
                ps = pp.get(); proj_cols(b, "r", 1024, 256, ps)
                act(akav.t[:, 0:256], ps.t[:, 0:256], AF.Copy, [ps.r], [akav.r]); pp.put(ps)
                sq = pool.get()
                tt("pool", sq.t[:], aq.t[:], aq.t[:], ALU.mult, [aq.r], [sq.r])
                red(st[:, 8:24], h3(sq.t[:]), [sq.r], [r_st])
                tt("pool", sq.t[:, 0:128], akav.t[:, 0:128], akav.t[:, 0:128], ALU.mult, [akav.r], [sq.r])
                red(st[:, 24:26], h3(sq.t[:, 0:128]), [sq.r], [r_st])
                pool.put(sq)
                rstd_of(st[:, 8:26], 1.0 / 64, 1e-6, [r_st])
                tt("dve", h3(aq.t[:]), h3(aq.t[:]), bch(st[:, 8:24]), ALU.mult, [aq.r, r_st], [aq.r])
                tt("pool", h3(aq.t[:]), h3(aq.t[:]), qkw[:, 0:64].unsqueeze(1).to_broadcast([P, 16, 64]), ALU.mult,
                   [aq.r, r_qkw], [aq.r])
                tt("dve", h3(akav.t[:, 0:128]), h3(akav.t[:, 0:128]), bch(st[:, 24:26], 2), ALU.mult,
                   [akav.r, r_st], [akav.r])
                tt("pool", h3(akav.t[:, 0:128]), h3(akav.t[:, 0:128]),
                   qkw[:, 64:128].unsqueeze(1).to_broadcast([P, 2, 64]), ALU.mult, [akav.r, r_qkw], [akav.r])
                op("pool", lambda e, b=b, akav=akav: e.tensor_copy(out=vv[b][:, :, 0:64], in_=h3(akav.t[:, 128:256])),
                   [akav.r], [r_vv[b]])
                qT = [pool.get(), pool.get()]
                for half in range(2):
                    ps = pp.get()
                    for j in range(8):
                        hd = half * 8 + j
                        tr(ps.t[0:64, j * P:(j + 1) * P], aq.t[:, hd * 64:(hd + 1) * 64], [aq.r], [ps.r])
                    act(qT[half].t[0:64, :], ps.t[0:64, :], AF.Copy, [ps.r], [qT[half].r]); pp.put(ps)
                ps = pp.get()
                for g in range(2):
                    tr(ps.t[0:64, g * P:(g + 1) * P], akav.t[:, g * 64:(g + 1) * 64], [akav.r], [ps.r])
                act(kT[b][:].rearrange("p g n -> p (g n)"), ps.t[0:64, 0:256], AF.Copy, [ps.r], [r_kT[b]]); pp.put(ps)
                pool.put(aq, akav)
                yb = pool.get()
                for g in range(2):
                    Ec = pool.get(); Ep = pool.get()
                    for (E, kb, mask) in ((Ec, b, TRI), (Ep, 1 - b, TRIL)):
                        if E is Ep and t == 0:
                            continue
                        ps = pp.get()
                        for hf in range(2):
                            mm(ps.t[:, hf * 512:(hf + 1) * 512], kT[kb][:, g, :], qT[g].t[0:64, hf * 512:(hf + 1) * 512],
                               True, True, [r_kT[kb], qT[g].r], [ps.r])
                        act(E.t[:], ps.t[:], AF.Exp, [ps.r], [E.r], scale=0.125); pp.put(ps)
                        tt("pool", E.t[:].rearrange("p (h n) -> p h n", n=P), E.t[:].rearrange("p (h n) -> p h n", n=P),
                           mask.unsqueeze(1).to_broadcast([P, 8, P]), ALU.mult, [E.r, r_tri], [E.r])
                    ps = pp.get()
                    for j in range(8):
                        mm(ps.t[:, j * 128:j * 128 + 65], Ec.t[:, j * P:(j + 1) * P], vv[b][:, g, :], True, t == 0,
                           [Ec.r, r_vv[b]], [ps.r])
                        if t > 0:
                            mm(ps.t[:, j * 128:j * 128 + 65], Ep.t[:, j * P:(j + 1) * P], vv[1 - b][:, g, :], False, True,
                               [Ep.r, r_vv[1 - b]], [ps.r])
                    o3 = ps.t[:].rearrange("p (h n) -> p h n", n=128)
                    tt("dve", st[:, 32:40], o3[:, :, 64], esk[:, g * 8:(g + 1) * 8], ALU.add, [ps.r, r_esk], [r_st])
                    op("dve", lambda e: e.reciprocal(out=st[:, 32:40], in_=st[:, 32:40]), [r_st], [r_st])
                    tt("dve", h3(yb.t[:, g * 512:(g + 1) * 512]), o3[:, :, 0:64], bch(st[:, 32:40], 8), ALU.mult,
                       [ps.r, r_st], [yb.r])
                    pp.put(ps); pool.put(Ec, Ep)
                pool.put(*qT)
                dump("yb", yb.t[:], [P, D], [yb.r], t * P, P, S_LEN)

                pr = pool.get(); pk = pool.get(); pv = pool.get(); lw = pool.get()
                for (dst, c0) in ((pr, 0), (pk, 1024), (pv, 2048)):
                    ps = pp.get(); proj_cols(b, "s", c0, 1024, ps)
                    act(dst.t[:], ps.t[:], AF.Copy, [ps.r], [dst.r]); pp.put(ps)
                ps = pp.get(); proj_cols(b, "s", 3072, 256, ps)
                act(lw.t[:, 0:64], ps.t[:, 0:64], AF.Tanh, [ps.r], [lw.r])
                act(lw.t[:, 64:128], ps.t[:, 64:128], AF.Copy, [ps.r], [lw.r])
                act(lw.t[:, 128:256], ps.t[:, 128:256], AF.Sigmoid, [ps.r], [lw.r]); pp.put(ps)
                ps = pp.get()
                tr(ps.t[:, 0:P], lw.t[:, 0:128], [lw.r], [ps.r])
                tr(ps.t[:, P:2 * P], lw.t[:, 128:256], [lw.r], [ps.r])
                act(lw.t[:, 256:512], ps.t[:, 0:256], AF.Copy, [ps.r], [lw.r]); pp.put(ps)
                sgT = lw.t[:, 384:512]
                sw = pool.get(); a_ = pool.get(); g_sb = pool.get()
                ps = pp.get()
                for hf in range(2):
                    cs = slice(hf * 512, (hf + 1) * 512)
                    mm(ps.t[:, cs], lw.t[0:64, 256:384], wa_up[0:64, cs], True, False, [lw.r, r_waup], [ps.r])
                    mm(ps.t[:, cs], ones_row[0:1, :], w0a0[0:1, hf * 512:(hf + 1) * 512], False, True,
                       [r_ones, r_w0a0], [ps.r])
                act(sw.t[:], ps.t[:], AF.Sigmoid, [ps.r], [sw.r]); pp.put(ps)
                ps = pp.get()
                for hf in range(2):
                    cs = slice(hf * 512, (hf + 1) * 512)
                    mm(ps.t[:, cs], lw.t[64:128, 256:384], wa_up[64:128, cs], True, False, [lw.r, r_waup], [ps.r])
                    mm(ps.t[:, cs], ones_row[0:1, :], w0a0[0:1, D + hf * 512:D + (hf + 1) * 512], False, True,
                       [r_ones, r_w0a0], [ps.r])
                act(a_.t[:], ps.t[:], AF.Sigmoid, [ps.r], [a_.r]); pp.put(ps)
                ps = pp.get()
                for hf in range(2):
                    cs = slice(hf * 512, (hf + 1) * 512)
                    mm(ps.t[:, cs], sgT, g_up[:, cs], True, True, [lw.r, r_gup], [ps.r])
                act(g_sb.t[:], ps.t[:], AF.Copy, [ps.r], [g_sb.r]); pp.put(ps)
                pool.put(lw)
                eW = pool.get(); eWi = pool.get(); eWx = pool.get()
                ps = pp.get()
                for hf in range(2):
                    cs = slice(hf * 512, (hf + 1) * 512)
                    mm(ps.t[:, cs], TRI, sw.t[:, cs], True, True, [r_tri, sw.r], [ps.r])
                act(eW.t[:], ps.t[:], AF.Exp, [ps.r], [eW.r], scale=-C0)
                act(eWi.t[:], ps.t[:], AF.Exp, [ps.r], [eWi.r], scale=C0); pp.put(ps)
                ps = pp.get()
                for hf in range(2):
                    cs = slice(hf * 512, (hf + 1) * 512)
                    mm(ps.t[:, cs], TRIS, sw.t[:, cs], True, True, [r_tri, sw.r], [ps.r])
                act(eWx.t[:], ps.t[:], AF.Exp, [ps.r], [eWx.r], scale=-C0); pp.put(ps)
                pool.put(sw)
                kk = pool.get(); tmp = pool.get()
                kkb = bvec(vec6_d[0:1, :])
                tt("dve", kk.t[:], pk.t[:], kkb.t[:], ALU.mult, [pk.r, kkb.r], [kk.r])
                pool.put(kkb)
                tt("pool", tmp.t[:], kk.t[:], kk.t[:], ALU.mult, [kk.r], [tmp.r])
                red(st[:, 40:56], h3(tmp.t[:]), [tmp.r], [r_st])
                act(st[:, 40:56], st[:, 40:56], AF.Sqrt, [r_st], [r_st])
                ts("dve", st[:, 40:56], st[:, 40:56], 1e-12, None, ALU.max, None, [r_st], [r_st])
                op("dve", lambda e: e.reciprocal(out=st[:, 40:56], in_=st[:, 40:56]), [r_st], [r_st])
                tt("dve", h3(kk.t[:]), h3(kk.t[:]), bch(st[:, 40:56]), ALU.mult, [kk.r, r_st], [kk.r])
                kv_ = pool.get()
                kab = bvec(vec6_d[1:2, :])
                stt(tmp.t[:], a_.t[:], -1.0, kab.t[:], ALU.add, ALU.mult, [a_.r, kab.r, tmp.r], [tmp.r])
                pool.put(kab)
                stt(kv_.t[:], tmp.t[:], 1.0, pk.t[:], ALU.add, ALU.mult, [tmp.r, pk.r], [kv_.r])
                pool.put(pk)
                At = pool.get(); Bt = pool.get(); Kt = pool.get(); Rt = pool.get()
                stt(At.t[:], kk.t[:], -1.0, eWx.t[:], ALU.mult, ALU.mult, [kk.r, eWx.r], [At.r])
                tt("pool", Bt.t[:], kk.t[:], a_.t[:], ALU.mult, [kk.r, a_.r], [Bt.r])
                tt("dve", Bt.t[:], Bt.t[:], eWi.t[:], ALU.mult, [Bt.r, eWi.r], [Bt.r])
                tt("dve", Kt.t[:], kv_.t[:], eWi.t[:], ALU.mult, [kv_.r, eWi.r], [Kt.r])
                tt("pool", Rt.t[:], pr.t[:], eW.t[:], ALU.mult, [pr.r, eW.r], [Rt.r])
                pool.put(kk, a_, eWi, eWx)
                tt("pool", tmp.t[:], pr.t[:], kv_.t[:], ALU.mult, [pr.r, kv_.r, tmp.r], [tmp.r])
                rkb = bvec(vec6_d[2:3, :])
                tt("dve", tmp.t[:], tmp.t[:], rkb.t[:], ALU.mult, [tmp.r, rkb.r], [tmp.r])
                pool.put(rkb)
                red(st[:, 8:24], h3(tmp.t[:]), [tmp.r], [r_st])
                tt("dve", h3(tmp.t[:]), h3(pv.t[:]), bch(st[:, 8:24]), ALU.mult, [pv.r, r_st, tmp.r], [tmp.r])
                bonus = tmp
                pool.put(pr, kv_)
                ps = pp.get()
                for hd in range(16):
                    mm(ps.t[0:64, hd:hd + 1], eW.t[:, hd * 64:(hd + 1) * 64], ident[:, P - 1:P], True, True,
                       [eW.r, r_ident], [ps.r])
                act(WC[:], ps.t[0:64, 0:16], AF.Copy, [ps.r], [r_WC]); pp.put(ps)
                pool.put(eW)
                yrec = pool.get()
                m_s = TRIS.unsqueeze(1).to_broadcast([P, 4, P]); m_i = TRI.unsqueeze(1).to_broadcast([P, 4, P])
                m_l = TRIL.unsqueeze(1).to_broadcast([P, 4, P])
                for hg in range(4):
                    FTa = pool.get(); FTb = pool.get()
                    for j in range(4):
                        hd = hg * 4 + j
                        hs = slice(hd * 64, (hd + 1) * 64)
                        ps = pp.get()
                        tr(ps.t[0:64, 0:128], At.t[:, hs], [At.r], [ps.r])
                        tr(ps.t[0:64, 128:256], Rt.t[:, hs], [Rt.r], [ps.r])
                        tr(ps.t[0:64, 256:384], Bt.t[:, hs], [Bt.r], [ps.r])
                        tr(ps.t[0:64, 384:512], Kt.t[:, hs], [Kt.r], [ps.r])
                        act(FTa.t[0:64, j * 256:(j + 1) * 256], ps.t[0:64, 0:256], AF.Copy, [ps.r], [FTa.r])
                        act(FTb.t[0:64, j * 256:(j + 1) * 256], ps.t[0:64, 256:512], AF.Copy, [ps.r], [FTb.r])
                        pp.put(ps)
                    MX = pool.get(); Lp = pool.get(); Aak = pool.get(); AR = pool.get(); QU = pool.get()
                    MX4 = MX.t[:].rearrange("p (h s n) -> p h s n", h=4, s=2)
                    AR4 = AR.t[:].rearrange("p (h s n) -> p h s n", h=4, s=2)
                    psM = pp.get(); psK = pp.get(); psL = pp.get()
                    for j in range(4):
                        fa = FTa.t[0:64, j * 256:(j + 1) * 256]
                        mm(psM.t[:, j * 256:(j + 1) * 256], FTb.t[0:64, j * 256:j * 256 + 128], fa, True, True,
                           [FTa.r, FTb.r], [psM.r])
                        mm(psK.t[:, j * 256:(j + 1) * 256], FTb.t[0:64, j * 256 + 128:(j + 1) * 256], fa, True, True,
                           [FTa.r, FTb.r], [psK.r])
                        mm(psL.t[:, j * 128:(j + 1) * 128], FTa.t[0:64, j * 256:j * 256 + 128],
                           FTb.t[0:64, j * 256:j * 256 + 128], True, True, [FTa.r, FTb.r], [psL.r])
                    pM4 = psM.t[:].rearrange("p (h s n) -> p h s n", h=4, s=2)
                    pK4 = psK.t[:].rearrange("p (h s n) -> p h s n", h=4, s=2)
                    tt("dve", MX4[:, :, 0, :], pM4[:, :, 0, :], m_s, ALU.mult, [psM.r, r_tri], [MX.r])
                    tt("dve", AR4[:, :, 0, :], pM4[:, :, 1, :], m_i, ALU.mult, [psM.r, r_tri], [AR.r])
                    tt("dve", Aak.t[:, 0:512].rearrange("p (h n) -> p h n", n=P), pK4[:, :, 0, :], m_s, ALU.mult,
                       [psK.r, r_tri], [Aak.r])
                    tt("dve", AR4[:, :, 1, :], pK4[:, :, 1, :], m_i, ALU.mult, [psK.r, r_tri], [AR.r])
                    tt("dve", Lp.t[:, 0:512].rearrange("p (h n) -> p h n", n=P),
                       psL.t[:, 0:512].rearrange("p (h n) -> p h n", n=P), m_l, ALU.mult, [psL.r, r_tri], [Lp.r])
                    pp.put(psM, psK, psL)
                    tt("pool", MX4[:, :, 1, :], MX4[:, :, 0, :], ident[:].unsqueeze(1).to_broadcast([P, 4, P]), ALU.add,
                       [MX.r, r_ident], [MX.r])
                    psQ = pp.get()
                    for j in range(4):
                        hd = hg * 4 + j
                        mm(psQ.t[:, j * 64:(j + 1) * 64], Aak.t[:, j * P:(j + 1) * P], pv.t[:, hd * 64:(hd + 1) * 64],
                           True, True, [Aak.r, pv.r], [psQ.r])
                    act(QU.t[:, 0:256], psQ.t[:, 0:256], AF.Copy, [psQ.r], [QU.r]); pp.put(psQ)
                    p_ = 1
                    while p_ <= 64:
                        if p_ < 64:
                            psA = pp.get(); psB = pp.get()
                            for j in range(4):
                                if p_ == 1:
                                    mm(psA.t[:, j * 256:j * 256 + 128], Lp.t[:, j * P:(j + 1) * P], MX4[:, j, 0, :],
                                       True, True, [Lp.r, MX.r], [psA.r])
                                else:
                                    mm(psA.t[:, j * 256:(j + 1) * 256], Lp.t[:, j * P:(j + 1) * P],
                                       MX.t[:, j * 256:(j + 1) * 256], True, True, [Lp.r, MX.r], [psA.r])
                                mm(psB.t[:, j * P:(j + 1) * P], MX4[:, j, 0, :], Lp.t[:, j * P:(j + 1) * P], True, True,
                                   [Lp.r, MX.r], [psB.r])
                            pA4 = psA.t[:].rearrange("p (h s n) -> p h s n", h=4, s=2)
                            act(MX4[:, :, 0, :], pA4[:, :, 0, :], AF.Copy, [psA.r], [MX.r])
                            if p_ > 1:
                                tt("dve", MX4[:, :, 1, :], pA4[:, :, 1, :], MX4[:, :, 1, :], ALU.add, [psA.r, MX.r], [MX.r])
                            act(Lp.t[:, 0:512], psB.t[:, 0:512], AF.Copy, [psB.r], [Lp.r])
                            pp.put(psA, psB)
                        else:
                            psA = pp.get()
                            for j in range(4):
                                mm(psA.t[:, j * P:(j + 1) * P], Lp.t[:, j * P:(j + 1) * P], MX4[:, j, 1, :], True, True,
                                   [Lp.r, MX.r], [psA.r])
                            tt("dve", MX4[:, :, 1, :], psA.t[:, 0:512].rearrange("p (h n) -> p h n", n=P), MX4[:, :, 1, :],
                               ALU.add, [psA.r, MX.r], [MX.r])
                            pp.put(psA)
                        p_ *= 2
                    psP = pp.get(); psU = pp.get()
                    for j in range(4):
                        hd = hg * 4 + j
                        mm(psP.t[0:64, j * P:(j + 1) * P], At.t[:, hd * 64:(hd + 1) * 64], MX4[:, j, 1, :], True, True,
                           [At.r, MX.r], [psP.r])
                        mm(psU.t[:, j * 64:(j + 1) * 64], MX4[:, j, 1, :], QU.t[:, j * 64:(j + 1) * 64], True, True,
                           [MX.r, QU.r], [psU.r])
                    P1g = Aak
                    act(P1g.t[0:64, 0:512], psP.t[0:64, 0:512], AF.Copy, [psP.r], [P1g.r])
                    act(QU.t[:, 256:512], psU.t[:, 0:256], AF.Copy, [psU.r], [QU.r])
                    pp.put(psP, psU)
                    pool.put(MX, Lp)
                    psS = pp.get()
                    for j in range(4):
                        hd = hg * 4 + j
                        mm(psS.t[:, j * 64:(j + 1) * 64], P1g.t[0:64, j * P:(j + 1) * P], STt[:, hd, :], True, True,
                           [P1g.r, r_ST], [psS.r])
                    tt("dve", QU.t[:, 512:768], psS.t[:, 0:256], QU.t[:, 256:512], ALU.add, [psS.r, QU.r], [QU.r])
                    pp.put(psS)
                    psY = pp.get(); psN = pp.get()
                    for j in range(4):
                        hd = hg * 4 + j
                        hs = slice(hd * 64, (hd + 1) * 64)
                        sa = QU.t[:, 512 + j * 64:512 + (j + 1) * 64]
                        mm(psY.t[:, j * 64:(j + 1) * 64], FTa.t[0:64, j * 256 + 128:(j + 1) * 256], STt[:, hd, :], True, False,
                           [FTa.r, r_ST], [psY.r])
                        mm(psY.t[:, j * 64:(j + 1) * 64], AR4[:, j, 0, :], sa, False, False, [AR.r, QU.r], [psY.r])
                        mm(psY.t[:, j * 64:(j + 1) * 64], AR4[:, j, 1, :], pv.t[:, hs], False, True, [AR.r, pv.r], [psY.r])
                        mm(psN.t[0:64, j * 64:(j + 1) * 64], Bt.t[:, hs], sa, True, False, [Bt.r, QU.r], [psN.r])
                        mm(psN.t[0:64, j * 64:(j + 1) * 64], Kt.t[:, hs], pv.t[:, hs], False, True, [Kt.r, pv.r], [psN.r])
                    act(yrec.t[:, hg * 256:(hg + 1) * 256], psY.t[:, 0:256], AF.Copy, [psY.r], [yrec.r]); pp.put(psY)
                    STg = STt[:, hg * 4:(hg + 1) * 4, :]
                    tt("dve", STg, psN.t[0:64, 0:256].rearrange("p (h v) -> p h v", v=64), STg, ALU.add,
                       [psN.r, r_ST], [r_ST])
                    tt("dve", STg, STg, WC[:, hg * 4:(hg + 1) * 4].unsqueeze(2).to_broadcast([64, 4, 64]), ALU.mult,
                       [r_ST, r_WC], [r_ST])
                    pp.put(psN)
                    pool.put(FTa, FTb, Aak, AR, QU)
                pool.put(Rt, At, Bt, Kt)
                dump("yrec", yrec.t[:], [P, D], [yrec.r], t * P, P, S_LEN)
                yc = yrec; sq = pool.get()
                red(st[:, 8:24], h3(yc.t[:]), [yc.r], [r_st])
                ts("dve", st[:, 8:24], st[:, 8:24], -1.0 / 64, None, ALU.mult, None, [r_st], [r_st])
                tt("dve", h3(yc.t[:]), h3(yc.t[:]), bch(st[:, 8:24]), ALU.add, [yc.r, r_st], [yc.r])
                tt("pool", sq.t[:], yc.t[:], yc.t[:], ALU.mult, [yc.r], [sq.r])
                red(st[:, 8:24], h3(sq.t[:]), [sq.r], [r_st])
                pool.put(sq)
                rstd_of(st[:, 8:24], 1.0 / 64, 64e-5, [r_st])
                tt("dve", h3(yc.t[:]), h3(yc.t[:]), bch(st[:, 8:24]), ALU.mult, [yc.r, r_st], [yc.r])
                lwb = bvec(vec6_d[3:4, :])
                tt("pool", yc.t[:], yc.t[:], lwb.t[:], ALU.mult, [yc.r, lwb.r], [yc.r])
                pool.put(lwb)
                lbb = bvec(vec6_d[4:5, :])
                tt("pool", yc.t[:], yc.t[:], lbb.t[:], ALU.add, [yc.r, lbb.r], [yc.r])
                pool.put(lbb)
                tt("pool", yc.t[:], yc.t[:], bonus.t[:], ALU.add, [yc.r, bonus.r], [yc.r])
                tt("dve", yc.t[:], yc.t[:], g_sb.t[:], ALU.mult, [yc.r, g_sb.r], [yc.r])
                pool.put(bonus, g_sb, pv)
                dump("ya", yc.t[:], [P, D], [yc.r], t * P, P, S_LEN)
                for (src_t, c0) in ((yc, 1280), (yb, 2304)):
                    ps = pp.get(); proj_cols(b, "r", c0, 1024, ps)
                    gsb = pool.get()
                    act(gsb.t[:], ps.t[:], AF.Sigmoid, [ps.r], [gsb.r]); pp.put(ps)
                    tt("dve", src_t.t[:], src_t.t[:], gsb.t[:], ALU.mult, [src_t.r, gsb.r], [src_t.r])
                    pool.put(gsb)
                tt("pool", yc.t[:], yc.t[:], yb.t[:], ALU.add, [yc.r, yb.r], [yc.r])
                pool.put(yb)
                dump("mixed", yc.t[:], [P, D], [yc.r], t * P, P, S_LEN)
                mT = pool.get()
                ps = pp.get()
                for kc in range(8):
                    tr(ps.t[:, kc * P:(kc + 1) * P], yc.t[:, kc * P:(kc + 1) * P], [yc.r], [ps.r])
                act(mT.t[:], ps.t[:], AF.Copy, [ps.r], [mT.r]); pp.put(ps)
                pool.put(yc)
                ps = pp.get()
                for g0 in range(0, 1024, 256):
                    s1 = wslot()
                    dma("sp", wk[s1][:], wov[:, :, g0:g0 + 256], [], [r_wk[s1]])
                    for kc in range(8):
                        mm(ps.t[:, g0:g0 + 256], mT.t[:, kc * P:(kc + 1) * P], wk[s1][:, kc, :], kc == 0, kc == 7,
                           [mT.r, r_wk[s1]], [ps.r])
                x1 = pool.get()
                gt1 = bvec(ada_row(2))
                tt("dve", x1.t[:], ps.t[:], gt1.t[:], ALU.mult, [ps.r, gt1.r], [x1.r]); pp.put(ps)
                pool.put(gt1)
                tt("pool", x1.t[:], x1.t[:], xt[b][:], ALU.add, [x1.r, r_xt[b]], [x1.r])
                pool.put(mT)
                dump("x1", x1.t[:], [P, D], [x1.r], t * P, P, S_LEN)
                drain()
                if 2 in phases:
                    gen[0] = peer_tile(t, x1)
                else:
                    ev = dma("sp", out_d[t * P:(t + 1) * P, :], x1.t[:], [x1.r], [])
                    out_evs.append(ev)
                    pool.put(x1)
            drain()
            op = base_op
            print("pool peak", pool.peak, "psum peak", pp.peak, "gs peak", gs.peak, "ops", S.n_ops)
            S.barrier()
            S.emit()

        S.final_wait("sp", out_evs)
    S.emit()
    return nc


def pack_inputs(x, c, p):
    f = np.float32
    A = lambda a: np.ascontiguousarray(np.asarray(a, f))
    d = {
        "x": A(x), "cT": A(np.asarray(c, f).reshape(8, 128).T),
        "ada_w": A(p["ada_w"]), "ada_b": A(p["ada_b"]).reshape(1, -1),
        "norm1_w": A(p["norm1_w"]).reshape(1, -1), "norm2_w": A(p["norm2_w"]).reshape(1, -1),
        "w_in": A(p["w_in"]), "shift_mu": A(p["shift_mu"]).reshape(1, -1),
        "w0a0": A(np.concatenate([np.asarray(p["w0"], f), np.asarray(p["a0"], f)])).reshape(1, -1),
        "wa_up": A(np.concatenate([np.asarray(p["w_lora_up"], f), np.asarray(p["a_lora_up"], f)], 0)),
        "g_up": A(p["g_lora_up"]),
        "vec6": A(np.stack([np.asarray(p[k], f).reshape(-1) for k in ("k_k", "k_a", "r_k", "lnx_w", "lnx_b", "lnx_b")])),
        "qk_nw": A(np.concatenate([np.asarray(p["q_norm_w"], f), np.asarray(p["k_norm_w"], f)])).reshape(1, -1),
        "sinks": A(p["sinks"]).reshape(1, -1),
        "w_out": A(p["w_out"]),
    }
    if "peer_w_q" in p:
        d["peer_w_q"] = A(p["peer_w_q"])
        d["keysT"] = A(np.concatenate([np.asarray(p["peer_keys_1"], f).T, np.asarray(p["peer_keys_2"], f).T], 1))
        d["peer_u"] = A(p["peer_u"])
        d["peer_v"] = A(p["peer_v"])
    return d


def kernel(**inputs):
    x = np.asarray(inputs["x"], np.float32)
    c = np.asarray(inputs["c"], np.float32)
    B, S_LEN, _ = x.shape
    p = {k: np.asarray(v, np.float32)[0] for k, v in inputs.items() if k not in ("x", "c")}
    nc = build(S_LEN)
    in_maps = [pack_inputs(x[b], c[b], p) for b in range(B)]
    res = run_bass_kernel_spmd(nc, in_maps, core_ids=list(range(B)))
    return np.stack([np.asarray(r["out"], np.float32) for r in res.results], 0)
```

```python
import numpy as np
import os
STOP = int(os.environ.get('STOP', '99'))
from contextlib import ExitStack
import concourse.bass as bass
import concourse.mybir as mybir
from concourse.bass_utils import run_bass_kernel_spmd

F32 = mybir.dt.float32
U32 = mybir.dt.uint32
I32 = mybir.dt.int32
ALU = mybir.AluOpType
AF = mybir.ActivationFunctionType
AX = mybir.AxisListType

D = 1024
NSH = 3328
INW = 6656
P = 128

ENG_EPOCH = int(os.environ.get('ENG_EPOCH', '30000'))
LANE_EPOCH = int(os.environ.get('LANE_EPOCH', '1900'))
N_LANES = 16


class Res:
    __slots__ = ("name", "w", "rd", "excl")

    def __init__(self, name="", excl=False):
        self.name = name
        self.w = None
        self.rd = {}
        self.excl = excl


def RL(n, name=""):
    return [Res(f"{name}{i}") for i in range(n)]


class Sched:
    def __init__(self, nc):
        self.nc = nc
        self.streams = {k: [] for k in ("pe", "act", "dve", "pool", "sp")}
        self.sems = {}
        self.ecount = {k: 0 for k in self.streams}
        self.eepoch = {k: 0 for k in self.streams}
        self.known = {k: {} for k in self.streams}
        self.lanes = {k: [[0, 0] for _ in range(N_LANES)] for k in self.streams}
        self.lane_i = {k: 0 for k in self.streams}
        self.n_ops = 0

    def op(self, eng, fn, reads=(), writes=(), dma=False):
        self.n_ops += 1
        deps = {}

        def add(ev):
            if ev is None:
                return
            k, v = ev
            if deps.get(k, 0) < v:
                deps[k] = v

        xr = [r for r in reads if r.excl]
        if xr:
            reads = [r for r in reads if not r.excl]
            writes = list(writes) + xr
        for r in reads:
            add(r.w)
        for w in writes:
            add(w.w)
            for k, v in w.rd.items():
                add((k, v))
        if dma:
            li = self.lane_i[eng]
            self.lane_i[eng] = (li + 1) % N_LANES
            lane = self.lanes[eng][li]
            if lane[1] >= LANE_EPOCH:
                add((("lane", eng, li, lane[0]), lane[1] * 16))
                lane[0] += 1
                lane[1] = 0
            key = ("lane", eng, li, lane[0])
            if lane[1] > 0:
                add((key, lane[1] * 16))
            lane[1] += 1
            ev = (key, lane[1] * 16)
            inc = 16
        else:
            if self.ecount[eng] >= ENG_EPOCH:
                self.eepoch[eng] += 1
                self.ecount[eng] = 0
            key = ("eng", eng, self.eepoch[eng])
            self.ecount[eng] += 1
            ev = (key, self.ecount[eng])
            inc = 1
        waits = []
        kn = self.known[eng]
        for k, v in deps.items():
            if eng == "pe" and k[0] == "eng" and k[1] == "pe":
                continue
            if kn.get(k, 0) >= v:
                continue
            kn[k] = v
            waits.append((k, v))
        self.streams[eng].append((waits, fn, ev[0], inc))
        for w in writes:
            w.w = ev
            w.rd = {}
        for r in reads:
            if r.rd.get(ev[0], 0) < ev[1]:
                r.rd[ev[0]] = ev[1]
        return ev

    def barrier(self):
        evs = []
        for e in self.streams:
            if self.ecount[e] > 0:
                evs.append((("eng", e, self.eepoch[e]), self.ecount[e]))
        for le in self.lanes:
            for li, lane in enumerate(self.lanes[le]):
                if lane[1] > 0:
                    evs.append((("lane", le, li, lane[0]), lane[1] * 16))
        for e in self.streams:
            waits = []
            for k, v in evs:
                if k[0] == "eng" and k[1] == e:
                    continue
                if self.known[e].get(k, 0) >= v:
                    continue
                self.known[e][k] = v
                waits.append((k, v))
            self.streams[e].append((waits, None, None, 0))

    def final_wait(self, eng, evs):
        self.streams[eng].append((list(evs), None, None, 0))

    def emit(self):
        nc = self.nc
        for eng, st in self.streams.items():
            for waits, fn, key, inc in st:
                for k, v in waits:
                    if k not in self.sems:
                        self.sems[k] = nc.alloc_semaphore("s%d" % len(self.sems))
                if key is not None and key not in self.sems:
                    self.sems[key] = nc.alloc_semaphore("s%d" % len(self.sems))
        streams = self.streams
        self.streams = {k: [] for k in streams}
        with nc.Block() as block:
            def mk(engname):
                def body(e):
                    for waits, fn, key, inc in streams[engname]:
                        for k, v in waits:
                            e.wait_ge(self.sems[k], v)
                        if fn is not None:
                            fn(e).then_inc(self.sems[key], inc)
                return body
            block.tensor(mk("pe"))
            block.scalar(mk("act"))
            block.vector(mk("dve"))
            block.gpsimd(mk("pool"))
            block.sync(mk("sp"))


class Ctx:
    def __init__(self, nc):
        self.nc = nc
        self.S = Sched(nc)
        self.n = 0

    def sb(self, es, shape, dt=F32, name=None):
        self.n += 1
        return es.enter_context(self.nc.sbuf_tensor(name or f"t{self.n}", list(shape), dt))

    def ps(self, es, shape, dt=F32, name=None):
        self.n += 1
        return es.enter_context(self.nc.psum_tensor(name or f"p{self.n}", list(shape), dt))


from collections import deque
STG = int(os.environ.get('STG', '9'))

C0 = float(np.exp(-0.5))


class TL:
    __slots__ = ("t", "r")

    def __init__(self, t, r):
        self.t = t
        self.r = r


class FPool:
    def __init__(self, items):
        self.q = deque(items)

    def get(self):
        self.out = getattr(self, "out", 0) + 1
        self.peak = max(getattr(self, "peak", 0), self.out)
        return self.q.popleft()

    def put(self, *items):
        for it in items:
            self.out -= 1
            self.q.append(it)


def build(S_LEN, dbg=None, phases=(1, 2)):
    nc = bass.Bass("TRN2", target_bir_lowering=False)
    NT = S_LEN // P
    C = Ctx(nc)
    S = C.S
    op = S.op

    def din(name, shape, dt=F32):
        return nc.dram_tensor(name, list(shape), dt, kind="ExternalInput").ap()

    x_d = din("x", [S_LEN, D])
    cT_d = din("cT", [P, 8])
    ada_w_d = din("ada_w", [D, 6 * D])
    ada_b_d = din("ada_b", [1, 6 * D])
    n1w_d = din("norm1_w", [1, D])
    n2w_d = din("norm2_w", [1, D])
    w_in_d = din("w_in", [D, INW])
    mu_d = din("shift_mu", [1, NSH])
    w0a0_d = din("w0a0", [1, 2 * D])
    wa_up_d = din("wa_up", [P, D])
    g_up_d = din("g_up", [P, D])
    vec6_d = din("vec6", [6, D])
    qk_nw_d = din("qk_nw", [1, 128])
    sinks_d = din("sinks", [1, 16])
    w_out_d = din("w_out", [D, D])
    wq_d = din("peer_w_q", [D, 2048])
    keysT_d = din("keysT", [P, 256])
    pu_d = din("peer_u", [16384, D])
    pv_d = din("peer_v", [16384, D])
    out_d = nc.dram_tensor("out", [S_LEN, D], F32, kind="ExternalOutput").ap()
    dbg_d = {}

    def dump(name, ap_sb, shape, reads, row0=None, nrows=None, total_rows=None):
        if not dbg or name not in dbg:
            return
        if name not in dbg_d:
            dbg_d[name] = nc.dram_tensor("dbg_" + name, [total_rows or shape[0]] + list(shape[1:]), F32,
                                         kind="ExternalOutput").ap()
        dst = dbg_d[name] if row0 is None else dbg_d[name][row0:row0 + nrows]
        ev = op("sp", lambda e: e.dma_start(out=dst, in_=ap_sb), reads, [], dma=True)
        out_evs.append(ev)

    ada_s = nc.dram_tensor("ada_s", [1, 6 * D], F32, kind="Internal").ap()

    out_evs = []
    r_w1s = Res(); r_w2s = Res(); r_x1 = RL(NT)

    def tt(eng, out, in0, in1, opc, R, W):
        return op(eng, lambda e: e.tensor_tensor(out=out, in0=in0, in1=in1, op=opc), R, W)

    def ts(eng, out, in0, s1, s2, o0, o1, R, W):
        if o1 is None:
            return op(eng, lambda e: e.tensor_scalar(out=out, in0=in0, scalar1=s1, scalar2=None, op0=o0), R, W)
        return op(eng, lambda e: e.tensor_scalar(out=out, in0=in0, scalar1=s1, scalar2=s2, op0=o0, op1=o1), R, W)

    def stt(out, in0, sc, in1, o0, o1, R, W):
        return op("dve", lambda e: e.scalar_tensor_tensor(out=out, in0=in0, scalar=sc, in1=in1, op0=o0, op1=o1), R, W)

    def act(out, in_, func, R, W, scale=None, bias=None, accum=None):
        kw = {}
        if scale is not None:
            kw["scale"] = scale
        if bias is not None:
            kw["bias"] = bias
        if accum is not None:
            kw["accum_out"] = accum
        return op("act", lambda e: e.activation(out=out, in_=in_, func=func, **kw), R, W)

    def mm(out, lhsT, rhs, start, stop, R, W):
        return op("pe", lambda e: e.matmul(out, lhsT, rhs, start=start, stop=stop), R, W)

    def tr(out, in_, R, W):
        return op("pe", lambda e: e.transpose(out, in_, ident[0:in_.shape[0], 0:in_.shape[0]]), list(R) + [r_ident], W)

    def red(out, in_, R, W, opc=ALU.add):
        return op("dve", lambda e: e.tensor_reduce(out=out, in_=in_, axis=AX.X, op=opc), R, W)

    def dma(eng, out, in_, R, W):
        return op(eng, lambda e: e.dma_start(out=out, in_=in_), R, W, dma=True)

    def h3(ap, k=64):
        return ap.rearrange("p (h k) -> p h k", k=k)

    def bch(ap16, n=16, k=64):
        return ap16.unsqueeze(2).to_broadcast([ap16.shape[0], n, k])

    with ExitStack() as es0:
        ident = C.sb(es0, [P, P]); r_ident = Res("ident")
        ones_row = C.sb(es0, [1, P]); r_ones = Res("ones")
        op("pool", lambda e: e.memset(ident[:], 0.0), [], [r_ident])
        op("pool", lambda e: e.affine_select(out=ident[:], in_=ident[:], pattern=[[-1, P]],
                                            compare_op=ALU.not_equal, fill=1.0, base=0,
                                            channel_multiplier=1), [r_ident], [r_ident])
        op("pool", lambda e: e.memset(ones_row[:], 1.0), [], [r_ones])

        with ExitStack() as es:
            cT = C.sb(es, [P, 8]); r_cT = Res()
            cond = C.sb(es, [P, 8]); r_cond = Res()
            arow = C.sb(es, [1, 6 * D]); r_arow = Res()
            brow = C.sb(es, [1, 6 * D]); r_brow = Res()
            nrow = C.sb(es, [1, 2 * D]); r_nrow = Res()
            wb = [C.sb(es, [P, 8, 512]) for _ in range(2)]; r_wb = RL(2)
            pa = [C.ps(es, [1, 512]) for _ in range(2)]; r_pa = RL(2)
            dma("sp", cT[:], cT_d, [], [r_cT])
            dma("sp", brow[:], ada_b_d, [], [r_brow])
            dma("sp", nrow[:, 0:D], n1w_d, [], [r_nrow])
            dma("sp", nrow[:, D:2 * D], n2w_d, [], [r_nrow])
            act(cond[:], cT[:], AF.Silu, [r_cT], [r_cond])
            aw = ada_w_d.rearrange("(kc p) n -> p kc n", p=P)
            for g in range(12):
                b = g % 2
                dma("sp", wb[b][:], aw[:, :, g * 512:(g + 1) * 512], [], [r_wb[b]])
                for kc in range(8):
                    mm(pa[b][:], cond[:, kc:kc + 1], wb[b][:, kc, :], kc == 0, kc == 7, [r_cond, r_wb[b]], [r_pa[b]])
                tt("dve", arow[:, g * 512:(g + 1) * 512], pa[b][:], brow[:, g * 512:(g + 1) * 512], ALU.add,
                   [r_pa[b], r_brow], [r_arow])
            for (slot, off) in ((1, 0), (4, D)):
                stt(arow[:, slot * D:(slot + 1) * D], arow[:, slot * D:(slot + 1) * D], 1.0, nrow[:, off:off + D],
                    ALU.add, ALU.mult, [r_arow, r_nrow], [r_arow])
            r_ada_s = Res()
            dma("sp", ada_s, arow[:], [r_arow], [r_ada_s])
            S.barrier()
            S.emit()

        if 1 in phases:
          with ExitStack() as es:
            wa_up = C.sb(es, [P, D]); r_waup = Res()
            g_up = C.sb(es, [P, D]); r_gup = Res()
            dma("sp", wa_up[:], wa_up_d, [], [r_waup])
            dma("sp", g_up[:], g_up_d, [], [r_gup])
            tri = C.sb(es, [P, 3, P]); r_tri = Res()
            op("pool", lambda e: e.memset(tri[:], 1.0), [], [r_tri])
            for i, (pat, cm, cop) in enumerate((([[1, P]], -1, ALU.is_ge), ([[1, P]], -1, ALU.is_gt),
                                                ([[-1, P]], 1, ALU.is_gt))):
                op("pool", lambda e, i=i, pat=pat, cm=cm, cop=cop: e.affine_select(
                    out=tri[:, i, :], in_=tri[:, i, :], pattern=pat, compare_op=cop, fill=0.0, base=0,
                    channel_multiplier=cm), [r_tri], [r_tri])
            TRI, TRIS, TRIL = tri[:, 0, :], tri[:, 1, :], tri[:, 2, :]
            shm = C.sb(es, [P, 2, P]); r_shm = Res()
            op("pool", lambda e: e.memset(shm[:], 1.0), [], [r_shm])
            op("pool", lambda e: e.affine_select(out=shm[:, 0, :], in_=shm[:, 0, :], pattern=[[1, P]],
                                                compare_op=ALU.is_equal, fill=0.0, base=-1, channel_multiplier=-1),
               [r_shm], [r_shm])
            op("pool", lambda e: e.memset(shm[:, 1, :], 0.0), [r_shm], [r_shm])
            op("pool", lambda e: e.tensor_copy(out=shm[:, 1, 0:1], in_=ident[:, P - 1:P]), [r_shm, r_ident], [r_shm])
            SHM, EMM = shm[:, 0, :], shm[:, 1, :]
            qkw = C.sb(es, [P, 128]); r_qkw = Res()
            dma("sp", qkw[:], qk_nw_d.partition_broadcast(P), [], [r_qkw])
            esk = C.sb(es, [P, 16]); r_esk = Res()
            dma("sp", esk[:], sinks_d.partition_broadcast(P), [], [r_esk])
            act(esk[:], esk[:], AF.Exp, [r_esk], [r_esk])
            keysT = C.sb(es, [P, 256]); r_keys = Res()
            dma("sp", keysT[:], keysT_d, [], [r_keys])
            iot = C.sb(es, [P, 16]); r_iot = Res()
            op("pool", lambda e: e.iota(iot[:], pattern=[[1, 16]], base=0, channel_multiplier=0,
                                        allow_small_or_imprecise_dtypes=True), [], [r_iot])

            xt = [C.sb(es, [P, D]) for _ in range(2)]; r_xt = RL(2)
            st = C.sb(es, [P, 64]); r_st = Res()
            st2 = C.sb(es, [P, 32]); r_st2 = Res()
            hT = [C.sb(es, [P, 8, P + 1]) for _ in range(2)]; r_hT = RL(2)
            NWK = 2
            wk = [C.sb(es, [P, 8, 512]) for _ in range(NWK)]; r_wk = RL(NWK)
            kT = [C.sb(es, [64, 2, P]) for _ in range(2)]; r_kT = RL(2)
            vv = [C.sb(es, [P, 2, 65]) for _ in range(2)]; r_vv = RL(2)
            STt = C.sb(es, [64, 16, 64]); r_ST = Res()
            WC = C.sb(es, [64, 16]); r_WC = Res()
            op("pool", lambda e: e.memset(STt[:], 0.0), [], [r_ST])
            for b_ in range(2):
                op("pool", lambda e, b_=b_: e.memset(vv[b_][:], 1.0), [], [r_vv[b_]])
            tmpk = C.sb(es, [P, 256]); r_tmpk = Res()
            V12 = C.sb(es, [P, 16, 16]); r_V12 = Res()
            I12 = C.sb(es, [P, 16, 16], U32); r_I12 = Res()
            I12f = C.sb(es, [P, 16, 16]); r_I12f = Res()
            scv = C.sb(es, [P, 8, 16]); r_scv = Res()
            posu = C.sb(es, [P, 8, 16], U32); r_posu = Res()
            pa_u = C.sb(es, [P, 2, 128], U32); r_pau = Res()
            pa_f = C.sb(es, [P, 2, 128]); r_paf = Res()
            isel = C.sb(es, [P, 2, 128]); r_isel = Res()
            idxf = C.sb(es, [P, 128]); r_idxf = Res()
            idxu = C.sb(es, [P, 128], I32); r_idxu = Res()
            gate = C.sb(es, [P, 8, 16]); r_gate = Res()
            acts = C.sb(es, [P, 128]); r_acts = Res()
            wts = C.sb(es, [P, 128]); r_wts = Res()
            NG = int(os.environ.get("NG", "8"))
            gs = FPool([TL(C.sb(es, [P, D]), Res(f"g{i}")) for i in range(NG)])
            NPOOL = int(os.environ.get("NPOOL", "23"))
            pool = FPool([TL(C.sb(es, [P, D]), Res(f"pl{i}")) for i in range(NPOOL)])
            pp = FPool([TL(C.ps(es, [P, 1024]), Res(f"ps{i}", excl=True)) for i in range(4)])
            wiv = w_in_d.rearrange("(kc p) n -> p kc n", p=P)
            wov = w_out_d.rearrange("(kc p) n -> p kc n", p=P)
            wqv = wq_d.rearrange("(kc p) n -> p kc n", p=P)
            wki = [0]
            op("pool", lambda e: e.memset(hT[1][:, :, P:P + 1], 0.0), [], [r_hT[1]])

            def wslot():
                s_ = wki[0] % NWK
                wki[0] += 1
                return s_

            def bvec(src_ap, width=D):
                tl = pool.get()
                dma("sp", tl.t[:, 0:width], src_ap.partition_broadcast(P), [r_ada_s], [tl.r])
                return tl

            def ada_row(slot):
                return ada_s[:, slot * D:(slot + 1) * D]

            def proj_cols(b, kind, c0, width, ps):
                off = 0 if kind == "s" else NSH
                for g0 in range(0, width, 512):
                    wd = min(512, width - g0)
                    cc = off + c0 + g0
                    s1 = wslot()
                    dma("sp", wk[s1][:, :, 0:wd], wiv[:, :, cc:cc + wd], [], [r_wk[s1]])
                    for kc in range(8):
                        mm(ps.t[:, g0:g0 + wd], hT[b][:, kc, 1:P + 1], wk[s1][:, kc, 0:wd], kc == 0, kc == 7,
                           [r_hT[b], r_wk[s1]], [ps.r])

            prev_raw = {}

            def shifted_proj(b, t, c0, width):
                ps = pp.get(); proj_cols(b, "s", c0, width, ps)
                raw = pool.get()
                act(raw.t[:, 0:width], ps.t[:, 0:width], AF.Copy, [ps.r], [raw.r]); pp.put(ps)
                ps2 = pp.get()
                prv = prev_raw.get(c0)
                for g0 in range(0, width, 512):
                    wd = min(512, width - g0)
                    mm(ps2.t[:, g0:g0 + wd], SHM, raw.t[:, g0:g0 + wd], True, prv is None, [r_shm, raw.r], [ps2.r])
                    if prv is not None:
                        mm(ps2.t[:, g0:g0 + wd], EMM, prv.t[:, g0:g0 + wd], False, True, [r_shm, prv.r], [ps2.r])
                d = pool.get()
                tt("dve", d.t[:, 0:width], ps2.t[:, 0:width], raw.t[:, 0:width], ALU.subtract, [ps2.r, raw.r], [d.r])
                pp.put(ps2)
                if prv is not None:
                    pool.put(prv)
                mub = bvec(mu_d[:, c0:c0 + width], width)
                tt("pool", d.t[:, 0:width], d.t[:, 0:width], mub.t[:, 0:width], ALU.mult, [d.r, mub.r], [d.r])
                pool.put(mub)
                tt("pool", d.t[:, 0:width], d.t[:, 0:width], raw.t[:, 0:width], ALU.add, [d.r, raw.r], [d.r])
                prev_raw[c0] = raw
                return d

            def rstd_of(ss_ap, scale, eps, R):
                ts("dve", ss_ap, ss_ap, scale, eps, ALU.mult, ALU.add, R, R)
                act(ss_ap, ss_ap, AF.Sqrt, R, R)
                op("dve", lambda e: e.reciprocal(out=ss_ap, in_=ss_ap), R, R)

            def top16(src, dstv, dsti, n, R_src):
                op("dve", lambda e: e.max(out=dstv[:, 0:8], in_=src), R_src, [r_V12])
                op("dve", lambda e: e.match_replace(out=tmpk[:, 0:n], in_to_replace=dstv[:, 0:8], in_values=src,
                                                    imm_value=-1e30), R_src + [r_V12], [r_tmpk])
                op("dve", lambda e: e.max(out=dstv[:, 8:16], in_=tmpk[:, 0:n]), [r_tmpk], [r_V12])
                op("dve", lambda e: e.max_index(out=dsti[:, 0:8], in_max=dstv[:, 0:8], in_values=src),
                   R_src + [r_V12], [r_I12])
                op("dve", lambda e: e.max_index(out=dsti[:, 8:16], in_max=dstv[:, 8:16], in_values=tmpk[:, 0:n]),
                   [r_tmpk, r_V12], [r_I12])

            def peer_tile(t, x1):
                h2 = pool.get()
                act(h2.t[:], x1.t[:], AF.Square, [x1.r], [h2.r, r_st2], accum=st2[:, 0:1])
                rstd_of(st2[:, 0:1], 1.0 / D, 1e-6, [r_st2])
                g2 = bvec(ada_row(4))
                stt(h2.t[:], x1.t[:], st2[:, 0:1], g2.t[:], ALU.mult, ALU.mult, [x1.r, r_st2, g2.r], [h2.r])
                pool.put(g2)
                sh2 = bvec(ada_row(3))
                tt("pool", h2.t[:], h2.t[:], sh2.t[:], ALU.add, [h2.r, sh2.r], [h2.r])
                pool.put(sh2)
                h2T = pool.get()
                ps = pp.get()
                for kc in range(8):
                    tr(ps.t[:, kc * P:(kc + 1) * P], h2.t[:, kc * P:(kc + 1) * P], [h2.r], [ps.r])
                act(h2T.t[:], ps.t[:], AF.Copy, [ps.r], [h2T.r]); pp.put(ps)
                qtm = [pool.get(), pool.get()]
                for g in range(4):
                    s1 = wslot()
                    dma("sp", wk[s1][:], wqv[:, :, g * 512:(g + 1) * 512], [], [r_wk[s1]])
                    if g % 2 == 0:
                        ps = pp.get()
                    for kc in range(8):
                        mm(ps.t[:, (g % 2) * 512:(g % 2 + 1) * 512], h2T.t[:, kc * P:(kc + 1) * P], wk[s1][:, kc, :],
                           kc == 0, kc == 7, [r_wk[s1], h2T.r], [ps.r])
                    if g % 2 == 1:
                        act(qtm[g // 2].t[:], ps.t[:], AF.Copy, [ps.r], [qtm[g // 2].r]); pp.put(ps)
                qT = [pool.get(), pool.get()]
                for half in range(2):
                    ps = pp.get()
                    for q_ in range(8):
                        tr(ps.t[:, q_ * P:(q_ + 1) * P], qtm[half].t[:, q_ * P:(q_ + 1) * P], [qtm[half].r], [ps.r])
                    act(qT[half].t[:], ps.t[:], AF.Copy, [ps.r], [qT[half].r]); pp.put(ps)
                pool.put(*qtm)
                pool.put(h2T)
                yield
                s12 = [pool.get(), pool.get()]
                for half in range(2):
                    ps = pp.get()
                    for q_ in range(8):
                        j = q_ % 2
                        mm(ps.t[:, q_ * P:(q_ + 1) * P], qT[half].t[:, q_ * P:(q_ + 1) * P], keysT[:, j * P:(j + 1) * P],
                           True, True, [qT[half].r, r_keys], [ps.r])
                    act(s12[half].t[:], ps.t[:], AF.Copy, [ps.r], [s12[half].r]); pp.put(ps)
                pool.put(*qT)
                for hj in range(16):
                    top16(s12[hj // 8].t[:, (hj % 8) * P:(hj % 8 + 1) * P], V12[:, hj, :], I12[:, hj, :], 128,
                          [s12[hj // 8].r])
                    yield
                pool.put(*s12)
                cand = [pool.get(), pool.get()]
                V4 = V12[:].rearrange("p (h j) k -> p h j k", j=2)
                for half in range(2):
                    tt("dve", cand[half].t[:].rearrange("p (h a b) -> p h a b", a=16, b=16),
                       V4[:, half * 4:(half + 1) * 4, 0, :].unsqueeze(3).to_broadcast([P, 4, 16, 16]),
                       V4[:, half * 4:(half + 1) * 4, 1, :].unsqueeze(2).to_broadcast([P, 4, 16, 16]), ALU.add,
                       [r_V12], [cand[half].r])
                op("dve", lambda e: e.tensor_copy(out=I12f[:], in_=I12[:]), [r_I12], [r_I12f])
                yield
                for h in range(8):
                    top16(cand[h // 4].t[:, (h % 4) * 256:(h % 4 + 1) * 256], scv[:, h, :], posu[:, h, :], 256,
                          [cand[h // 4].r])
                    yield
                pool.put(*cand)
                posf2 = posu[:].rearrange("p h k -> p (h k)")
                op("dve", lambda e: e.tensor_single_scalar(out=pa_u[:, 0, :], in_=posf2, scalar=4,
                                                           op=ALU.logical_shift_right), [r_I12], [r_pau])
                op("dve", lambda e: e.tensor_single_scalar(out=pa_u[:, 1, :], in_=posf2, scalar=15,
                                                           op=ALU.bitwise_and), [r_I12], [r_pau])
                op("dve", lambda e: e.tensor_copy(out=pa_f[:], in_=pa_u[:]), [r_pau], [r_paf])
                I4 = I12f[:].rearrange("p (h j) k -> p h j k", j=2)
                for j in range(2):
                    for half in range(2):
                        oh = pool.get()
                        oh4 = oh.t[:].rearrange("p (h k a) -> p h k a", k=16, a=16)
                        sel_f = pa_f[:, j, half * 64:(half + 1) * 64].rearrange("p (h k) -> p h k", k=16)
                        tt("dve", oh4, sel_f.unsqueeze(3).to_broadcast([P, 4, 16, 16]),
                           iot[:].unsqueeze(1).unsqueeze(1).to_broadcast([P, 4, 16, 16]), ALU.is_equal,
                           [r_paf, r_iot], [oh.r])
                        tt("dve", oh4, oh4, I4[:, half * 4:(half + 1) * 4, j, :].unsqueeze(2).to_broadcast([P, 4, 16, 16]),
                           ALU.mult, [oh.r, r_I12f], [oh.r])
                        red(isel[:, j, half * 64:(half + 1) * 64], oh.t[:].rearrange("p (hk a) -> p hk a", a=16),
                            [oh.r], [r_isel])
                        pool.put(oh)
                        yield
                stt(idxf[:], isel[:, 0, :], 128.0, isel[:, 1, :], ALU.mult, ALU.add, [r_isel], [r_idxf])
                op("dve", lambda e: e.tensor_copy(out=idxu[:], in_=idxf[:]), [r_idxf], [r_idxu])
                dump("idx", idxf[:], [P, 128], [r_idxf], t * P, P, S_LEN)
                tt("dve", gate[:], scv[:], scv[:, :, 0:1].to_broadcast([P, 8, 16]), ALU.subtract, [r_V12], [r_gate])
                act(gate[:], gate[:], AF.Exp, [r_gate], [r_gate])
                red(st2[:, 8:16], gate[:], [r_gate], [r_st2])
                op("dve", lambda e: e.reciprocal(out=st2[:, 8:16], in_=st2[:, 8:16]), [r_st2], [r_st2])
                tt("dve", gate[:], gate[:], st2[:, 8:16].unsqueeze(2).to_broadcast([P, 8, 16]), ALU.mult,
                   [r_gate, r_st2], [r_gate])
                yield
                for k in range(128):
                    g_ = gs.get()
                    op("pool", lambda e, g_=g_, k=k: e.indirect_dma_start(
                        out=g_.t[:, :], out_offset=None, in_=pu_d[:, :],
                        in_offset=bass.IndirectOffsetOnAxis(ap=idxu[:, k:k + 1], axis=0)),
                       [r_idxu], [g_.r], dma=True)
                    op("dve", lambda e, g_=g_, k=k, h2=h2: e.scalar_tensor_tensor(
                        out=g_.t[:], in0=g_.t[:], scalar=1.0, in1=h2.t[:], op0=ALU.mult, op1=ALU.mult,
                        accum_out=acts[:, k:k + 1]), [g_.r, h2.r], [g_.r, r_acts])
                    gs.put(g_)
                    yield
                pool.put(h2)
                act(wts[:], acts[:], AF.Gelu, [r_acts], [r_wts])
                tt("dve", wts[:], wts[:], gate[:].rearrange("p h k -> p (h k)"), ALU.mult, [r_wts, r_gate], [r_wts])
                acc = pool.get()
                for k in range(128):
                    g_ = gs.get()
                    op("pool", lambda e, g_=g_, k=k: e.indirect_dma_start(
                        out=g_.t[:, :], out_offset=None, in_=pv_d[:, :],
                        in_offset=bass.IndirectOffsetOnAxis(ap=idxu[:, k:k + 1], axis=0)),
                       [r_idxu], [g_.r], dma=True)
                    if k == 0:
                        ts("dve", acc.t[:], g_.t[:], wts[:, 0:1], None, ALU.mult, None, [g_.r, r_wts], [acc.r])
                    else:
                        stt(acc.t[:], g_.t[:], wts[:, k:k + 1], acc.t[:], ALU.mult, ALU.add, [g_.r, r_wts, acc.r], [acc.r])
                    gs.put(g_)
                    yield
                dump("peer", acc.t[:], [P, D], [acc.r], t * P, P, S_LEN)
                gt2 = bvec(ada_row(5))
                tt("dve", acc.t[:], acc.t[:], gt2.t[:], ALU.mult, [acc.r, gt2.r], [acc.r])
                pool.put(gt2)
                tt("pool", acc.t[:], acc.t[:], x1.t[:], ALU.add, [acc.r, x1.r], [acc.r])
                ev = dma("sp", out_d[t * P:(t + 1) * P, :], acc.t[:], [acc.r], [])
                out_evs.append(ev)
                pool.put(acc, x1)

            gen = [None]
            PUMP = int(os.environ.get("PUMP", "3"))
            base_op = S.op
            busy = [False]

            def pump(n):
                if gen[0] is None or busy[0]:
                    return
                busy[0] = True
                try:
                    for _ in range(n):
                        next(gen[0])
                except StopIteration:
                    gen[0] = None
                busy[0] = False

            def op_p(eng, fn, reads=(), writes=(), dma=False):
                ev = base_op(eng, fn, reads, writes, dma)
                if eng == "dve" and not busy[0]:
                    pump(PUMP)
                return ev

            def drain():
                while gen[0] is not None:
                    pump(64)

            for t in range(NT):
                b = t % 2
                op = op_p
                dma("sp", xt[b][:], x_d[t * P:(t + 1) * P, :], [], [r_xt[b]])
                hh = pool.get()
                act(hh.t[:], xt[b][:], AF.Square, [r_xt[b]], [hh.r, r_st], accum=st[:, 0:1])
                rstd_of(st[:, 0:1], 1.0 / D, 1e-6, [r_st])
                g1 = bvec(ada_row(1))
                stt(hh.t[:], xt[b][:], st[:, 0:1], g1.t[:], ALU.mult, ALU.mult, [r_xt[b], r_st, g1.r], [hh.r])
                pool.put(g1)
                sh1 = bvec(ada_row(0))
                tt("pool", hh.t[:], hh.t[:], sh1.t[:], ALU.add, [hh.r, sh1.r], [hh.r])
                pool.put(sh1)
                op("pool", lambda e, b=b: e.tensor_copy(out=hT[b][:, :, 0:1], in_=hT[1 - b][:, :, P:P + 1]),
                   [r_hT[1 - b]], [r_hT[b]])
                ps = pp.get()
                for kc in range(8):
                    tr(ps.t[:, kc * P:(kc + 1) * P], hh.t[:, kc * P:(kc + 1) * P], [hh.r], [ps.r])
                act(hT[b][:, :, 1:P + 1], ps.t[:].rearrange("p (q n) -> p q n", q=8), AF.Copy, [ps.r], [r_hT[b]])
                pp.put(ps); pool.put(hh)

                aq = pool.get(); akav = pool.get()
                ps = pp.get(); proj_cols(b, "r", 0, 1024, ps)
                act(aq.t[:], ps.t[:], AF.Copy, [ps.r], [aq.r]); pp.put(ps)
                ps = pp.get(); proj_cols(b, "r", 1024, 256, ps)
                act(akav.t[:, 0:256], ps.t[:, 0:256], AF.Copy, [ps.r], [akav.r]); pp.put(ps)
                sq = pool.get()
                tt("pool", sq.t[:], aq.t[:], aq.t[:], ALU.mult, [aq.r], [sq.r])
                red(st[:, 8:24], h3(sq.t[:]), [sq.r], [r_st])
                tt("pool", sq.t[:, 0:128], akav.t[:, 0:128], akav.t[:, 0:128], ALU.mult, [akav.r], [sq.r])
                red(st[:, 24:26], h3(sq.t[:, 0:128]), [sq.r], [r_st])
                pool.put(sq)
                rstd_of(st[:, 8:26], 1.0 / 64, 1e-6, [r_st])
                tt("dve", h3(aq.t[:]), h3(aq.t[:]), bch(st[:, 8:24]), ALU.mult, [aq.r, r_st], [aq.r])
                tt("pool", h3(aq.t[:]), h3(aq.t[:]), qkw[:, 0:64].unsqueeze(1).to_broadcast([P, 16, 64]), ALU.mult,
                   [aq.r, r_qkw], [aq.r])
                tt("dve", h3(akav.t[:, 0:128]), h3(akav.t[:, 0:128]), bch(st[:, 24:26], 2), ALU.mult,
                   [akav.r, r_st], [akav.r])
                tt("pool", h3(akav.t[:, 0:128]), h3(akav.t[:, 0:128]),
                   qkw[:, 64:128].unsqueeze(1).to_broadcast([P, 2, 64]), ALU.mult, [akav.r, r_qkw], [akav.r])
                op("pool", lambda e, b=b, akav=akav: e.tensor_copy(out=vv[b][:, :, 0:64], in_=h3(akav.t[:, 128:256])),
                   [akav.r], [r_vv[b]])
                qT = [pool.get(), pool.get()]
                for half in range(2):
                    ps = pp.get()
                    for j in range(8):
                        hd = half * 8 + j
                        tr(ps.t[0:64, j * P:(j + 1) * P], aq.t[:, hd * 64:(hd + 1) * 64], [aq.r], [ps.r])
                    act(qT[half].t[0:64, :], ps.t[0:64, :], AF.Copy, [ps.r], [qT[half].r]); pp.put(ps)
                ps = pp.get()
                for g in range(2):
                    tr(ps.t[0:64, g * P:(g + 1) * P], akav.t[:, g * 64:(g + 1) * 64], [akav.r], [ps.r])
                act(kT[b][:].rearrange("p g n -> p (g n)"), ps.t[0:64, 0:256], AF.Copy, [ps.r], [r_kT[b]]); pp.put(ps)
                pool.put(aq, akav)
                yb = pool.get()
                for g in range(2):
                    Ec = pool.get(); Ep = pool.get()
                    for (E, kb, mask) in ((Ec, b, TRI), (Ep, 1 - b, TRIL)):
                        if E is Ep and t == 0:
                            continue
                        ps = pp.get()
                        for hf in range(2):
                            mm(ps.t[:, hf * 512:(hf + 1) * 512], kT[kb][:, g, :], qT[g].t[0:64, hf * 512:(hf + 1) * 512],
                               True, True, [r_kT[kb], qT[g].r], [ps.r])
                        act(E.t[:], ps.t[:], AF.Exp, [ps.r], [E.r], scale=0.125); pp.put(ps)
                        tt("pool", E.t[:].rearrange("p (h n) -> p h n", n=P), E.t[:].rearrange("p (h n) -> p h n", n=P),
                           mask.unsqueeze(1).to_broadcast([P, 8, P]), ALU.mult, [E.r, r_tri], [E.r])
                    ps = pp.get()
                    for j in range(8):
                        mm(ps.t[:, j * 128:j * 128 + 65], Ec.t[:, j * P:(j + 1) * P], vv[b][:, g, :], True, t == 0,
                           [Ec.r, r_vv[b]], [ps.r])
                        if t > 0:
                            mm(ps.t[:, j * 128:j * 128 + 65], Ep.t[:, j * P:(j + 1) * P], vv[1 - b][:, g, :], False, True,
                               [Ep.r, r_vv[1 - b]], [ps.r])
                    o3 = ps.t[:].rearrange("p (h n) -> p h n", n=128)
                    tt("dve", st[:, 32:40], o3[:, :, 64], esk[:, g * 8:(g + 1) * 8], ALU.add, [ps.r, r_esk], [r_st])
                    op("dve", lambda e: e.reciprocal(out=st[:, 32:40], in_=st[:, 32:40]), [r_st], [r_st])
                    tt("dve", h3(yb.t[:, g * 512:(g + 1) * 512]), o3[:, :, 0:64], bch(st[:, 32:40], 8), ALU.mult,
                       [ps.r, r_st], [yb.r])
                    pp.put(ps); pool.put(Ec, Ep)
                pool.put(*qT)
                dump("yb", yb.t[:], [P, D], [yb.r], t * P, P, S_LEN)

                pr = shifted_proj(b, t, 0, 1024)
                pk = shifted_proj(b, t, 1024, 1024)
                pv = shifted_proj(b, t, 2048, 1024)
                lw = shifted_proj(b, t, 3072, 256)
                act(lw.t[:, 0:64], lw.t[:, 0:64], AF.Tanh, [lw.r], [lw.r])
                act(lw.t[:, 128:256], lw.t[:, 128:256], AF.Sigmoid, [lw.r], [lw.r])
                ps = pp.get()
                tr(ps.t[:, 0:P], lw.t[:, 0:128], [lw.r], [ps.r])
                tr(ps.t[:, P:2 * P], lw.t[:, 128:256], [lw.r], [ps.r])
                act(lw.t[:, 256:512], ps.t[:, 0:256], AF.Copy, [ps.r], [lw.r]); pp.put(ps)
                sgT = lw.t[:, 384:512]
                sw = pool.get(); a_ = pool.get(); g_sb = pool.get()
                for (dst_, r0, voff) in ((sw, 0, 0), (a_, 64, D)):
                    ps = pp.get()
                    for hf in range(2):
                        cs = slice(hf * 512, (hf + 1) * 512)
                        mm(ps.t[:, cs], lw.t[r0:r0 + 64, 256:384], wa_up[r0:r0 + 64, cs], True, True, [lw.r, r_waup], [ps.r])
                    bb_ = bvec(w0a0_d[:, voff:voff + D])
                    tt("dve", dst_.t[:], ps.t[:], bb_.t[:], ALU.add, [ps.r, bb_.r], [dst_.r]); pp.put(ps)
                    pool.put(bb_)
                    act(dst_.t[:], dst_.t[:], AF.Sigmoid, [dst_.r], [dst_.r])
                ps = pp.get()
                for hf in range(2):
                    cs = slice(hf * 512, (hf + 1) * 512)
                    mm(ps.t[:, cs], sgT, g_up[:, cs], True, True, [lw.r, r_gup], [ps.r])
                act(g_sb.t[:], ps.t[:], AF.Copy, [ps.r], [g_sb.r]); pp.put(ps)
                pool.put(lw)
                eW = pool.get(); eWi = pool.get(); eWx = pool.get()
                ps = pp.get()
                for hf in range(2):
                    cs = slice(hf * 512, (hf + 1) * 512)
                    mm(ps.t[:, cs], TRI, sw.t[:, cs], True, True, [r_tri, sw.r], [ps.r])
                act(eW.t[:], ps.t[:], AF.Exp, [ps.r], [eW.r], scale=-C0)
                act(eWi.t[:], ps.t[:], AF.Exp, [ps.r], [eWi.r], scale=C0); pp.put(ps)
                ps = pp.get()
                for hf in range(2):
                    cs = slice(hf * 512, (hf + 1) * 512)
                    mm(ps.t[:, cs], TRIS, sw.t[:, cs], True, True, [r_tri, sw.r], [ps.r])
                act(eWx.t[:], ps.t[:], AF.Exp, [ps.r], [eWx.r], scale=-C0); pp.put(ps)
                pool.put(sw)
                kk = pool.get(); tmp = pool.get()
                kkb = bvec(vec6_d[0:1, :])
                tt("dve", kk.t[:], pk.t[:], kkb.t[:], ALU.mult, [pk.r, kkb.r], [kk.r])
                pool.put(kkb)
                tt("pool", tmp.t[:], kk.t[:], kk.t[:], ALU.mult, [kk.r], [tmp.r])
                red(st[:, 40:56], h3(tmp.t[:]), [tmp.r], [r_st])
                act(st[:, 40:56], st[:, 40:56], AF.Sqrt, [r_st], [r_st])
                ts("dve", st[:, 40:56], st[:, 40:56], 1e-12, None, ALU.max, None, [r_st], [r_st])
                op("dve", lambda e: e.reciprocal(out=st[:, 40:56], in_=st[:, 40:56]), [r_st], [r_st])
                tt("dve", h3(kk.t[:]), h3(kk.t[:]), bch(st[:, 40:56]), ALU.mult, [kk.r, r_st], [kk.r])
                kv_ = pool.get()
                kab = bvec(vec6_d[1:2, :])
                stt(tmp.t[:], a_.t[:], -1.0, kab.t[:], ALU.add, ALU.mult, [a_.r, kab.r, tmp.r], [tmp.r])
                pool.put(kab)
                stt(kv_.t[:], tmp.t[:], 1.0, pk.t[:], ALU.add, ALU.mult, [tmp.r, pk.r], [kv_.r])
                pool.put(pk)
                At = pool.get(); Bt = pool.get(); Kt = pool.get(); Rt = pool.get()
                stt(At.t[:], kk.t[:], -1.0, eWx.t[:], ALU.mult, ALU.mult, [kk.r, eWx.r], [At.r])
                tt("pool", Bt.t[:], kk.t[:], a_.t[:], ALU.mult, [kk.r, a_.r], [Bt.r])
                tt("dve", Bt.t[:], Bt.t[:], eWi.t[:], ALU.mult, [Bt.r, eWi.r], [Bt.r])
                tt("dve", Kt.t[:], kv_.t[:], eWi.t[:], ALU.mult, [kv_.r, eWi.r], [Kt.r])
                tt("pool", Rt.t[:], pr.t[:], eW.t[:], ALU.mult, [pr.r, eW.r], [Rt.r])
                pool.put(kk, a_, eWi, eWx)
                tt("pool", tmp.t[:], pr.t[:], kv_.t[:], ALU.mult, [pr.r, kv_.r, tmp.r], [tmp.r])
                rkb = bvec(vec6_d[2:3, :])
                tt("dve", tmp.t[:], tmp.t[:], rkb.t[:], ALU.mult, [tmp.r, rkb.r], [tmp.r])
                pool.put(rkb)
                red(st[:, 8:24], h3(tmp.t[:]), [tmp.r], [r_st])
                tt("dve", h3(tmp.t[:]), h3(pv.t[:]), bch(st[:, 8:24]), ALU.mult, [pv.r, r_st, tmp.r], [tmp.r])
                bonus = tmp
                pool.put(pr, kv_)
                ps = pp.get()
                for hd in range(16):
                    mm(ps.t[0:64, hd:hd + 1], eW.t[:, hd * 64:(hd + 1) * 64], ident[:, P - 1:P], True, True,
                       [eW.r, r_ident], [ps.r])
                act(WC[:], ps.t[0:64, 0:16], AF.Copy, [ps.r], [r_WC]); pp.put(ps)
                pool.put(eW)
                yrec = pool.get()
                m_s = TRIS.unsqueeze(1).to_broadcast([P, 4, P]); m_i = TRI.unsqueeze(1).to_broadcast([P, 4, P])
                m_l = TRIL.unsqueeze(1).to_broadcast([P, 4, P])
                for hg in range(4):
                    FTa = pool.get(); FTb = pool.get()
                    for j in range(4):
                        hd = hg * 4 + j
                        hs = slice(hd * 64, (hd + 1) * 64)
                        ps = pp.get()
                        tr(ps.t[0:64, 0:128], At.t[:, hs], [At.r], [ps.r])
                        tr(ps.t[0:64, 128:256], Rt.t[:, hs], [Rt.r], [ps.r])
                        tr(ps.t[0:64, 256:384], Bt.t[:, hs], [Bt.r], [ps.r])
                        tr(ps.t[0:64, 384:512], Kt.t[:, hs], [Kt.r], [ps.r])
                        act(FTa.t[0:64, j * 256:(j + 1) * 256], ps.t[0:64, 0:256], AF.Copy, [ps.r], [FTa.r])
                        act(FTb.t[0:64, j * 256:(j + 1) * 256], ps.t[0:64, 256:512], AF.Copy, [ps.r], [FTb.r])
                        pp.put(ps)
                    MX = pool.get(); Lp = pool.get(); Aak = pool.get(); AR = pool.get(); QU = pool.get()
                    MX4 = MX.t[:].rearrange("p (h s n) -> p h s n", h=4, s=2)
                    AR4 = AR.t[:].rearrange("p (h s n) -> p h s n", h=4, s=2)
                    psM = pp.get(); psK = pp.get(); psL = pp.get()
                    for j in range(4):
                        fa = FTa.t[0:64, j * 256:(j + 1) * 256]
                        mm(psM.t[:, j * 256:(j + 1) * 256], FTb.t[0:64, j * 256:j * 256 + 128], fa, True, True,
                           [FTa.r, FTb.r], [psM.r])
                        mm(psK.t[:, j * 256:(j + 1) * 256], FTb.t[0:64, j * 256 + 128:(j + 1) * 256], fa, True, True,
                           [FTa.r, FTb.r], [psK.r])
                        mm(psL.t[:, j * 128:(j + 1) * 128], FTa.t[0:64, j * 256:j * 256 + 128],
                           FTb.t[0:64, j * 256:j * 256 + 128], True, True, [FTa.r, FTb.r], [psL.r])
                    pM4 = psM.t[:].rearrange("p (h s n) -> p h s n", h=4, s=2)
                    pK4 = psK.t[:].rearrange("p (h s n) -> p h s n", h=4, s=2)
                    tt("dve", MX4[:, :, 0, :], pM4[:, :, 0, :], m_s, ALU.mult, [psM.r, r_tri], [MX.r])
                    tt("dve", AR4[:, :, 0, :], pM4[:, :, 1, :], m_i, ALU.mult, [psM.r, r_tri], [AR.r])
                    tt("dve", Aak.t[:, 0:512].rearrange("p (h n) -> p h n", n=P), pK4[:, :, 0, :], m_s, ALU.mult,
                       [psK.r, r_tri], [Aak.r])
                    tt("dve", AR4[:, :, 1, :], pK4[:, :, 1, :], m_i, ALU.mult, [psK.r, r_tri], [AR.r])
                    tt("dve", Lp.t[:, 0:512].rearrange("p (h n) -> p h n", n=P),
                       psL.t[:, 0:512].rearrange("p (h n) -> p h n", n=P), m_l, ALU.mult, [psL.r, r_tri], [Lp.r])
                    pp.put(psM, psK, psL)
                    tt("pool", MX4[:, :, 1, :], MX4[:, :, 0, :], ident[:].unsqueeze(1).to_broadcast([P, 4, P]), ALU.add,
                       [MX.r, r_ident], [MX.r])
                    psQ = pp.get()
                    for j in range(4):
                        hd = hg * 4 + j
                        mm(psQ.t[:, j * 64:(j + 1) * 64], Aak.t[:, j * P:(j + 1) * P], pv.t[:, hd * 64:(hd + 1) * 64],
                           True, True, [Aak.r, pv.r], [psQ.r])
                    act(QU.t[:, 0:256], psQ.t[:, 0:256], AF.Copy, [psQ.r], [QU.r]); pp.put(psQ)
                    p_ = 1
                    while p_ <= 64:
                        if p_ < 64:
                            psA = pp.get(); psB = pp.get()
                            for j in range(4):
                                if p_ == 1:
                                    mm(psA.t[:, j * 256:j * 256 + 128], Lp.t[:, j * P:(j + 1) * P], MX4[:, j, 0, :],
                                       True, True, [Lp.r, MX.r], [psA.r])
                                else:
                                    mm(psA.t[:, j * 256:(j + 1) * 256], Lp.t[:, j * P:(j + 1) * P],
                                       MX.t[:, j * 256:(j + 1) * 256], True, True, [Lp.r, MX.r], [psA.r])
                                mm(psB.t[:, j * P:(j + 1) * P], MX4[:, j, 0, :], Lp.t[:, j * P:(j + 1) * P], True, True,
                                   [Lp.r, MX.r], [psB.r])
                            pA4 = psA.t[:].rearrange("p (h s n) -> p h s n", h=4, s=2)
                            act(MX4[:, :, 0, :], pA4[:, :, 0, :], AF.Copy, [psA.r], [MX.r])
                            if p_ > 1:
                                tt("dve", MX4[:, :, 1, :], pA4[:, :, 1, :], MX4[:, :, 1, :], ALU.add, [psA.r, MX.r], [MX.r])
                            act(Lp.t[:, 0:512], psB.t[:, 0:512], AF.Copy, [psB.r], [Lp.r])
                            pp.put(psA, psB)
                        else:
                            psA = pp.get()
                            for j in range(4):
                                mm(psA.t[:, j * P:(j + 1) * P], Lp.t[:, j * P:(j + 1) * P], MX4[:, j, 1, :], True, True,
                                   [Lp.r, MX.r], [psA.r])
                            tt("dve", MX4[:, :, 1, :], psA.t[:, 0:512].rearrange("p (h n) -> p h n", n=P), MX4[:, :, 1, :],
                               ALU.add, [psA.r, MX.r], [MX.r])
                            pp.put(psA)
                        p_ *= 2
                    psP = pp.get(); psU = pp.get()
                    for j in range(4):
                        hd = hg * 4 + j
                        mm(psP.t[0:64, j * P:(j + 1) * P], At.t[:, hd * 64:(hd + 1) * 64], MX4[:, j, 1, :], True, True,
                           [At.r, MX.r], [psP.r])
                        mm(psU.t[:, j * 64:(j + 1) * 64], MX4[:, j, 1, :], QU.t[:, j * 64:(j + 1) * 64], True, True,
                           [MX.r, QU.r], [psU.r])
                    P1g = Aak
                    act(P1g.t[0:64, 0:512], psP.t[0:64, 0:512], AF.Copy, [psP.r], [P1g.r])
                    act(QU.t[:, 256:512], psU.t[:, 0:256], AF.Copy, [psU.r], [QU.r])
                    pp.put(psP, psU)
                    pool.put(MX, Lp)
                    psS = pp.get()
                    for j in range(4):
                        hd = hg * 4 + j
                        mm(psS.t[:, j * 64:(j + 1) * 64], P1g.t[0:64, j * P:(j + 1) * P], STt[:, hd, :], True, True,
                           [P1g.r, r_ST], [psS.r])
                    tt("dve", QU.t[:, 512:768], psS.t[:, 0:256], QU.t[:, 256:512], ALU.add, [psS.r, QU.r], [QU.r])
                    pp.put(psS)
                    psY = pp.get(); psN = pp.get()
                    for j in range(4):
                        hd = hg * 4 + j
                        hs = slice(hd * 64, (hd + 1) * 64)
                        sa = QU.t[:, 512 + j * 64:512 + (j + 1) * 64]
                        mm(psY.t[:, j * 64:(j + 1) * 64], FTa.t[0:64, j * 256 + 128:(j + 1) * 256], STt[:, hd, :], True, False,
                           [FTa.r, r_ST], [psY.r])
                        mm(psY.t[:, j * 64:(j + 1) * 64], AR4[:, j, 0, :], sa, False, False, [AR.r, QU.r], [psY.r])
                        mm(psY.t[:, j * 64:(j + 1) * 64], AR4[:, j, 1, :], pv.t[:, hs], False, True, [AR.r, pv.r], [psY.r])
                        mm(psN.t[0:64, j * 64:(j + 1) * 64], Bt.t[:, hs], sa, True, False, [Bt.r, QU.r], [psN.r])
                        mm(psN.t[0:64, j * 64:(j + 1) * 64], Kt.t[:, hs], pv.t[:, hs], False, True, [Kt.r, pv.r], [psN.r])
                    act(yrec.t[:, hg * 256:(hg + 1) * 256], psY.t[:, 0:256], AF.Copy, [psY.r], [yrec.r]); pp.put(psY)
                    STg = STt[:, hg * 4:(hg + 1) * 4, :]
                    tt("dve", STg, psN.t[0:64, 0:256].rearrange("p (h v) -> p h v", v=64), STg, ALU.add,
                       [psN.r, r_ST], [r_ST])
                    tt("dve", STg, STg, WC[:, hg * 4:(hg + 1) * 4].unsqueeze(2).to_broadcast([64, 4, 64]), ALU.mult,
                       [r_ST, r_WC], [r_ST])
                    pp.put(psN)
                    pool.put(FTa, FTb, Aak, AR, QU)
                pool.put(Rt, At, Bt, Kt)
                dump("yrec", yrec.t[:], [P, D], [yrec.r], t * P, P, S_LEN)
                yc = yrec; sq = pool.get()
                red(st[:, 8:24], h3(yc.t[:]), [yc.r], [r_st])
                ts("dve", st[:, 8:24], st[:, 8:24], -1.0 / 64, None, ALU.mult, None, [r_st], [r_st])
                tt("dve", h3(yc.t[:]), h3(yc.t[:]), bch(st[:, 8:24]), ALU.add, [yc.r, r_st], [yc.r])
                tt("pool", sq.t[:], yc.t[:], yc.t[:], ALU.mult, [yc.r], [sq.r])
                red(st[:, 8:24], h3(sq.t[:]), [sq.r], [r_st])
                pool.put(sq)
                rstd_of(st[:, 8:24], 1.0 / 64, 64e-5, [r_st])
                tt("dve", h3(yc.t[:]), h3(yc.t[:]), bch(st[:, 8:24]), ALU.mult, [yc.r, r_st], [yc.r])
                lwb = bvec(vec6_d[3:4, :])
                tt("pool", yc.t[:], yc.t[:], lwb.t[:], ALU.mult, [yc.r, lwb.r], [yc.r])
                pool.put(lwb)
                lbb = bvec(vec6_d[4:5, :])
                tt("pool", yc.t[:], yc.t[:], lbb.t[:], ALU.add, [yc.r, lbb.r], [yc.r])
                pool.put(lbb)
                tt("pool", yc.t[:], yc.t[:], bonus.t[:], ALU.add, [yc.r, bonus.r], [yc.r])
                tt("dve", yc.t[:], yc.t[:], g_sb.t[:], ALU.mult, [yc.r, g_sb.r], [yc.r])
                pool.put(bonus, g_sb, pv)
                dump("ya", yc.t[:], [P, D], [yc.r], t * P, P, S_LEN)
                for (src_t, c0) in ((yc, 1280), (yb, 2304)):
                    ps = pp.get(); proj_cols(b, "r", c0, 1024, ps)
                    gsb = pool.get()
                    act(gsb.t[:], ps.t[:], AF.Sigmoid, [ps.r], [gsb.r]); pp.put(ps)
                    tt("dve", src_t.t[:], src_t.t[:], gsb.t[:], ALU.mult, [src_t.r, gsb.r], [src_t.r])
                    pool.put(gsb)
                tt("pool", yc.t[:], yc.t[:], yb.t[:], ALU.add, [yc.r, yb.r], [yc.r])
                pool.put(yb)
                dump("mixed", yc.t[:], [P, D], [yc.r], t * P, P, S_LEN)
                mT = pool.get()
                ps = pp.get()
                for kc in range(8):
                    tr(ps.t[:, kc * P:(kc + 1) * P], yc.t[:, kc * P:(kc + 1) * P], [yc.r], [ps.r])
                act(mT.t[:], ps.t[:], AF.Copy, [ps.r], [mT.r]); pp.put(ps)
                pool.put(yc)
                ps = pp.get()
                for g0 in range(0, 1024, 512):
                    s1 = wslot()
                    dma("sp", wk[s1][:], wov[:, :, g0:g0 + 512], [], [r_wk[s1]])
                    for kc in range(8):
                        mm(ps.t[:, g0:g0 + 512], mT.t[:, kc * P:(kc + 1) * P], wk[s1][:, kc, :], kc == 0, kc == 7,
                           [mT.r, r_wk[s1]], [ps.r])
                x1 = pool.get()
                gt1 = bvec(ada_row(2))
                tt("dve", x1.t[:], ps.t[:], gt1.t[:], ALU.mult, [ps.r, gt1.r], [x1.r]); pp.put(ps)
                pool.put(gt1)
                tt("pool", x1.t[:], x1.t[:], xt[b][:], ALU.add, [x1.r, r_xt[b]], [x1.r])
                pool.put(mT)
                dump("x1", x1.t[:], [P, D], [x1.r], t * P, P, S_LEN)
                drain()
                if 2 in phases:
                    gen[0] = peer_tile(t, x1)
                else:
                    ev = dma("sp", out_d[t * P:(t + 1) * P, :], x1.t[:], [x1.r], [])
                    out_evs.append(ev)
                    pool.put(x1)
            drain()
            op = base_op
            print("sbuf left", nc.sbuf_bytes_remaining, "pool peak", pool.peak, "psum peak", pp.peak, "gs peak", gs.peak, "ops", S.n_ops)
            S.barrier()
            S.emit()

        S.final_wait("sp", out_evs)
    S.emit()
    return nc


def pack_inputs(x, c, p):
    f = np.float32
    A = lambda a: np.ascontiguousarray(np.asarray(a, f))
    d = {
        "x": A(x), "cT": A(np.asarray(c, f).reshape(8, 128).T),
        "ada_w": A(p["ada_w"]), "ada_b": A(p["ada_b"]).reshape(1, -1),
        "norm1_w": A(p["norm1_w"]).reshape(1, -1), "norm2_w": A(p["norm2_w"]).reshape(1, -1),
        "w_in": A(p["w_in"]), "shift_mu": A(p["shift_mu"]).reshape(1, -1),
        "w0a0": A(np.concatenate([np.asarray(p["w0"], f), np.asarray(p["a0"], f)])).reshape(1, -1),
        "wa_up": A(np.concatenate([np.asarray(p["w_lora_up"], f), np.asarray(p["a_lora_up"], f)], 0)),
        "g_up": A(p["g_lora_up"]),
        "vec6": A(np.stack([np.asarray(p[k], f).reshape(-1) for k in ("k_k", "k_a", "r_k", "lnx_w", "lnx_b", "lnx_b")])),
        "qk_nw": A(np.concatenate([np.asarray(p["q_norm_w"], f), np.asarray(p["k_norm_w"], f)])).reshape(1, -1),
        "sinks": A(p["sinks"]).reshape(1, -1),
        "w_out": A(p["w_out"]),
    }
    if "peer_w_q" in p:
        d["peer_w_q"] = A(p["peer_w_q"])
        d["keysT"] = A(np.concatenate([np.asarray(p["peer_keys_1"], f).T, np.asarray(p["peer_keys_2"], f).T], 1))
        d["peer_u"] = A(p["peer_u"])
        d["peer_v"] = A(p["peer_v"])
    return d


def kernel(**inputs):
    x = np.asarray(inputs["x"], np.float32)
    c = np.asarray(inputs["c"], np.float32)
    B, S_LEN, _ = x.shape
    p = {k: np.asarray(v, np.float32)[0] for k, v in inputs.items() if k not in ("x", "c")}
    nc = build(S_LEN)
    in_maps = [pack_inputs(x[b], c[b], p) for b in range(B)]
    res = run_bass_kernel_spmd(nc, in_maps, core_ids=list(range(B)))
    return np.stack([np.asarray(r["out"], np.float32) for r in res.results], 0)
```

```python
import numpy as np
import os
from contextlib import ExitStack
import concourse.bass as bass
import concourse.mybir as mybir
from concourse.bass_utils import run_bass_kernel_spmd

F32 = mybir.dt.float32
U32 = mybir.dt.uint32
I32 = mybir.dt.int32
ALU = mybir.AluOpType
AF = mybir.ActivationFunctionType
AX = mybir.AxisListType

D = 1024
NSH = 3328
INW = 6656
P = 128

ENG_EPOCH = 30000
LANE_EPOCH = 1900
N_LANES = 16


class Res:
    __slots__ = ("name", "w", "rd", "excl")

    def __init__(self, name="", excl=False):
        self.name = name
        self.w = None
        self.rd = {}
        self.excl = excl


def RL(n, name=""):
    return [Res(f"{name}{i}") for i in range(n)]


class Sched:
    def __init__(self, nc):
        self.nc = nc
        self.streams = {k: [] for k in ("pe", "act", "dve", "pool", "sp")}
        self.sems = {}
        self.ecount = {k: 0 for k in self.streams}
        self.eepoch = {k: 0 for k in self.streams}
        self.known = {k: {} for k in self.streams}
        self.lanes = {k: [[0, 0] for _ in range(N_LANES)] for k in self.streams}
        self.lane_i = {k: 0 for k in self.streams}
        self.n_ops = 0

    def op(self, eng, fn, reads=(), writes=(), dma=False):
        self.n_ops += 1
        deps = {}

        def add(ev):
            if ev is None:
                return
            k, v = ev
            if deps.get(k, 0) < v:
                deps[k] = v

        xr = [r for r in reads if r.excl]
        if xr:
            reads = [r for r in reads if not r.excl]
            writes = list(writes) + xr
        for r in reads:
            add(r.w)
        for w in writes:
            add(w.w)
            for k, v in w.rd.items():
                add((k, v))
        if dma:
            li = self.lane_i[eng]
            self.lane_i[eng] = (li + 1) % N_LANES
            lane = self.lanes[eng][li]
            if lane[1] >= LANE_EPOCH:
                add((("lane", eng, li, lane[0]), lane[1] * 16))
                lane[0] += 1
                lane[1] = 0
            key = ("lane", eng, li, lane[0])
            if lane[1] > 0:
                add((key, lane[1] * 16))
            lane[1] += 1
            ev = (key, lane[1] * 16)
            inc = 16
        else:
            if self.ecount[eng] >= ENG_EPOCH:
                self.eepoch[eng] += 1
                self.ecount[eng] = 0
            key = ("eng", eng, self.eepoch[eng])
            self.ecount[eng] += 1
            ev = (key, self.ecount[eng])
            inc = 1
        waits = []
        kn = self.known[eng]
        for k, v in deps.items():
            if eng == "pe" and k[0] == "eng" and k[1] == "pe":
                continue
            if kn.get(k, 0) >= v:
                continue
            kn[k] = v
            waits.append((k, v))
        self.streams[eng].append((waits, fn, ev[0], inc))
        for w in writes:
            w.w = ev
            w.rd = {}
        for r in reads:
            if r.rd.get(ev[0], 0) < ev[1]:
                r.rd[ev[0]] = ev[1]
        return ev

    def barrier(self):
        evs = []
        for e in self.streams:
            if self.ecount[e] > 0:
                evs.append((("eng", e, self.eepoch[e]), self.ecount[e]))
        for le in self.lanes:
            for li, lane in enumerate(self.lanes[le]):
                if lane[1] > 0:
                    evs.append((("lane", le, li, lane[0]), lane[1] * 16))
        for e in self.streams:
            waits = []
            for k, v in evs:
                if k[0] == "eng" and k[1] == e:
                    continue
                if self.known[e].get(k, 0) >= v:
                    continue
                self.known[e][k] = v
                waits.append((k, v))
            self.streams[e].append((waits, None, None, 0))

    def final_wait(self, eng, evs):
        self.streams[eng].append((list(evs), None, None, 0))

    def emit(self):
        nc = self.nc
        for eng, st in self.streams.items():
            for waits, fn, key, inc in st:
                for k, v in waits:
                    if k not in self.sems:
                        self.sems[k] = nc.alloc_semaphore("s%d" % len(self.sems))
                if key is not None and key not in self.sems:
                    self.sems[key] = nc.alloc_semaphore("s%d" % len(self.sems))
        streams = self.streams
        self.streams = {k: [] for k in streams}
        with nc.Block() as block:
            def mk(engname):
                def body(e):
                    for waits, fn, key, inc in streams[engname]:
                        for k, v in waits:
                            e.wait_ge(self.sems[k], v)
                        if fn is not None:
                            fn(e).then_inc(self.sems[key], inc)
                return body
            block.tensor(mk("pe"))
            block.scalar(mk("act"))
            block.vector(mk("dve"))
            block.gpsimd(mk("pool"))
            block.sync(mk("sp"))


class Ctx:
    def __init__(self, nc):
        self.nc = nc
        self.S = Sched(nc)
        self.n = 0

    def sb(self, es, shape, dt=F32, name=None):
        self.n += 1
        return es.enter_context(self.nc.sbuf_tensor(name or f"t{self.n}", list(shape), dt))

    def ps(self, es, shape, dt=F32, name=None):
        self.n += 1
        return es.enter_context(self.nc.psum_tensor(name or f"p{self.n}", list(shape), dt))


from collections import deque
EW = 'dve'

C0 = float(np.exp(-0.5))


class TL:
    __slots__ = ("t", "r")

    def __init__(self, t, r):
        self.t = t
        self.r = r


class FPool:
    def __init__(self, items):
        self.q = deque(items)

    def get(self):
        self.out = getattr(self, "out", 0) + 1
        self.peak = max(getattr(self, "peak", 0), self.out)
        return self.q.popleft()

    def put(self, *items):
        for it in items:
            self.out -= 1
            self.q.append(it)


def build(S_LEN, dbg=None, phases=(1, 2)):
    nc = bass.Bass("TRN2", target_bir_lowering=False)
    NT = S_LEN // P
    C = Ctx(nc)
    S = C.S
    op = S.op

    def din(name, shape, dt=F32):
        return nc.dram_tensor(name, list(shape), dt, kind="ExternalInput").ap()

    x_d = din("x", [S_LEN, D])
    cT_d = din("cT", [P, 8])
    ada_w_d = din("ada_w", [D, 6 * D])
    ada_b_d = din("ada_b", [1, 6 * D])
    n1w_d = din("norm1_w", [1, D])
    n2w_d = din("norm2_w", [1, D])
    w_in_d = din("w_in", [D, INW])
    mu_d = din("shift_mu", [1, NSH])
    w0a0_d = din("w0a0", [1, 2 * D])
    wa_up_d = din("wa_up", [P, D])
    g_up_d = din("g_up", [P, D])
    vec6_d = din("vec6", [6, D])
    qk_nw_d = din("qk_nw", [1, 128])
    sinks_d = din("sinks", [1, 16])
    w_out_d = din("w_out", [D, D])
    wq_d = din("peer_w_q", [D, 2048])
    keysT_d = din("keysT", [P, 256])
    pu_d = din("peer_u", [16384, D])
    pv_d = din("peer_v", [16384, D])
    out_d = nc.dram_tensor("out", [S_LEN, D], F32, kind="ExternalOutput").ap()
    dbg_d = {}

    def dump(name, ap_sb, shape, reads, row0=None, nrows=None, total_rows=None):
        if not dbg or name not in dbg:
            return
        if name not in dbg_d:
            dbg_d[name] = nc.dram_tensor("dbg_" + name, [total_rows or shape[0]] + list(shape[1:]), F32,
                                         kind="ExternalOutput").ap()
        dst = dbg_d[name] if row0 is None else dbg_d[name][row0:row0 + nrows]
        ev = op("sp", lambda e: e.dma_start(out=dst, in_=ap_sb), reads, [], dma=True)
        out_evs.append(ev)

    ada_s = nc.dram_tensor("ada_s", [1, 6 * D], F32, kind="Internal").ap()

    out_evs = []
    r_w1s = Res(); r_w2s = Res(); r_x1 = RL(NT)

    def tt(eng, out, in0, in1, opc, R, W):
        return op(eng, lambda e: e.tensor_tensor(out=out, in0=in0, in1=in1, op=opc), R, W)

    def ts(eng, out, in0, s1, s2, o0, o1, R, W):
        if o1 is None:
            return op(eng, lambda e: e.tensor_scalar(out=out, in0=in0, scalar1=s1, scalar2=None, op0=o0), R, W)
        return op(eng, lambda e: e.tensor_scalar(out=out, in0=in0, scalar1=s1, scalar2=s2, op0=o0, op1=o1), R, W)

    def stt(out, in0, sc, in1, o0, o1, R, W):
        return op("dve", lambda e: e.scalar_tensor_tensor(out=out, in0=in0, scalar=sc, in1=in1, op0=o0, op1=o1), R, W)

    def act(out, in_, func, R, W, scale=None, bias=None, accum=None):
        kw = {}
        if scale is not None:
            kw["scale"] = scale
        if bias is not None:
            kw["bias"] = bias
        if accum is not None:
            kw["accum_out"] = accum
        return op("act", lambda e: e.activation(out=out, in_=in_, func=func, **kw), R, W)

    def mm(out, lhsT, rhs, start, stop, R, W):
        return op("pe", lambda e: e.matmul(out, lhsT, rhs, start=start, stop=stop), R, W)

    def tr(out, in_, R, W):
        return op("pe", lambda e: e.transpose(out, in_, ident[0:in_.shape[0], 0:in_.shape[0]]), list(R) + [r_ident], W)

    def red(out, in_, R, W, opc=ALU.add):
        return op("dve", lambda e: e.tensor_reduce(out=out, in_=in_, axis=AX.X, op=opc), R, W)

    def dma(eng, out, in_, R, W):
        return op(eng, lambda e: e.dma_start(out=out, in_=in_), R, W, dma=True)

    def h3(ap, k=64):
        return ap.rearrange("p (h k) -> p h k", k=k)

    def bch(ap16, n=16, k=64):
        return ap16.unsqueeze(2).to_broadcast([ap16.shape[0], n, k])

    with ExitStack() as es0:
        ident = C.sb(es0, [P, P]); r_ident = Res("ident")
        ones_row = C.sb(es0, [1, P]); r_ones = Res("ones")
        op("pool", lambda e: e.memset(ident[:], 0.0), [], [r_ident])
        op("pool", lambda e: e.affine_select(out=ident[:], in_=ident[:], pattern=[[-1, P]],
                                            compare_op=ALU.not_equal, fill=1.0, base=0,
                                            channel_multiplier=1), [r_ident], [r_ident])
        op("pool", lambda e: e.memset(ones_row[:], 1.0), [], [r_ones])

        with ExitStack() as es:
            cT = C.sb(es, [P, 8]); r_cT = Res()
            cond = C.sb(es, [P, 8]); r_cond = Res()
            arow = C.sb(es, [1, 6 * D]); r_arow = Res()
            brow = C.sb(es, [1, 6 * D]); r_brow = Res()
            nrow = C.sb(es, [1, 2 * D]); r_nrow = Res()
            wb = [C.sb(es, [P, 8, 512]) for _ in range(2)]; r_wb = RL(2)
            pa = [C.ps(es, [1, 512]) for _ in range(2)]; r_pa = RL(2)
            dma("sp", cT[:], cT_d, [], [r_cT])
            dma("sp", brow[:], ada_b_d, [], [r_brow])
            dma("sp", nrow[:, 0:D], n1w_d, [], [r_nrow])
            dma("sp", nrow[:, D:2 * D], n2w_d, [], [r_nrow])
            act(cond[:], cT[:], AF.Silu, [r_cT], [r_cond])
            aw = ada_w_d.rearrange("(kc p) n -> p kc n", p=P)
            for g in range(12):
                b = g % 2
                dma("sp", wb[b][:], aw[:, :, g * 512:(g + 1) * 512], [], [r_wb[b]])
                for kc in range(8):
                    mm(pa[b][:], cond[:, kc:kc + 1], wb[b][:, kc, :], kc == 0, kc == 7, [r_cond, r_wb[b]], [r_pa[b]])
                tt("dve", arow[:, g * 512:(g + 1) * 512], pa[b][:], brow[:, g * 512:(g + 1) * 512], ALU.add,
                   [r_pa[b], r_brow], [r_arow])
            for (slot, off) in ((1, 0), (4, D)):
                stt(arow[:, slot * D:(slot + 1) * D], arow[:, slot * D:(slot + 1) * D], 1.0, nrow[:, off:off + D],
                    ALU.add, ALU.mult, [r_arow, r_nrow], [r_arow])
            r_ada_s = Res()
            dma("sp", ada_s, arow[:], [r_arow], [r_ada_s])
            S.barrier()
            S.emit()

        if 1 in phases:
          with ExitStack() as es:
            wa_up = C.sb(es, [P, D]); r_waup = Res()
            g_up = C.sb(es, [P, D]); r_gup = Res()
            dma("sp", wa_up[:], wa_up_d, [], [r_waup])
            dma("sp", g_up[:], g_up_d, [], [r_gup])
            tri = C.sb(es, [P, 3, P]); r_tri = Res()
            op("pool", lambda e: e.memset(tri[:], 1.0), [], [r_tri])
            for i, (pat, cm, cop) in enumerate((([[1, P]], -1, ALU.is_ge), ([[1, P]], -1, ALU.is_gt),
                                                ([[-1, P]], 1, ALU.is_gt))):
                op("pool", lambda e, i=i, pat=pat, cm=cm, cop=cop: e.affine_select(
                    out=tri[:, i, :], in_=tri[:, i, :], pattern=pat, compare_op=cop, fill=0.0, base=0,
                    channel_multiplier=cm), [r_tri], [r_tri])
            TRI, TRIS, TRIL = tri[:, 0, :], tri[:, 1, :], tri[:, 2, :]
            shm = C.sb(es, [P, 2, P]); r_shm = Res()
            op("pool", lambda e: e.memset(shm[:], 1.0), [], [r_shm])
            op("pool", lambda e: e.affine_select(out=shm[:, 0, :], in_=shm[:, 0, :], pattern=[[1, P]],
                                                compare_op=ALU.is_equal, fill=0.0, base=-1, channel_multiplier=-1),
               [r_shm], [r_shm])
            op("pool", lambda e: e.memset(shm[:, 1, :], 0.0), [r_shm], [r_shm])
            op("pool", lambda e: e.tensor_copy(out=shm[:, 1, 0:1], in_=ident[:, P - 1:P]), [r_shm, r_ident], [r_shm])
            SHM, EMM = shm[:, 0, :], shm[:, 1, :]
            qkw = C.sb(es, [P, 128]); r_qkw = Res()
            dma("sp", qkw[:], qk_nw_d.partition_broadcast(P), [], [r_qkw])
            esk = C.sb(es, [P, 16]); r_esk = Res()
            dma("sp", esk[:], sinks_d.partition_broadcast(P), [], [r_esk])
            act(esk[:], esk[:], AF.Exp, [r_esk], [r_esk])
            keysT = C.sb(es, [P, 256]); r_keys = Res()
            dma("sp", keysT[:], keysT_d, [], [r_keys])
            iot = C.sb(es, [P, 16]); r_iot = Res()
            op("pool", lambda e: e.iota(iot[:], pattern=[[1, 16]], base=0, channel_multiplier=0,
                                        allow_small_or_imprecise_dtypes=True), [], [r_iot])

            xt = [C.sb(es, [P, D]) for _ in range(2)]; r_xt = RL(2)
            st = C.sb(es, [P, 64]); r_st = Res()
            st2 = C.sb(es, [P, 32]); r_st2 = Res()
            hT = [C.sb(es, [P, 8, P + 1]) for _ in range(2)]; r_hT = RL(2)
            NWK = 2
            WKW = 512
            wk = [C.sb(es, [P, 8, WKW]) for _ in range(NWK)]; r_wk = RL(NWK)
            kT = [C.sb(es, [64, 2, P]) for _ in range(2)]; r_kT = RL(2)
            vv = [C.sb(es, [P, 2, 65]) for _ in range(2)]; r_vv = RL(2)
            STt = C.sb(es, [64, 16, 64]); r_ST = Res()
            WC = C.sb(es, [64, 16]); r_WC = Res()
            op("pool", lambda e: e.memset(STt[:], 0.0), [], [r_ST])
            for b_ in range(2):
                op("pool", lambda e, b_=b_: e.memset(vv[b_][:], 1.0), [], [r_vv[b_]])
            tmpk = C.sb(es, [P, 256]); r_tmpk = Res()
            V12 = C.sb(es, [P, 16, 16]); r_V12 = Res()
            I12 = C.sb(es, [P, 16, 16], U32); r_I12 = Res()
            I12f = C.sb(es, [P, 16, 16]); r_I12f = Res()
            scv = C.sb(es, [P, 8, 16]); r_scv = Res()
            posu = C.sb(es, [P, 8, 16], U32); r_posu = Res()
            pa_u = C.sb(es, [P, 2, 128], U32); r_pau = Res()
            pa_f = C.sb(es, [P, 2, 128]); r_paf = Res()
            isel = C.sb(es, [P, 2, 128]); r_isel = Res()
            idxf = C.sb(es, [P, 128]); r_idxf = Res()
            idxu = C.sb(es, [P, 128], I32); r_idxu = Res()
            gate = C.sb(es, [P, 8, 16]); r_gate = Res()
            acts = C.sb(es, [P, 128]); r_acts = Res()
            wts = C.sb(es, [P, 128]); r_wts = Res()
            dg = C.sb(es, [P, 4, P]); r_dg = RL(4)
            NG = 8
            gs = FPool([TL(C.sb(es, [P, D]), Res(f"g{i}")) for i in range(NG)])
            NPOOL = 23
            pool = FPool([TL(C.sb(es, [P, D]), Res(f"pl{i}")) for i in range(NPOOL)])
            pp = FPool([TL(C.ps(es, [P, 1024]), Res(f"ps{i}", excl=True)) for i in range(4)])
            wiv = w_in_d.rearrange("(kc p) n -> p kc n", p=P)
            wov = w_out_d.rearrange("(kc p) n -> p kc n", p=P)
            wqv = wq_d.rearrange("(kc p) n -> p kc n", p=P)
            wki = [0]
            op("pool", lambda e: e.memset(hT[1][:, :, P:P + 1], 0.0), [], [r_hT[1]])

            def wslot():
                s_ = wki[0] % NWK
                wki[0] += 1
                return s_

            def bvec(src_ap, width=D):
                tl = pool.get()
                dma("sp", tl.t[:, 0:width], src_ap.partition_broadcast(P), [r_ada_s], [tl.r])
                return tl

            def ada_row(slot):
                return ada_s[:, slot * D:(slot + 1) * D]

            def proj_cols(b, kind, c0, width, ps):
                off = 0 if kind == "s" else NSH
                for g0 in range(0, width, WKW):
                    wd = min(WKW, width - g0)
                    cc = off + c0 + g0
                    s1 = wslot()
                    dma("sp", wk[s1][:, :, 0:wd], wiv[:, :, cc:cc + wd], [], [r_wk[s1]])
                    for kc in range(8):
                        mm(ps.t[:, g0:g0 + wd], hT[b][:, kc, 1:P + 1], wk[s1][:, kc, 0:wd], kc == 0, kc == 7,
                           [r_hT[b], r_wk[s1]], [ps.r])

            prev_raw = {}

            def shifted_proj(b, t, c0, width):
                ps = pp.get(); proj_cols(b, "s", c0, width, ps)
                raw = pool.get()
                act(raw.t[:, 0:width], ps.t[:, 0:width], AF.Copy, [ps.r], [raw.r]); pp.put(ps)
                ps2 = pp.get()
                prv = prev_raw.get(c0)
                for g0 in range(0, width, 512):
                    wd = min(512, width - g0)
                    mm(ps2.t[:, g0:g0 + wd], SHM, raw.t[:, g0:g0 + wd], True, prv is None, [r_shm, raw.r], [ps2.r])
                    if prv is not None:
                        mm(ps2.t[:, g0:g0 + wd], EMM, prv.t[:, g0:g0 + wd], False, True, [r_shm, prv.r], [ps2.r])
                d = pool.get()
                tt("dve", d.t[:, 0:width], ps2.t[:, 0:width], raw.t[:, 0:width], ALU.subtract, [ps2.r, raw.r], [d.r])
                pp.put(ps2)
                if prv is not None:
                    pool.put(prv)
                mub = bvec(mu_d[:, c0:c0 + width], width)
                tt(EW, d.t[:, 0:width], d.t[:, 0:width], mub.t[:, 0:width], ALU.mult, [d.r, mub.r], [d.r])
                pool.put(mub)
                tt(EW, d.t[:, 0:width], d.t[:, 0:width], raw.t[:, 0:width], ALU.add, [d.r, raw.r], [d.r])
                prev_raw[c0] = raw
                return d

            def rstd_of(ss_ap, scale, eps, R):
                ts("dve", ss_ap, ss_ap, scale, eps, ALU.mult, ALU.add, R, R)
                act(ss_ap, ss_ap, AF.Sqrt, R, R)
                op("dve", lambda e: e.reciprocal(out=ss_ap, in_=ss_ap), R, R)

            def top16(src, dstv, dsti, n, R_src):
                op("dve", lambda e: e.max(out=dstv[:, 0:8], in_=src), R_src, [r_V12])
                op("dve", lambda e: e.match_replace(out=tmpk[:, 0:n], in_to_replace=dstv[:, 0:8], in_values=src,
                                                    imm_value=-1e30), R_src + [r_V12], [r_tmpk])
                op("dve", lambda e: e.max(out=dstv[:, 8:16], in_=tmpk[:, 0:n]), [r_tmpk], [r_V12])
                op("dve", lambda e: e.max_index(out=dsti[:, 0:8], in_max=dstv[:, 0:8], in_values=src),
                   R_src + [r_V12], [r_I12])
                op("dve", lambda e: e.max_index(out=dsti[:, 8:16], in_max=dstv[:, 8:16], in_values=tmpk[:, 0:n]),
                   [r_tmpk, r_V12], [r_I12])

            def peer_tile(t, x1):
                h2 = pool.get()
                act(h2.t[:], x1.t[:], AF.Square, [x1.r], [h2.r, r_st2], accum=st2[:, 0:1])
                rstd_of(st2[:, 0:1], 1.0 / D, 1e-6, [r_st2])
                g2 = bvec(ada_row(4))
                stt(h2.t[:], x1.t[:], st2[:, 0:1], g2.t[:], ALU.mult, ALU.mult, [x1.r, r_st2, g2.r], [h2.r])
                pool.put(g2)
                sh2 = bvec(ada_row(3))
                tt(EW, h2.t[:], h2.t[:], sh2.t[:], ALU.add, [h2.r, sh2.r], [h2.r])
                pool.put(sh2)
                h2T = pool.get()
                ps = pp.get()
                for kc in range(8):
                    tr(ps.t[:, kc * P:(kc + 1) * P], h2.t[:, kc * P:(kc + 1) * P], [h2.r], [ps.r])
                act(h2T.t[:], ps.t[:], AF.Copy, [ps.r], [h2T.r]); pp.put(ps)
                qtm = [pool.get(), pool.get()]
                for half in range(2):
                    ps = pp.get()
                    for g0 in range(0, 1024, WKW):
                        s1 = wslot()
                        dma("sp", wk[s1][:], wqv[:, :, half * 1024 + g0:half * 1024 + g0 + WKW], [], [r_wk[s1]])
                        for kc in range(8):
                            mm(ps.t[:, g0:g0 + WKW], h2T.t[:, kc * P:(kc + 1) * P], wk[s1][:, kc, :],
                               kc == 0, kc == 7, [r_wk[s1], h2T.r], [ps.r])
                    act(qtm[half].t[:], ps.t[:], AF.Copy, [ps.r], [qtm[half].r]); pp.put(ps)
                qT = [pool.get(), pool.get()]
                for half in range(2):
                    ps = pp.get()
                    for q_ in range(8):
                        tr(ps.t[:, q_ * P:(q_ + 1) * P], qtm[half].t[:, q_ * P:(q_ + 1) * P], [qtm[half].r], [ps.r])
                    act(qT[half].t[:], ps.t[:], AF.Copy, [ps.r], [qT[half].r]); pp.put(ps)
                pool.put(*qtm)
                pool.put(h2T)
                yield
                s12 = [pool.get(), pool.get()]
                for half in range(2):
                    ps = pp.get()
                    for q_ in range(8):
                        j = q_ % 2
                        mm(ps.t[:, q_ * P:(q_ + 1) * P], qT[half].t[:, q_ * P:(q_ + 1) * P], keysT[:, j * P:(j + 1) * P],
                           True, True, [qT[half].r, r_keys], [ps.r])
                    act(s12[half].t[:], ps.t[:], AF.Copy, [ps.r], [s12[half].r]); pp.put(ps)
                pool.put(*qT)
                for hj in range(16):
                    top16(s12[hj // 8].t[:, (hj % 8) * P:(hj % 8 + 1) * P], V12[:, hj, :], I12[:, hj, :], 128,
                          [s12[hj // 8].r])
                    yield
                pool.put(*s12)
                cand = [pool.get(), pool.get()]
                V4 = V12[:].rearrange("p (h j) k -> p h j k", j=2)
                for half in range(2):
                    tt("dve", cand[half].t[:].rearrange("p (h a b) -> p h a b", a=16, b=16),
                       V4[:, half * 4:(half + 1) * 4, 0, :].unsqueeze(3).to_broadcast([P, 4, 16, 16]),
                       V4[:, half * 4:(half + 1) * 4, 1, :].unsqueeze(2).to_broadcast([P, 4, 16, 16]), ALU.add,
                       [r_V12], [cand[half].r])
                op("dve", lambda e: e.tensor_copy(out=I12f[:], in_=I12[:]), [r_I12], [r_I12f])
                yield
                for h in range(8):
                    top16(cand[h // 4].t[:, (h % 4) * 256:(h % 4 + 1) * 256], scv[:, h, :], posu[:, h, :], 256,
                          [cand[h // 4].r])
                    yield
                pool.put(*cand)
                posf2 = posu[:].rearrange("p h k -> p (h k)")
                op("dve", lambda e: e.tensor_single_scalar(out=pa_u[:, 0, :], in_=posf2, scalar=4,
                                                           op=ALU.logical_shift_right), [r_I12], [r_pau])
                op("dve", lambda e: e.tensor_single_scalar(out=pa_u[:, 1, :], in_=posf2, scalar=15,
                                                           op=ALU.bitwise_and), [r_I12], [r_pau])
                op("dve", lambda e: e.tensor_copy(out=pa_f[:], in_=pa_u[:]), [r_pau], [r_paf])
                I4 = I12f[:].rearrange("p (h j) k -> p h j k", j=2)
                for j in range(2):
                    for half in range(2):
                        oh = pool.get()
                        oh4 = oh.t[:].rearrange("p (h k a) -> p h k a", k=16, a=16)
                        sel_f = pa_f[:, j, half * 64:(half + 1) * 64].rearrange("p (h k) -> p h k", k=16)
                        tt("dve", oh4, sel_f.unsqueeze(3).to_broadcast([P, 4, 16, 16]),
                           iot[:].unsqueeze(1).unsqueeze(1).to_broadcast([P, 4, 16, 16]), ALU.is_equal,
                           [r_paf, r_iot], [oh.r])
                        tt("dve", oh4, oh4, I4[:, half * 4:(half + 1) * 4, j, :].unsqueeze(2).to_broadcast([P, 4, 16, 16]),
                           ALU.mult, [oh.r, r_I12f], [oh.r])
                        red(isel[:, j, half * 64:(half + 1) * 64], oh.t[:].rearrange("p (hk a) -> p hk a", a=16),
                            [oh.r], [r_isel])
                        pool.put(oh)
                        yield
                stt(idxf[:], isel[:, 0, :], 128.0, isel[:, 1, :], ALU.mult, ALU.add, [r_isel], [r_idxf])
                op("dve", lambda e: e.tensor_copy(out=idxu[:], in_=idxf[:]), [r_idxf], [r_idxu])
                dump("idx", idxf[:], [P, 128], [r_idxf], t * P, P, S_LEN)
                tt("dve", gate[:], scv[:], scv[:, :, 0:1].to_broadcast([P, 8, 16]), ALU.subtract, [r_V12], [r_gate])
                act(gate[:], gate[:], AF.Exp, [r_gate], [r_gate])
                red(st2[:, 8:16], gate[:], [r_gate], [r_st2])
                op("dve", lambda e: e.reciprocal(out=st2[:, 8:16], in_=st2[:, 8:16]), [r_st2], [r_st2])
                tt("dve", gate[:], gate[:], st2[:, 8:16].unsqueeze(2).to_broadcast([P, 8, 16]), ALU.mult,
                   [r_gate, r_st2], [r_gate])
                yield
                for k in range(128):
                    g_ = gs.get()
                    op("pool", lambda e, g_=g_, k=k: e.indirect_dma_start(
                        out=g_.t[:, :], out_offset=None, in_=pu_d[:, :],
                        in_offset=bass.IndirectOffsetOnAxis(ap=idxu[:, k:k + 1], axis=0)),
                       [r_idxu], [g_.r], dma=True)
                    op("dve", lambda e, g_=g_, k=k, h2=h2: e.scalar_tensor_tensor(
                        out=g_.t[:], in0=g_.t[:], scalar=1.0, in1=h2.t[:], op0=ALU.mult, op1=ALU.mult,
                        accum_out=acts[:, k:k + 1]), [g_.r, h2.r], [g_.r, r_acts])
                    gs.put(g_)
                    yield
                pool.put(h2)
                act(wts[:], acts[:], AF.Gelu, [r_acts], [r_wts])
                tt("dve", wts[:], wts[:], gate[:].rearrange("p h k -> p (h k)"), ALU.mult, [r_wts, r_gate], [r_wts])
                acc = pool.get()
                VPE = 0
                pe_ks = set(range(0, 128, 2)[:VPE]) if VPE <= 64 else set(range(128)) - set(range(1, 128, 2)[:128 - VPE])
                psV = pp.get() if pe_ks else None
                n_pe = 0; first_dve = True
                for k in range(128):
                    g_ = gs.get()
                    op("pool", lambda e, g_=g_, k=k: e.indirect_dma_start(
                        out=g_.t[:, :], out_offset=None, in_=pv_d[:, :],
                        in_offset=bass.IndirectOffsetOnAxis(ap=idxu[:, k:k + 1], axis=0)),
                       [r_idxu], [g_.r], dma=True)
                    if k in pe_ks:
                        dgi = n_pe % 4
                        act(dg[:, dgi, :], ident[:], AF.Copy, [r_ident, r_wts], [r_dg[dgi]], scale=wts[:, k:k + 1])
                        for hf in range(2):
                            mm(psV.t[:, hf * 512:(hf + 1) * 512], dg[:, dgi, :], g_.t[:, hf * 512:(hf + 1) * 512],
                               n_pe == 0, n_pe == len(pe_ks) - 1, [r_dg[dgi], g_.r], [psV.r])
                        n_pe += 1
                    elif first_dve:
                        ts("dve", acc.t[:], g_.t[:], wts[:, k:k + 1], None, ALU.mult, None, [g_.r, r_wts], [acc.r])
                        first_dve = False
                    else:
                        stt(acc.t[:], g_.t[:], wts[:, k:k + 1], acc.t[:], ALU.mult, ALU.add, [g_.r, r_wts, acc.r], [acc.r])
                    gs.put(g_)
                    yield
                if psV is not None:
                    if first_dve:
                        act(acc.t[:], psV.t[:], AF.Copy, [psV.r], [acc.r])
                    else:
                        tt("dve", acc.t[:], psV.t[:], acc.t[:], ALU.add, [psV.r, acc.r], [acc.r])
                    pp.put(psV)
                dump("peer", acc.t[:], [P, D], [acc.r], t * P, P, S_LEN)
                gt2 = bvec(ada_row(5))
                tt("dve", acc.t[:], acc.t[:], gt2.t[:], ALU.mult, [acc.r, gt2.r], [acc.r])
                pool.put(gt2)
                tt(EW, acc.t[:], acc.t[:], x1.t[:], ALU.add, [acc.r, x1.r], [acc.r])
                ev = dma("sp", out_d[t * P:(t + 1) * P, :], acc.t[:], [acc.r], [])
                out_evs.append(ev)
                pool.put(acc, x1)

            gen = [None]
            PUMP = 3
            base_op = S.op
            busy = [False]

            def pump(n):
                if gen[0] is None or busy[0]:
                    return
                busy[0] = True
                try:
                    for _ in range(n):
                        next(gen[0])
                except StopIteration:
                    gen[0] = None
                busy[0] = False

            def op_p(eng, fn, reads=(), writes=(), dma=False):
                ev = base_op(eng, fn, reads, writes, dma)
                if eng == "dve" and not busy[0]:
                    pump(PUMP)
                return ev

            def drain():
                while gen[0] is not None:
                    pump(64)

            for t in range(NT):
                b = t % 2
                op = op_p
                dma("sp", xt[b][:], x_d[t * P:(t + 1) * P, :], [], [r_xt[b]])
                hh = pool.get()
                act(hh.t[:], xt[b][:], AF.Square, [r_xt[b]], [hh.r, r_st], accum=st[:, 0:1])
                rstd_of(st[:, 0:1], 1.0 / D, 1e-6, [r_st])
                g1 = bvec(ada_row(1))
                stt(hh.t[:], xt[b][:], st[:, 0:1], g1.t[:], ALU.mult, ALU.mult, [r_xt[b], r_st, g1.r], [hh.r])
                pool.put(g1)
                sh1 = bvec(ada_row(0))
                tt(EW, hh.t[:], hh.t[:], sh1.t[:], ALU.add, [hh.r, sh1.r], [hh.r])
                pool.put(sh1)
                act(hT[b][:, :, 0:1], hT[1 - b][:, :, P:P + 1], AF.Copy, [r_hT[1 - b]], [r_hT[b]])
                ps = pp.get()
                for kc in range(8):
                    tr(ps.t[:, kc * P:(kc + 1) * P], hh.t[:, kc * P:(kc + 1) * P], [hh.r], [ps.r])
                act(hT[b][:, :, 1:P + 1], ps.t[:].rearrange("p (q n) -> p q n", q=8), AF.Copy, [ps.r], [r_hT[b]])
                pp.put(ps); pool.put(hh)

                aq = pool.get(); akav = pool.get()
                ps = pp.get(); proj_cols(b, "r", 0, 1024, ps)
                act(aq.t[:], ps.t[:], AF.Copy, [ps.r], [aq.r]); pp.put(ps)
                ps = pp.get(); proj_cols(b, "r", 1024, 256, ps)
                act(akav.t[:, 0:256], ps.t[:, 0:256], AF.Copy, [ps.r], [akav.r]); pp.put(ps)
                sq = pool.get()
                act(sq.t[:], aq.t[:], AF.Square, [aq.r], [sq.r])
                red(st[:, 8:24], h3(sq.t[:]), [sq.r], [r_st])
                act(sq.t[:, 0:128], akav.t[:, 0:128], AF.Square, [akav.r], [sq.r])
                red(st[:, 24:26], h3(sq.t[:, 0:128]), [sq.r], [r_st])
                pool.put(sq)
                rstd_of(st[:, 8:26], 1.0 / 64, 1e-6, [r_st])
                tt("dve", h3(aq.t[:]), h3(aq.t[:]), bch(st[:, 8:24]), ALU.mult, [aq.r, r_st], [aq.r])
                tt(EW, h3(aq.t[:]), h3(aq.t[:]), qkw[:, 0:64].unsqueeze(1).to_broadcast([P, 16, 64]), ALU.mult,
                   [aq.r, r_qkw], [aq.r])
                tt("dve", h3(akav.t[:, 0:128]), h3(akav.t[:, 0:128]), bch(st[:, 24:26], 2), ALU.mult,
                   [akav.r, r_st], [akav.r])
                tt(EW, h3(akav.t[:, 0:128]), h3(akav.t[:, 0:128]),
                   qkw[:, 64:128].unsqueeze(1).to_broadcast([P, 2, 64]), ALU.mult, [akav.r, r_qkw], [akav.r])
                act(vv[b][:, :, 0:64], h3(akav.t[:, 128:256]), AF.Copy, [akav.r], [r_vv[b]])
                qT = [pool.get(), pool.get()]
                for half in range(2):
                    ps = pp.get()
                    for j in range(8):
                        hd = half * 8 + j
                        tr(ps.t[0:64, j * P:(j + 1) * P], aq.t[:, hd * 64:(hd + 1) * 64], [aq.r], [ps.r])
                    act(qT[half].t[0:64, :], ps.t[0:64, :], AF.Copy, [ps.r], [qT[half].r]); pp.put(ps)
                ps = pp.get()
                for g in range(2):
                    tr(ps.t[0:64, g * P:(g + 1) * P], akav.t[:, g * 64:(g + 1) * 64], [akav.r], [ps.r])
                act(kT[b][:].rearrange("p g n -> p (g n)"), ps.t[0:64, 0:256], AF.Copy, [ps.r], [r_kT[b]]); pp.put(ps)
                pool.put(aq, akav)
                yb = pool.get()
                for g in range(2):
                    Ec = pool.get(); Ep = pool.get()
                    for (E, kb, mask) in ((Ec, b, TRI), (Ep, 1 - b, TRIL)):
                        if E is Ep and t == 0:
                            continue
                        ps = pp.get()
                        for hf in range(2):
                            mm(ps.t[:, hf * 512:(hf + 1) * 512], kT[kb][:, g, :], qT[g].t[0:64, hf * 512:(hf + 1) * 512],
                               True, True, [r_kT[kb], qT[g].r], [ps.r])
                        act(E.t[:], ps.t[:], AF.Exp, [ps.r], [E.r], scale=0.125); pp.put(ps)
                        tt(EW, E.t[:].rearrange("p (h n) -> p h n", n=P), E.t[:].rearrange("p (h n) -> p h n", n=P),
                           mask.unsqueeze(1).to_broadcast([P, 8, P]), ALU.mult, [E.r, r_tri], [E.r])
                    ps = pp.get()
                    for j in range(8):
                        mm(ps.t[:, j * 128:j * 128 + 65], Ec.t[:, j * P:(j + 1) * P], vv[b][:, g, :], True, t == 0,
                           [Ec.r, r_vv[b]], [ps.r])
                        if t > 0:
                            mm(ps.t[:, j * 128:j * 128 + 65], Ep.t[:, j * P:(j + 1) * P], vv[1 - b][:, g, :], False, True,
                               [Ep.r, r_vv[1 - b]], [ps.r])
                    o3 = ps.t[:].rearrange("p (h n) -> p h n", n=128)
                    tt("dve", st[:, 32:40], o3[:, :, 64], esk[:, g * 8:(g + 1) * 8], ALU.add, [ps.r, r_esk], [r_st])
                    op("dve", lambda e: e.reciprocal(out=st[:, 32:40], in_=st[:, 32:40]), [r_st], [r_st])
                    tt("dve", h3(yb.t[:, g * 512:(g + 1) * 512]), o3[:, :, 0:64], bch(st[:, 32:40], 8), ALU.mult,
                       [ps.r, r_st], [yb.r])
                    pp.put(ps); pool.put(Ec, Ep)
                pool.put(*qT)
                dump("yb", yb.t[:], [P, D], [yb.r], t * P, P, S_LEN)

                pr = shifted_proj(b, t, 0, 1024)
                pk = shifted_proj(b, t, 1024, 1024)
                pv = shifted_proj(b, t, 2048, 1024)
                lw = shifted_proj(b, t, 3072, 256)
                act(lw.t[:, 0:64], lw.t[:, 0:64], AF.Tanh, [lw.r], [lw.r])
                act(lw.t[:, 128:256], lw.t[:, 128:256], AF.Sigmoid, [lw.r], [lw.r])
                ps = pp.get()
                tr(ps.t[:, 0:P], lw.t[:, 0:128], [lw.r], [ps.r])
                tr(ps.t[:, P:2 * P], lw.t[:, 128:256], [lw.r], [ps.r])
                act(lw.t[:, 256:512], ps.t[:, 0:256], AF.Copy, [ps.r], [lw.r]); pp.put(ps)
                sgT = lw.t[:, 384:512]
                sw = pool.get(); a_ = pool.get(); g_sb = pool.get()
                for (dst_, r0, voff) in ((sw, 0, 0), (a_, 64, D)):
                    ps = pp.get()
                    for hf in range(2):
                        cs = slice(hf * 512, (hf + 1) * 512)
                        mm(ps.t[:, cs], lw.t[r0:r0 + 64, 256:384], wa_up[r0:r0 + 64, cs], True, True, [lw.r, r_waup], [ps.r])
                    bb_ = bvec(w0a0_d[:, voff:voff + D])
                    tt("dve", dst_.t[:], ps.t[:], bb_.t[:], ALU.add, [ps.r, bb_.r], [dst_.r]); pp.put(ps)
                    pool.put(bb_)
                    act(dst_.t[:], dst_.t[:], AF.Sigmoid, [dst_.r], [dst_.r])
                ps = pp.get()
                for hf in range(2):
                    cs = slice(hf * 512, (hf + 1) * 512)
                    mm(ps.t[:, cs], sgT, g_up[:, cs], True, True, [lw.r, r_gup], [ps.r])
                act(g_sb.t[:], ps.t[:], AF.Copy, [ps.r], [g_sb.r]); pp.put(ps)
                pool.put(lw)
                eW = pool.get(); eWi = pool.get(); eWx = pool.get()
                ps = pp.get()
                for hf in range(2):
                    cs = slice(hf * 512, (hf + 1) * 512)
                    mm(ps.t[:, cs], TRI, sw.t[:, cs], True, True, [r_tri, sw.r], [ps.r])
                act(eW.t[:], ps.t[:], AF.Exp, [ps.r], [eW.r], scale=-C0)
                act(eWi.t[:], ps.t[:], AF.Exp, [ps.r], [eWi.r], scale=C0); pp.put(ps)
                ps = pp.get()
                for hf in range(2):
                    cs = slice(hf * 512, (hf + 1) * 512)
                    mm(ps.t[:, cs], TRIS, sw.t[:, cs], True, True, [r_tri, sw.r], [ps.r])
                act(eWx.t[:], ps.t[:], AF.Exp, [ps.r], [eWx.r], scale=-C0); pp.put(ps)
                pool.put(sw)
                kk = pool.get(); tmp = pool.get()
                kkb = bvec(vec6_d[0:1, :])
                tt("dve", kk.t[:], pk.t[:], kkb.t[:], ALU.mult, [pk.r, kkb.r], [kk.r])
                pool.put(kkb)
                act(tmp.t[:], kk.t[:], AF.Square, [kk.r], [tmp.r])
                red(st[:, 40:56], h3(tmp.t[:]), [tmp.r], [r_st])
                act(st[:, 40:56], st[:, 40:56], AF.Sqrt, [r_st], [r_st])
                ts("dve", st[:, 40:56], st[:, 40:56], 1e-12, None, ALU.max, None, [r_st], [r_st])
                op("dve", lambda e: e.reciprocal(out=st[:, 40:56], in_=st[:, 40:56]), [r_st], [r_st])
                tt("dve", h3(kk.t[:]), h3(kk.t[:]), bch(st[:, 40:56]), ALU.mult, [kk.r, r_st], [kk.r])
                kv_ = pool.get()
                kab = bvec(vec6_d[1:2, :])
                stt(tmp.t[:], a_.t[:], -1.0, kab.t[:], ALU.add, ALU.mult, [a_.r, kab.r, tmp.r], [tmp.r])
                pool.put(kab)
                stt(kv_.t[:], tmp.t[:], 1.0, pk.t[:], ALU.add, ALU.mult, [tmp.r, pk.r], [kv_.r])
                pool.put(pk)
                At = pool.get(); Bt = pool.get(); Kt = pool.get(); Rt = pool.get()
                stt(At.t[:], kk.t[:], -1.0, eWx.t[:], ALU.mult, ALU.mult, [kk.r, eWx.r], [At.r])
                tt(EW, Bt.t[:], kk.t[:], a_.t[:], ALU.mult, [kk.r, a_.r], [Bt.r])
                tt("dve", Bt.t[:], Bt.t[:], eWi.t[:], ALU.mult, [Bt.r, eWi.r], [Bt.r])
                tt("dve", Kt.t[:], kv_.t[:], eWi.t[:], ALU.mult, [kv_.r, eWi.r], [Kt.r])
                tt(EW, Rt.t[:], pr.t[:], eW.t[:], ALU.mult, [pr.r, eW.r], [Rt.r])
                pool.put(kk, a_, eWi, eWx)
                tt(EW, tmp.t[:], pr.t[:], kv_.t[:], ALU.mult, [pr.r, kv_.r, tmp.r], [tmp.r])
                rkb = bvec(vec6_d[2:3, :])
                tt("dve", tmp.t[:], tmp.t[:], rkb.t[:], ALU.mult, [tmp.r, rkb.r], [tmp.r])
                pool.put(rkb)
                red(st[:, 8:24], h3(tmp.t[:]), [tmp.r], [r_st])
                tt("dve", h3(tmp.t[:]), h3(pv.t[:]), bch(st[:, 8:24]), ALU.mult, [pv.r, r_st, tmp.r], [tmp.r])
                bonus = tmp
                pool.put(pr, kv_)
                ps = pp.get()
                for hd in range(16):
                    mm(ps.t[0:64, hd:hd + 1], eW.t[:, hd * 64:(hd + 1) * 64], ident[:, P - 1:P], True, True,
                       [eW.r, r_ident], [ps.r])
                act(WC[:], ps.t[0:64, 0:16], AF.Copy, [ps.r], [r_WC]); pp.put(ps)
                pool.put(eW)
                yrec = pool.get()
                m_s = TRIS.unsqueeze(1).to_broadcast([P, 4, P]); m_i = TRI.unsqueeze(1).to_broadcast([P, 4, P])
                m_l = TRIL.unsqueeze(1).to_broadcast([P, 4, P])
                def group_gen(hg):
                    FTa = pool.get(); FTb = pool.get()
                    for j in range(4):
                        hd = hg * 4 + j
                        hs = slice(hd * 64, (hd + 1) * 64)
                        ps = pp.get()
                        tr(ps.t[0:64, 0:128], At.t[:, hs], [At.r], [ps.r])
                        tr(ps.t[0:64, 128:256], Rt.t[:, hs], [Rt.r], [ps.r])
                        tr(ps.t[0:64, 256:384], Bt.t[:, hs], [Bt.r], [ps.r])
                        tr(ps.t[0:64, 384:512], Kt.t[:, hs], [Kt.r], [ps.r])
                        act(FTa.t[0:64, j * 256:(j + 1) * 256], ps.t[0:64, 0:256], AF.Copy, [ps.r], [FTa.r])
                        act(FTb.t[0:64, j * 256:(j + 1) * 256], ps.t[0:64, 256:512], AF.Copy, [ps.r], [FTb.r])
                        pp.put(ps)
                    yield
                    MX = pool.get(); Lp = pool.get(); Aak = pool.get(); AR = pool.get(); QU = pool.get()
                    MX4 = MX.t[:].rearrange("p (h s n) -> p h s n", h=4, s=2)
                    AR4 = AR.t[:].rearrange("p (h s n) -> p h s n", h=4, s=2)
                    psM = pp.get(); psK = pp.get(); psL = pp.get()
                    for j in range(4):
                        fa = FTa.t[0:64, j * 256:(j + 1) * 256]
                        mm(psM.t[:, j * 256:(j + 1) * 256], FTb.t[0:64, j * 256:j * 256 + 128], fa, True, True,
                           [FTa.r, FTb.r], [psM.r])
                        mm(psK.t[:, j * 256:(j + 1) * 256], FTb.t[0:64, j * 256 + 128:(j + 1) * 256], fa, True, True,
                           [FTa.r, FTb.r], [psK.r])
                        mm(psL.t[:, j * 128:(j + 1) * 128], FTa.t[0:64, j * 256:j * 256 + 128],
                           FTb.t[0:64, j * 256:j * 256 + 128], True, True, [FTa.r, FTb.r], [psL.r])
                    pM4 = psM.t[:].rearrange("p (h s n) -> p h s n", h=4, s=2)
                    pK4 = psK.t[:].rearrange("p (h s n) -> p h s n", h=4, s=2)
                    tt("dve", MX4[:, :, 0, :], pM4[:, :, 0, :], m_s, ALU.mult, [psM.r, r_tri], [MX.r])
                    tt("dve", AR4[:, :, 0, :], pM4[:, :, 1, :], m_i, ALU.mult, [psM.r, r_tri], [AR.r])
                    tt("dve", Aak.t[:, 0:512].rearrange("p (h n) -> p h n", n=P), pK4[:, :, 0, :], m_s, ALU.mult,
                       [psK.r, r_tri], [Aak.r])
                    tt("dve", AR4[:, :, 1, :], pK4[:, :, 1, :], m_i, ALU.mult, [psK.r, r_tri], [AR.r])
                    tt("dve", Lp.t[:, 0:512].rearrange("p (h n) -> p h n", n=P),
                       psL.t[:, 0:512].rearrange("p (h n) -> p h n", n=P), m_l, ALU.mult, [psL.r, r_tri], [Lp.r])
                    pp.put(psM, psK, psL)
                    pool.put(FTb)
                    yield
                    tt(EW, MX4[:, :, 1, :], MX4[:, :, 0, :], ident[:].unsqueeze(1).to_broadcast([P, 4, P]), ALU.add,
                       [MX.r, r_ident], [MX.r])
                    psQ = pp.get()
                    for j in range(4):
                        hd = hg * 4 + j
                        mm(psQ.t[:, j * 64:(j + 1) * 64], Aak.t[:, j * P:(j + 1) * P], pv.t[:, hd * 64:(hd + 1) * 64],
                           True, True, [Aak.r, pv.r], [psQ.r])
                    act(QU.t[:, 0:256], psQ.t[:, 0:256], AF.Copy, [psQ.r], [QU.r]); pp.put(psQ)
                    yield
                    p_ = 1
                    while p_ <= 64:
                        if p_ < 64:
                            psA = pp.get(); psB = pp.get()
                            for j in range(4):
                                if p_ == 1:
                                    mm(psA.t[:, j * 256:j * 256 + 128], Lp.t[:, j * P:(j + 1) * P], MX4[:, j, 0, :],
                                       True, True, [Lp.r, MX.r], [psA.r])
                                else:
                                    mm(psA.t[:, j * 256:(j + 1) * 256], Lp.t[:, j * P:(j + 1) * P],
                                       MX.t[:, j * 256:(j + 1) * 256], True, True, [Lp.r, MX.r], [psA.r])
                                mm(psB.t[:, j * P:(j + 1) * P], MX4[:, j, 0, :], Lp.t[:, j * P:(j + 1) * P], True, True,
                                   [Lp.r, MX.r], [psB.r])
                            pA4 = psA.t[:].rearrange("p (h s n) -> p h s n", h=4, s=2)
                            act(MX4[:, :, 0, :], pA4[:, :, 0, :], AF.Copy, [psA.r], [MX.r])
                            if p_ > 1:
                                tt("dve", MX4[:, :, 1, :], pA4[:, :, 1, :], MX4[:, :, 1, :], ALU.add, [psA.r, MX.r], [MX.r])
                            act(Lp.t[:, 0:512], psB.t[:, 0:512], AF.Copy, [psB.r], [Lp.r])
                            pp.put(psA, psB)
                        else:
                            psA = pp.get()
                            for j in range(4):
                                mm(psA.t[:, j * P:(j + 1) * P], Lp.t[:, j * P:(j + 1) * P], MX4[:, j, 1, :], True, True,
                                   [Lp.r, MX.r], [psA.r])
                            tt("dve", MX4[:, :, 1, :], psA.t[:, 0:512].rearrange("p (h n) -> p h n", n=P), MX4[:, :, 1, :],
                               ALU.add, [psA.r, MX.r], [MX.r])
                            pp.put(psA)
                        p_ *= 2
                        yield
                    psP = pp.get(); psU = pp.get()
                    for j in range(4):
                        hd = hg * 4 + j
                        mm(psP.t[0:64, j * P:(j + 1) * P], At.t[:, hd * 64:(hd + 1) * 64], MX4[:, j, 1, :], True, True,
                           [At.r, MX.r], [psP.r])
                        mm(psU.t[:, j * 64:(j + 1) * 64], MX4[:, j, 1, :], QU.t[:, j * 64:(j + 1) * 64], True, True,
                           [MX.r, QU.r], [psU.r])
                    P1g = Aak
                    act(P1g.t[0:64, 0:512], psP.t[0:64, 0:512], AF.Copy, [psP.r], [P1g.r])
                    act(QU.t[:, 256:512], psU.t[:, 0:256], AF.Copy, [psU.r], [QU.r])
                    pp.put(psP, psU)
                    pool.put(MX, Lp)
                    yield
                    psS = pp.get()
                    for j in range(4):
                        hd = hg * 4 + j
                        mm(psS.t[:, j * 64:(j + 1) * 64], P1g.t[0:64, j * P:(j + 1) * P], STt[:, hd, :], True, True,
                           [P1g.r, r_ST], [psS.r])
                    tt("dve", QU.t[:, 512:768], psS.t[:, 0:256], QU.t[:, 256:512], ALU.add, [psS.r, QU.r], [QU.r])
                    pp.put(psS)
                    yield
                    psY = pp.get(); psN = pp.get()
                    for j in range(4):
                        hd = hg * 4 + j
                        hs = slice(hd * 64, (hd + 1) * 64)
                        sa = QU.t[:, 512 + j * 64:512 + (j + 1) * 64]
                        mm(psY.t[:, j * 64:(j + 1) * 64], FTa.t[0:64, j * 256 + 128:(j + 1) * 256], STt[:, hd, :], True, False,
                           [FTa.r, r_ST], [psY.r])
                        mm(psY.t[:, j * 64:(j + 1) * 64], AR4[:, j, 0, :], sa, False, False, [AR.r, QU.r], [psY.r])
                        mm(psY.t[:, j * 64:(j + 1) * 64], AR4[:, j, 1, :], pv.t[:, hs], False, True, [AR.r, pv.r], [psY.r])
                        mm(psN.t[0:64, j * 64:(j + 1) * 64], Bt.t[:, hs], sa, True, False, [Bt.r, QU.r], [psN.r])
                        mm(psN.t[0:64, j * 64:(j + 1) * 64], Kt.t[:, hs], pv.t[:, hs], False, True, [Kt.r, pv.r], [psN.r])
                    act(yrec.t[:, hg * 256:(hg + 1) * 256], psY.t[:, 0:256], AF.Copy, [psY.r], [yrec.r]); pp.put(psY)
                    STg = STt[:, hg * 4:(hg + 1) * 4, :]
                    tt("dve", STg, psN.t[0:64, 0:256].rearrange("p (h v) -> p h v", v=64), STg, ALU.add,
                       [psN.r, r_ST], [r_ST])
                    tt("dve", STg, STg, WC[:, hg * 4:(hg + 1) * 4].unsqueeze(2).to_broadcast([64, 4, 64]), ALU.mult,
                       [r_ST, r_WC], [r_ST])
                    pp.put(psN)
                    pool.put(FTa, Aak, AR, QU)
                GPAR = 1
                active = []; nxt = 0
                while nxt < 4 or active:
                    while len(active) < GPAR and nxt < 4:
                        active.append(group_gen(nxt)); nxt += 1
                    for g_ in list(active):
                        try:
                            next(g_)
                        except StopIteration:
                            active.remove(g_)
                pool.put(Rt, At, Bt, Kt)
                dump("yrec", yrec.t[:], [P, D], [yrec.r], t * P, P, S_LEN)
                yc = yrec; sq = pool.get()
                red(st[:, 8:24], h3(yc.t[:]), [yc.r], [r_st])
                ts("dve", st[:, 8:24], st[:, 8:24], -1.0 / 64, None, ALU.mult, None, [r_st], [r_st])
                tt("dve", h3(yc.t[:]), h3(yc.t[:]), bch(st[:, 8:24]), ALU.add, [yc.r, r_st], [yc.r])
                act(sq.t[:], yc.t[:], AF.Square, [yc.r], [sq.r])
                red(st[:, 8:24], h3(sq.t[:]), [sq.r], [r_st])
                pool.put(sq)
                rstd_of(st[:, 8:24], 1.0 / 64, 64e-5, [r_st])
                tt("dve", h3(yc.t[:]), h3(yc.t[:]), bch(st[:, 8:24]), ALU.mult, [yc.r, r_st], [yc.r])
                lwb = bvec(vec6_d[3:4, :])
                tt(EW, yc.t[:], yc.t[:], lwb.t[:], ALU.mult, [yc.r, lwb.r], [yc.r])
                pool.put(lwb)
                lbb = bvec(vec6_d[4:5, :])
                tt(EW, yc.t[:], yc.t[:], lbb.t[:], ALU.add, [yc.r, lbb.r], [yc.r])
                pool.put(lbb)
                tt(EW, yc.t[:], yc.t[:], bonus.t[:], ALU.add, [yc.r, bonus.r], [yc.r])
                tt("dve", yc.t[:], yc.t[:], g_sb.t[:], ALU.mult, [yc.r, g_sb.r], [yc.r])
                pool.put(bonus, g_sb, pv)
                dump("ya", yc.t[:], [P, D], [yc.r], t * P, P, S_LEN)
                for (src_t, c0) in ((yc, 1280), (yb, 2304)):
                    ps = pp.get(); proj_cols(b, "r", c0, 1024, ps)
                    gsb = pool.get()
                    act(gsb.t[:], ps.t[:], AF.Sigmoid, [ps.r], [gsb.r]); pp.put(ps)
                    tt("dve", src_t.t[:], src_t.t[:], gsb.t[:], ALU.mult, [src_t.r, gsb.r], [src_t.r])
                    pool.put(gsb)
                tt(EW, yc.t[:], yc.t[:], yb.t[:], ALU.add, [yc.r, yb.r], [yc.r])
                pool.put(yb)
                dump("mixed", yc.t[:], [P, D], [yc.r], t * P, P, S_LEN)
                mT = pool.get()
                ps = pp.get()
                for kc in range(8):
                    tr(ps.t[:, kc * P:(kc + 1) * P], yc.t[:, kc * P:(kc + 1) * P], [yc.r], [ps.r])
                act(mT.t[:], ps.t[:], AF.Copy, [ps.r], [mT.r]); pp.put(ps)
                pool.put(yc)
                ps = pp.get()
                for g0 in range(0, 1024, WKW):
                    s1 = wslot()
                    dma("sp", wk[s1][:], wov[:, :, g0:g0 + WKW], [], [r_wk[s1]])
                    for kc in range(8):
                        mm(ps.t[:, g0:g0 + WKW], mT.t[:, kc * P:(kc + 1) * P], wk[s1][:, kc, :], kc == 0, kc == 7,
                           [mT.r, r_wk[s1]], [ps.r])
                x1 = pool.get()
                gt1 = bvec(ada_row(2))
                tt("dve", x1.t[:], ps.t[:], gt1.t[:], ALU.mult, [ps.r, gt1.r], [x1.r]); pp.put(ps)
                pool.put(gt1)
                tt(EW, x1.t[:], x1.t[:], xt[b][:], ALU.add, [x1.r, r_xt[b]], [x1.r])
                pool.put(mT)
                dump("x1", x1.t[:], [P, D], [x1.r], t * P, P, S_LEN)
                drain()
                if 2 in phases:
                    gen[0] = peer_tile(t, x1)
                else:
                    ev = dma("sp", out_d[t * P:(t + 1) * P, :], x1.t[:], [x1.r], [])
                    out_evs.append(ev)
                    pool.put(x1)
            drain()
            op = base_op
            print("sbuf left", nc.sbuf_bytes_remaining, "pool peak", pool.peak, "psum peak", pp.peak, "gs peak", gs.peak, "ops", S.n_ops)
            S.barrier()
            S.emit()

        S.final_wait("sp", out_evs)
    S.emit()
    return nc


def pack_inputs(x, c, p):
    f = np.float32
    A = lambda a: np.ascontiguousarray(np.asarray(a, f))
    d = {
        "x": A(x), "cT": A(np.asarray(c, f).reshape(8, 128).T),
        "ada_w": A(p["ada_w"]), "ada_b": A(p["ada_b"]).reshape(1, -1),
        "norm1_w": A(p["norm1_w"]).reshape(1, -1), "norm2_w": A(p["norm2_w"]).reshape(1, -1),
        "w_in": A(p["w_in"]), "shift_mu": A(p["shift_mu"]).reshape(1, -1),
        "w0a0": A(np.concatenate([np.asarray(p["w0"], f), np.asarray(p["a0"], f)])).reshape(1, -1),
        "wa_up": A(np.concatenate([np.asarray(p["w_lora_up"], f), np.asarray(p["a_lora_up"], f)], 0)),
        "g_up": A(p["g_lora_up"]),
        "vec6": A(np.stack([np.asarray(p[k], f).reshape(-1) for k in ("k_k", "k_a", "r_k", "lnx_w", "lnx_b", "lnx_b")])),
        "qk_nw": A(np.concatenate([np.asarray(p["q_norm_w"], f), np.asarray(p["k_norm_w"], f)])).reshape(1, -1),
        "sinks": A(p["sinks"]).reshape(1, -1),
        "w_out": A(p["w_out"]),
    }
    if "peer_w_q" in p:
        d["peer_w_q"] = A(p["peer_w_q"])
        d["keysT"] = A(np.concatenate([np.asarray(p["peer_keys_1"], f).T, np.asarray(p["peer_keys_2"], f).T], 1))
        d["peer_u"] = A(p["peer_u"])
        d["peer_v"] = A(p["peer_v"])
    return d


def kernel(**inputs):
    x = np.asarray(inputs["x"], np.float32)
    c = np.asarray(inputs["c"], np.float32)
    B, S_LEN, _ = x.shape
    p = {k: np.asarray(v, np.float32)[0] for k, v in inputs.items() if k not in ("x", "c")}
    nc = build(S_LEN)
    in_maps = [pack_inputs(x[b], c[b], p) for b in range(B)]
    res = run_bass_kernel_spmd(nc, in_maps, core_ids=list(range(B)))
    return np.stack([np.asarray(r["out"], np.float32) for r in res.results], 0)
```
